# Optimizing a Trainium2 kernel written in Bass

```python
import math
import jax, jax.numpy as jnp
from jax import lax
import numpy as np

D_MODEL = 2048
BATCH = 8
SEQ = 2048
DEPTH = 2

N_MIXERS = 2
RMS_EPS = 1e-6
D_SSM = D_MODEL // 2
SSM_GROUP_CH = 16
N_SSM_GROUPS = D_SSM // SSM_GROUP_CH
SSM_STATE = 64
STEP_MIN = 1e-3
STEP_MAX = 1e-1
FOX_HEAD_DIM = 128
FOX_HEADS = D_MODEL // FOX_HEAD_DIM
Q_BLOCK = 128
FORGET_BIAS_LO = 1.0
FORGET_BIAS_HI = 4.0
N_EXPERT_GROUPS = 8
EXPERTS_PER_GROUP = 8
N_EXPERTS = N_EXPERT_GROUPS * EXPERTS_PER_GROUP
TOP_K = 2
D_EXPERT = D_MODEL // 4
DISPATCH_BLOCK = 256

kernel_name = "hybrid_s5_fox_hier_moe"


def rms_norm(x, gain):
    xf = x.astype(jnp.float32)
    y = xf * lax.rsqrt(jnp.mean(xf * xf, axis=-1, keepdims=True) + RMS_EPS)
    return (y * gain.astype(jnp.float32)).astype(x.dtype)


def _diag_complex_combine(e1, e2):
    a1r, a1i, b1r, b1i = e1
    a2r, a2i, b2r, b2i = e2
    return (a2r * a1r - a2i * a1i,
            a2r * a1i + a2i * a1r,
            a2r * b1r - a2i * b1i + b2r,
            a2r * b1i + a2i * b1r + b2i)


def s5_mixer(h, w_in, lam_re, lam_im, b_re, b_im, c_re, c_im, d_skip, log_step, w_glu, b_glu, w_out):
    f32 = jnp.float32
    bsz, seq, _ = h.shape
    u = h @ w_in
    uf = u.astype(f32).reshape(bsz, seq, N_SSM_GROUPS, SSM_GROUP_CH)
    lr = lam_re.astype(f32)
    li = lam_im.astype(f32)
    step = jnp.exp(log_step.astype(f32))[:, None]
    mag = jnp.exp(lr * step)
    ab_re = mag * jnp.cos(li * step)
    ab_im = mag * jnp.sin(li * step)
    den = lr * lr + li * li
    nr = ab_re - 1.0
    ni = ab_im
    fr = (nr * lr + ni * li) / den
    fi = (ni * lr - nr * li) / den
    br = b_re.astype(f32)
    bi = b_im.astype(f32)
    bb_re = fr[..., None] * br - fi[..., None] * bi
    bb_im = fr[..., None] * bi + fi[..., None] * br
    bu_re = jnp.einsum('bsgc,gnc->sbgn', uf, bb_re)
    bu_im = jnp.einsum('bsgc,gnc->sbgn', uf, bb_im)
    a_re = jnp.broadcast_to(ab_re, (seq, 1) + ab_re.shape)
    a_im = jnp.broadcast_to(ab_im, (seq, 1) + ab_im.shape)
    _, _, x_re, x_im = lax.associative_scan(_diag_complex_combine, (a_re, a_im, bu_re, bu_im), axis=0)
    y = (jnp.einsum('sbgn,gcn->bsgc', x_re, c_re.astype(f32))
         - jnp.einsum('sbgn,gcn->bsgc', x_im, c_im.astype(f32)))
    y = y.reshape(bsz, seq, D_SSM) + d_skip.astype(f32) * u.astype(f32)
    y = y.astype(h.dtype)
    g = jax.nn.gelu(y)
    out = g * jax.nn.sigmoid(g @ w_glu + b_glu)
    return out @ w_out


def fox_mixer(h, w_in, b_forget, w_out):
    f32 = jnp.float32
    bsz, seq, _ = h.shape
    proj = h @ w_in
    q, k, v, gate, f_logit = jnp.split(proj, [D_MODEL, 2 * D_MODEL, 3 * D_MODEL, 4 * D_MODEL], axis=-1)

    def heads(t):
        return t.reshape(bsz, seq, FOX_HEADS, FOX_HEAD_DIM).transpose(0, 2, 1, 3)

    q, k, v = heads(q), heads(k), heads(v)
    log_f = jax.nn.log_sigmoid((f_logit + b_forget).astype(f32))
    cum = jnp.cumsum(log_f, axis=1).transpose(0, 2, 1)
    n_blocks = seq // Q_BLOCK
    q_blocks = q.reshape(bsz, FOX_HEADS, n_blocks, Q_BLOCK, FOX_HEAD_DIM).transpose(2, 0, 1, 3, 4)
    c_blocks = cum.reshape(bsz, FOX_HEADS, n_blocks, Q_BLOCK).transpose(2, 0, 1, 3)
    key_pos = jnp.arange(seq)
    scale = FOX_HEAD_DIM ** -0.5

    def one_block(args):
        qb, cqb, blk = args
        q_pos = blk * Q_BLOCK + jnp.arange(Q_BLOCK)
        logits = jnp.einsum('bhqd,bhkd->bhqk', qb, k).astype(f32) * scale
        logits = logits + (cqb[..., :, None] - cum[:, :, None, :])
        logits = jnp.where(key_pos[None, :] <= q_pos[:, None], logits, -jnp.inf)
        probs = jax.nn.softmax(logits, axis=-1).astype(v.dtype)
        return jnp.einsum('bhqk,bhkd->bhqd', probs, v)

    o = lax.map(one_block, (q_blocks, c_blocks, jnp.arange(n_blocks)))
    o = o.transpose(1, 0, 3, 2, 4).reshape(bsz, seq, D_MODEL)
    o = o * jax.nn.sigmoid(gate)
    return o @ w_out


def hier_moe(h, w_group, b_group, w_expert, b_expert, w_gate, w_up, w_down):
    f32 = jnp.float32
    bsz, seq, d = h.shape
    xt = h.reshape(-1, d)
    n_tok = xt.shape[0]
    group_logits = (xt @ w_group + b_group).astype(f32)
    group_probs = jax.nn.softmax(group_logits, axis=-1)
    g_sel = jnp.argmax(group_logits, axis=-1)
    p_group = jnp.take_along_axis(group_probs, g_sel[:, None], axis=1)[:, 0]
    expert_logits = (xt @ w_expert + b_expert).astype(f32).reshape(n_tok, N_EXPERT_GROUPS, EXPERTS_PER_GROUP)
    in_group = jnp.take_along_axis(expert_logits, g_sel[:, None, None], axis=1)[:, 0]
    top_logits, top_local = lax.top_k(in_group, TOP_K)
    top_w = jax.nn.softmax(top_logits, axis=-1) * p_group[:, None]
    expert_ids = g_sel[:, None] * EXPERTS_PER_GROUP + top_local

    n_pairs = n_tok * TOP_K
    flat_e = expert_ids.reshape(-1).astype(jnp.int32)
    flat_w = top_w.reshape(-1)
    flat_tok = jnp.repeat(jnp.arange(n_tok, dtype=jnp.int32), TOP_K)
    order = jnp.argsort(flat_e)
    se, stok, sw = flat_e[order], flat_tok[order], flat_w[order]
    counts = jnp.zeros((N_EXPERTS,), jnp.int32).at[flat_e].add(1)
    padded = ((counts + DISPATCH_BLOCK - 1) // DISPATCH_BLOCK) * DISPATCH_BLOCK
    start = jnp.cumsum(counts) - counts
    pend = jnp.cumsum(padded)
    pstart = pend - padded
    dest = pstart[se] + (jnp.arange(n_pairs, dtype=jnp.int32) - start[se])
    n_blocks = -(-n_pairs // DISPATCH_BLOCK) + N_EXPERTS
    rows = n_blocks * DISPATCH_BLOCK
    x_disp = jnp.zeros((rows, d), h.dtype).at[dest].set(xt[stok])
    w_disp = jnp.zeros((rows,), f32).at[dest].set(sw)
    tok_disp = jnp.full((rows,), n_tok, jnp.int32).at[dest].set(stok)
    block_start = jnp.arange(n_blocks, dtype=jnp.int32) * DISPATCH_BLOCK
    block_expert = jnp.minimum(jnp.searchsorted(pend, block_start, side='right'), N_EXPERTS - 1)

    def expert_block(args):
        xb, e = args
        hid = jax.nn.silu(xb @ w_gate[e]) * (xb @ w_up[e])
        return hid @ w_down[e]

    y_disp = lax.map(expert_block, (x_disp.reshape(n_blocks, DISPATCH_BLOCK, d), block_expert)).reshape(rows, d)
    y_disp = y_disp * w_disp[:, None].astype(y_disp.dtype)
    out = jnp.zeros((n_tok, d), h.dtype).at[tok_disp].add(y_disp, mode='drop')
    return out.reshape(bsz, seq, d)


def _normal(key, shape, scale):
    return jax.random.normal(key, shape, jnp.float32) * scale


def _gain(key):
    return 1.0 + _normal(key, (D_MODEL,), 0.02)


def _moe_params(key):
    k = jax.random.split(key, 7)
    return (
        _normal(k[0], (D_MODEL, N_EXPERT_GROUPS), D_MODEL ** -0.5),
        _normal(k[1], (N_EXPERT_GROUPS,), 0.01),
        _normal(k[2], (D_MODEL, N_EXPERTS), D_MODEL ** -0.5),
        _normal(k[3], (N_EXPERTS,), 0.01),
        _normal(k[4], (N_EXPERTS, D_MODEL, D_EXPERT), D_MODEL ** -0.5),
        _normal(k[5], (N_EXPERTS, D_MODEL, D_EXPERT), D_MODEL ** -0.5),
        _normal(k[6], (N_EXPERTS, D_EXPERT, D_MODEL), D_EXPERT ** -0.5),
    )


def setup_inputs(seed: int = 0) -> dict:
    key = jax.random.key(seed)
    ks = jax.random.split(key, 24)
    x = jax.random.normal(ks[0], (BATCH, SEQ, D_MODEL), jnp.float32)
    n_idx = jnp.arange(SSM_STATE, dtype=jnp.float32)
    lam_re = -0.5 + _normal(ks[2], (N_SSM_GROUPS, SSM_STATE), 0.01)
    lam_im = math.pi * n_idx[None, :] + _normal(ks[3], (N_SSM_GROUPS, SSM_STATE), 0.01)
    log_step = jax.random.uniform(ks[4], (N_SSM_GROUPS,), jnp.float32, math.log(STEP_MIN), math.log(STEP_MAX))
    b_scale = (2.0 * SSM_GROUP_CH) ** -0.5
    c_scale = SSM_STATE ** -0.5
    m0 = _moe_params(ks[14])
    m1 = _moe_params(ks[15])
    return {
        "x": x,
        "l0_mix_norm": _gain(ks[1]),
        "l0_s5_w_in": _normal(ks[5], (D_MODEL, D_SSM), D_MODEL ** -0.5),
        "l0_s5_lambda_re": lam_re,
        "l0_s5_lambda_im": lam_im,
        "l0_s5_b_re": _normal(ks[6], (N_SSM_GROUPS, SSM_STATE, SSM_GROUP_CH), b_scale),
        "l0_s5_b_im": _normal(ks[7], (N_SSM_GROUPS, SSM_STATE, SSM_GROUP_CH), b_scale),
        "l0_s5_c_re": _normal(ks[8], (N_SSM_GROUPS, SSM_GROUP_CH, SSM_STATE), c_scale),
        "l0_s5_c_im": _normal(ks[9], (N_SSM_GROUPS, SSM_GROUP_CH, SSM_STATE), c_scale),
        "l0_s5_d": _normal(ks[10], (D_SSM,), 1.0),
        "l0_s5_log_step": log_step,
        "l0_s5_w_glu": _normal(ks[11], (D_SSM, D_SSM), D_SSM ** -0.5),
        "l0_s5_b_glu": _normal(ks[12], (D_SSM,), 0.01),
        "l0_s5_w_out": _normal(ks[13], (D_SSM, D_MODEL), D_SSM ** -0.5),
        "l0_ffn_norm": _gain(ks[16]),
        "l0_moe_w_group": m0[0],
        "l0_moe_b_group": m0[1],
        "l0_moe_w_expert": m0[2],
        "l0_moe_b_expert": m0[3],
        "l0_moe_w_gate": m0[4],
        "l0_moe_w_up": m0[5],
        "l0_moe_w_down": m0[6],
        "l1_mix_norm": _gain(ks[17]),
        "l1_fox_w_in": _normal(ks[18], (D_MODEL, 4 * D_MODEL + FOX_HEADS), D_MODEL ** -0.5),
        "l1_fox_b_forget": jax.random.uniform(ks[19], (FOX_HEADS,), jnp.float32, FORGET_BIAS_LO, FORGET_BIAS_HI),
        "l1_fox_w_out": _normal(ks[20], (D_MODEL, D_MODEL), D_MODEL ** -0.5),
        "l1_ffn_norm": _gain(ks[21]),
        "l1_moe_w_group": m1[0],
        "l1_moe_b_group": m1[1],
        "l1_moe_w_expert": m1[2],
        "l1_moe_b_expert": m1[3],
        "l1_moe_w_gate": m1[4],
        "l1_moe_w_up": m1[5],
        "l1_moe_w_down": m1[6],
        "final_norm": _gain(ks[22]),
    }


def reference(x, l0_mix_norm, l0_s5_w_in, l0_s5_lambda_re, l0_s5_lambda_im, l0_s5_b_re, l0_s5_b_im,
              l0_s5_c_re, l0_s5_c_im, l0_s5_d, l0_s5_log_step, l0_s5_w_glu, l0_s5_b_glu, l0_s5_w_out,
              l0_ffn_norm, l0_moe_w_group, l0_moe_b_group, l0_moe_w_expert, l0_moe_b_expert,
              l0_moe_w_gate, l0_moe_w_up, l0_moe_w_down,
              l1_mix_norm, l1_fox_w_in, l1_fox_b_forget, l1_fox_w_out,
              l1_ffn_norm, l1_moe_w_group, l1_moe_b_group, l1_moe_w_expert, l1_moe_b_expert,
              l1_moe_w_gate, l1_moe_w_up, l1_moe_w_down,
              final_norm):
    mix_norms = [l0_mix_norm, l1_mix_norm]
    ffn_norms = [l0_ffn_norm, l1_ffn_norm]
    mixer_params = [
        (l0_s5_w_in, l0_s5_lambda_re, l0_s5_lambda_im, l0_s5_b_re, l0_s5_b_im, l0_s5_c_re, l0_s5_c_im,
         l0_s5_d, l0_s5_log_step, l0_s5_w_glu, l0_s5_b_glu, l0_s5_w_out),
        (l1_fox_w_in, l1_fox_b_forget, l1_fox_w_out),
    ]
    moe_params = [
        (l0_moe_w_group, l0_moe_b_group, l0_moe_w_expert, l0_moe_b_expert, l0_moe_w_gate, l0_moe_w_up, l0_moe_w_down),
        (l1_moe_w_group, l1_moe_b_group, l1_moe_w_expert, l1_moe_b_expert, l1_moe_w_gate, l1_moe_w_up, l1_moe_w_down),
    ]
    h = x
    for i in range(DEPTH):
        hn = rms_norm(h, mix_norms[i])
        if i % N_MIXERS == 0:
            h = h + s5_mixer(hn, *mixer_params[i])
        else:
            h = h + fox_mixer(hn, *mixer_params[i])
        h = h + hier_moe(rms_norm(h, ffn_norms[i]), *moe_params[i])
    return rms_norm(h, final_norm)
```

```python
import numpy as np
from contextlib import ExitStack
import concourse.bass as bass
import concourse.mybir as mybir
from concourse.bass_utils import run_bass_kernel_spmd

F32 = mybir.dt.float32
BF16 = mybir.dt.bfloat16
I32 = mybir.dt.int32
AF = mybir.ActivationFunctionType
ALU = mybir.AluOpType
AX = mybir.AxisListType

D = 2048
S = 2048
NT = S // 128
NF = D // 128
DS = 1024
NQ = DS // 128
NPAIR = 32
NH = 16
EPS = 1e-6
NG = 8
EPG = 8
NE = 64
DE = 512
CAP = 384
NST = CAP // 128

SAME_SYNC = True


class Buf:
    __slots__ = ("w", "r", "ds", "name")

    def __init__(self, name=""):
        self.w = None
        self.r = {}
        self.ds = None
        self.name = name


class K:
    def __init__(self):
        nc = bass.Bass("TRN2", target_bir_lowering=False)
        self.nc = nc
        self.eng = dict(pe=nc.tensor, dve=nc.vector, act=nc.scalar, pool=nc.gpsimd, sp=nc.sync)
        self.sem = {k: nc.alloc_semaphore("s_" + k) for k in self.eng}
        self.cnt = {k: 0 for k in self.eng}
        self.seen = {k: {} for k in self.eng}
        self.dpool = []
        self.dall = []
        self.nbuf = 0
        self.ps = []
        self.psb = []
        for i in range(8):
            t = nc.alloc_psum_tensor("ps%d" % i, [128, 512], F32)
            self.ps.append(t)
            self.psb.append(Buf("ps%d" % i))

    def _dslot(self, b):
        if b.ds is None:
            if self.dpool:
                b.ds = self.dpool.pop()
            else:
                b.ds = [self.nc.alloc_semaphore("d%d" % len(self.dall)), 0]
                self.dall.append(b.ds)
        return b.ds

    def release(self, bufs):
        for b in bufs:
            if b.ds is not None:
                self.dpool.append(b.ds)
                b.ds = None

    def _waits(self, eng, reads, writes):
        need = {}

        def add(t):
            k, v = t
            kk = id(k) if isinstance(k, list) else k
            if kk not in need or need[kk][1] < v:
                need[kk] = (k, v)

        for b in reads:
            if b.w:
                add(b.w)
        for b in writes:
            if b.w:
                add(b.w)
            for t in b.r.values():
                add(t)
        e = self.eng[eng]
        for kk, (k, v) in need.items():
            if isinstance(k, list):
                v = max(v, k[1])
                h = k[0]
            else:
                if k == eng and (eng == "pe" or not SAME_SYNC):
                    continue
                h = self.sem[k]
            if self.seen[eng].get(kk, 0) >= v:
                continue
            e.wait_ge(h, v)
            self.seen[eng][kk] = v

    def op(self, eng, emit, reads=(), writes=()):
        self._waits(eng, reads, writes)
        ins = emit(self.eng[eng])
        self.cnt[eng] += 1
        ins.then_inc(self.sem[eng], 1)
        t = (eng, self.cnt[eng])
        for b in reads:
            b.r[eng] = t
        for b in writes:
            b.w = t
            b.r = {}
        return ins

    def dma(self, q, out, in_, sb, reads=(), writes=()):
        self._waits(q, reads, writes)
        ds = self._dslot(sb)
        ins = self.eng[q].dma_start(out=out, in_=in_)
        ds[1] += 16
        ins.then_inc(ds[0], 16)
        t = (ds, ds[1])
        for b in reads:
            b.r[id(ds)] = t
        for b in writes:
            b.w = t
            b.r = {}
        return ins

    def barrier(self):
        for e in self.eng:
            for k in self.eng:
                if k == e:
                    continue
                v = self.cnt[k]
                if v and self.seen[e].get(k, 0) < v:
                    self.eng[e].wait_ge(self.sem[k], v)
                    self.seen[e][k] = v
            for ds in self.dall:
                if ds[1] and self.seen[e].get(id(ds), 0) < ds[1]:
                    self.eng[e].wait_ge(ds[0], ds[1])
                    self.seen[e][id(ds)] = ds[1]

    def mm(self, bank_ap, bank_buf, lhsT, rhs, start, stop, reads):
        return self.op("pe", lambda e: e.matmul(bank_ap, lhsT, rhs, start=start, stop=stop),
                       reads=reads, writes=[bank_buf])

    def tr(self, out_ap, out_buf, in_ap, ident, reads):
        return self.op("pe", lambda e: e.transpose(out_ap, in_ap, ident), reads=reads, writes=[out_buf])


class Scope:
    def __init__(self, k):
        self.k = k
        self.es = ExitStack()
        self.bufs = []

    def __enter__(self):
        self.es.__enter__()
        return self

    def __exit__(self, *a):
        self.k.barrier()
        self.k.release(self.bufs)
        return self.es.__exit__(*a)

    def sb(self, name, shape, dt):
        self.k.nbuf += 1
        t = self.es.enter_context(self.k.nc.sbuf_tensor("%s_%d" % (name, self.k.nbuf), list(shape), dt))
        b = Buf(name)
        self.bufs.append(b)
        return t, b

    def buf(self, name=""):
        b = Buf(name)
        self.bufs.append(b)
        return b


def make_ident(k, sc):
    it, ib = sc.sb("iota_i", [128, 128], I32)
    k.op("pool", lambda e: e.iota(it[:], pattern=[[1, 128]], base=0, channel_multiplier=-1), writes=[ib])
    idb, idbb = sc.sb("ident_bf", [128, 128], BF16)
    idf, idfb = sc.sb("ident_f", [128, 128], F32)
    k.op("dve", lambda e: e.tensor_scalar(idb[:], it[:], 0, None, op0=ALU.is_equal), reads=[ib], writes=[idbb])
    k.op("dve", lambda e: e.tensor_scalar(idf[:], it[:], 0, None, op0=ALU.is_equal), reads=[ib], writes=[idfb])
    return (idb, idbb), (idf, idfb)


def rms_tile(k, sc, x_t, x_b, gain_t, gain_b, out_t, out_b, tmp):
    junk, junkb, ss, ssb = tmp["junk"], tmp["junkb"], tmp["ss"], tmp["ssb"]
    k.op("act", lambda e: e.activation(out=junk[:], in_=x_t, func=AF.Square, accum_out=ss[:, 0:1]),
         reads=[x_b], writes=[junkb, ssb])
    k.op("dve", lambda e: e.tensor_scalar(ss[:, 1:2], ss[:, 0:1], 1.0 / D, EPS, op0=ALU.mult, op1=ALU.add),
         reads=[ssb], writes=[ssb])
    k.op("act", lambda e: e.activation(out=ss[:, 2:3], in_=ss[:, 1:2], func=AF.Sqrt), reads=[ssb], writes=[ssb])
    k.op("dve", lambda e: e.reciprocal(ss[:, 3:4], ss[:, 2:3]), reads=[ssb], writes=[ssb])
    k.op("dve", lambda e: e.scalar_tensor_tensor(out=out_t, in0=x_t, scalar=ss[:, 3:4], in1=gain_t,
                                                 op0=ALU.mult, op1=ALU.mult),
         reads=[x_b, ssb, gain_b], writes=[out_b])


def rms_tmp(sc):
    junk, junkb = sc.sb("junk", [128, D], BF16)
    ss, ssb = sc.sb("ss", [128, 4], F32)
    return dict(junk=junk, junkb=junkb, ss=ss, ssb=ssb)


def small_ops(k, buf):
    def tt(out, a, b, op, extra_r=(), eng="dve"):
        k.op(eng, lambda e: e.tensor_tensor(out=out, in0=a, in1=b, op=op), reads=[buf, *extra_r], writes=[buf])

    def ts(out, a, s1, s2, op0, op1=None, extra_r=(), eng="dve"):
        if op1 is None:
            k.op(eng, lambda e: e.tensor_scalar(out, a, s1, None, op0=op0), reads=[buf, *extra_r], writes=[buf])
        else:
            k.op(eng, lambda e: e.tensor_scalar(out, a, s1, s2, op0=op0, op1=op1), reads=[buf, *extra_r], writes=[buf])

    def stt(out, a, sc, b, op0, op1, extra_r=()):
        k.op("dve", lambda e: e.scalar_tensor_tensor(out=out, in0=a, scalar=sc, in1=b, op0=op0, op1=op1),
             reads=[buf, *extra_r], writes=[buf])

    def act(out, a, func, scale=None, bias=None, extra_r=()):
        kw = {}
        if scale is not None:
            kw["scale"] = scale
        if bias is not None:
            kw["bias"] = bias
        k.op("act", lambda e: e.activation(out=out, in_=a, func=func, **kw), reads=[buf, *extra_r], writes=[buf])
    return tt, ts, stt, act


def phase_s5(k, A, x_in, h_out):
    with Scope(k) as sc0:
        uT, _ = sc0.sb("uT", [128, NQ, S], F32)
        ub = [[sc0.buf() for _ in range(4)] for _ in range(NQ)]
        (idb, idbb), _ = make_ident(k, sc0)
        with Scope(k) as sc:
            gain, gainb = sc.sb("gain", [128, D], F32)
            k.dma("sp", gain[:], A["l0_mix_norm"], gainb, writes=[gainb])
            win, winb = sc.sb("win", [128, NF, DS], BF16)
            wv = A["l0_s5_w_in"].rearrange("(fc p) n -> p fc n", p=128)
            for i in range(4):
                k.dma("pool", win[:, 4 * i:4 * i + 4, :], wv[:, 4 * i:4 * i + 4, :], winb, writes=[winb])
            tmp = rms_tmp(sc)
            xt = [sc.sb("xt", [128, D], F32) for _ in range(2)]
            hn = [sc.sb("hn", [128, D], BF16) for _ in range(2)]
            hnT = [sc.sb("hnT", [128, NF, 512], BF16) for _ in range(2)]
            for st in range(4):
                hT, hTb = hnT[st % 2]
                for tl in range(4):
                    tt_ = st * 4 + tl
                    x_t, x_b = xt[tt_ % 2]
                    h_t, h_b = hn[tt_ % 2]
                    k.dma("sp", x_t[:], x_in[tt_ * 128:(tt_ + 1) * 128, :], x_b, writes=[x_b])
                    rms_tile(k, sc, x_t[:], x_b, gain[:], gainb, h_t[:], h_b, tmp)
                    for half in range(2):
                        bk = (tt_ % 2) * 2 + half
                        pv = k.ps[bk][:].bitcast(BF16)
                        for j in range(8):
                            fc = half * 8 + j
                            k.tr(pv[:, j * 128:(j + 1) * 128], k.psb[bk], h_t[:, fc * 128:(fc + 1) * 128], idb[:],
                                 reads=[h_b, idbb])
                        src = pv.rearrange("p (a b) -> p a b", a=8)
                        dst = hT[:, half * 8:half * 8 + 8, tl * 128:(tl + 1) * 128]
                        if half == 0:
                            k.op("act", lambda e: e.activation(out=dst, in_=src, func=AF.Copy),
                                 reads=[k.psb[bk]], writes=[hTb])
                        else:
                            k.op("dve", lambda e: e.tensor_copy(out=dst, in_=src), reads=[k.psb[bk]], writes=[hTb])
                for q in range(NQ):
                    bk = 4 + (q % 4)
                    for fc in range(NF):
                        k.mm(k.ps[bk][:], k.psb[bk], win[:, fc, q * 128:(q + 1) * 128], hT[:, fc, :],
                             fc == 0, fc == NF - 1, reads=[winb, hTb])
                    k.op("act", lambda e: e.activation(out=uT[:, q, st * 512:(st + 1) * 512], in_=k.ps[bk][:], func=AF.Copy),
                         reads=[k.psb[bk]], writes=[ub[q][st]])
        with Scope(k) as sc1:
            gT, _ = sc1.sb("gT", [128, NQ, S], BF16)
            gb = [[sc1.buf() for _ in range(4)] for _ in range(NQ)]
            with Scope(k) as sc:
                prm, prmb = sc.sb("prm", [128, 24, NPAIR], F32)
                lam, lamb = sc.sb("lam", [128, 3, NPAIR], F32)
                k.dma("sp", lam[:], A["s5_lam"], lamb, writes=[lamb])
                CD, _ = sc.sb("CD", [128, 11, NPAIR], F32)
                SD, _ = sc.sb("SD", [128, 11, NPAIR], F32)
                dsk, dskb = sc.sb("dsk", [128, NQ], F32)
                k.dma("sp", dsk[:], A["s5_d"], dskb, writes=[dskb])
                bwr, bwrb = sc.sb("bwr", [128, NQ, 4, 128], BF16)
                bwi, bwib = sc.sb("bwi", [128, NQ, 4, 128], BF16)
                k.dma("pool", bwr[:], A["s5_bw_re"], bwrb, writes=[bwrb])
                k.dma("pool", bwi[:], A["s5_bw_im"], bwib, writes=[bwib])
                cfr, cfrb = sc.sb("cfr", [128, NPAIR, 128], BF16)
                cfi, cfib = sc.sb("cfi", [128, NPAIR, 128], BF16)
                tt, ts, stt, act = small_ops(k, prmb)
                P = lambda i: prm[:, i, :]
                lr, li, lst = lam[:, 0, :], lam[:, 1, :], lam[:, 2, :]
                act(P(0), lst, AF.Exp, extra_r=[lamb])
                tt(P(1), lr, P(0), ALU.mult, extra_r=[lamb])
                tt(P(2), li, P(0), ALU.mult, extra_r=[lamb])
                act(P(3), P(1), AF.Exp)
                ts(P(4), P(2), 1.0 / 64, None, ALU.mult)
                tt(P(5), P(4), P(4), ALU.mult)
                ts(P(6), P(5), -1.0 / 42, 1.0, ALU.mult, ALU.add)
                tt(P(6), P(6), P(5), ALU.mult)
                ts(P(6), P(6), -1.0 / 20, 1.0, ALU.mult, ALU.add)
                tt(P(6), P(6), P(5), ALU.mult)
                ts(P(6), P(6), -1.0 / 6, 1.0, ALU.mult, ALU.add)
                tt(P(7), P(6), P(4), ALU.mult)
                ts(P(6), P(5), -1.0 / 56, 1.0, ALU.mult, ALU.add)
                tt(P(6), P(6), P(5), ALU.mult)
                ts(P(6), P(6), -1.0 / 30, 1.0, ALU.mult, ALU.add)
                tt(P(6), P(6), P(5), ALU.mult)
                ts(P(6), P(6), -1.0 / 12, 1.0, ALU.mult, ALU.add)
                tt(P(6), P(6), P(5), ALU.mult)
                ts(P(8), P(6), -0.5, 1.0, ALU.mult, ALU.add)
                ci, si = 8, 7
                free = [9, 10]
                for _ in range(6):
                    cn, sn = free
                    tt(P(11), P(ci), P(ci), ALU.mult)
                    tt(P(12), P(si), P(si), ALU.mult)
                    tt(P(cn), P(11), P(12), ALU.subtract)
                    stt(P(sn), P(ci), 2.0, P(si), ALU.mult, ALU.mult)
                    free = [ci, si]
                    ci, si = cn, sn
                k.op("dve", lambda e: e.tensor_copy(out=CD[:, 0, :], in_=P(ci)), reads=[prmb], writes=[prmb])
                k.op("dve", lambda e: e.tensor_copy(out=SD[:, 0, :], in_=P(si)), reads=[prmb], writes=[prmb])
                for kk in range(10):
                    tt(P(11), CD[:, kk, :], CD[:, kk, :], ALU.mult)
                    tt(P(12), SD[:, kk, :], SD[:, kk, :], ALU.mult)
                    tt(CD[:, kk + 1, :], P(11), P(12), ALU.subtract)
                    stt(SD[:, kk + 1, :], CD[:, kk, :], 2.0, SD[:, kk, :], ALU.mult, ALU.mult)
                NSD, _ = sc.sb("NSD", [128, 11, NPAIR], F32)
                ts(NSD[:], SD[:], -1.0, None, ALU.mult)
                tt(P(13), P(3), P(ci), ALU.mult)
                tt(P(14), P(3), P(si), ALU.mult)
                ts(P(15), P(13), -1.0, None, ALU.add)
                tt(P(16), lr, lr, ALU.mult, extra_r=[lamb])
                tt(P(17), li, li, ALU.mult, extra_r=[lamb])
                tt(P(16), P(16), P(17), ALU.add)
                k.op("dve", lambda e: e.reciprocal(P(17), P(16)), reads=[prmb], writes=[prmb])
                tt(P(18), P(15), lr, ALU.mult, extra_r=[lamb])
                tt(P(19), P(14), li, ALU.mult, extra_r=[lamb])
                tt(P(18), P(18), P(19), ALU.add)
                tt(P(20), P(18), P(17), ALU.mult)
                tt(P(18), P(14), lr, ALU.mult, extra_r=[lamb])
                tt(P(19), P(15), li, ALU.mult, extra_r=[lamb])
                tt(P(18), P(18), P(19), ALU.subtract)
                tt(P(21), P(18), P(17), ALU.mult)
                ts(P(22), P(21), -1.0, None, ALU.mult)
                with Scope(k) as scc:
                    cwr, cwrb = scc.sb("cwr", [128, NPAIR, 128], F32)
                    cwi, cwib = scc.sb("cwi", [128, NPAIR, 128], F32)
                    ctmp, ctmpb = scc.sb("ctmp", [128, 128], F32)
                    k.dma("sp", cwr[:], A["s5_cw_re"], cwrb, writes=[cwrb])
                    k.dma("sp", cwi[:], A["s5_cw_im"], cwib, writes=[cwib])
                    for j in range(NPAIR):
                        k.op("dve", lambda e: e.tensor_scalar(ctmp[:], cwi[:, j, :], prm[:, 22, j:j + 1], None, op0=ALU.mult),
                             reads=[cwib, prmb], writes=[ctmpb])
                        k.op("dve", lambda e: e.scalar_tensor_tensor(out=cfr[:, j, :], in0=cwr[:, j, :], scalar=prm[:, 20, j:j + 1],
                                                                     in1=ctmp[:], op0=ALU.mult, op1=ALU.add),
                             reads=[cwrb, prmb, ctmpb], writes=[cfrb])
                        k.op("dve", lambda e: e.tensor_scalar(ctmp[:], cwi[:, j, :], prm[:, 20, j:j + 1], -1.0, op0=ALU.mult, op1=ALU.mult),
                             reads=[cwib, prmb], writes=[ctmpb])
                        k.op("dve", lambda e: e.scalar_tensor_tensor(out=cfi[:, j, :], in0=cwr[:, j, :], scalar=prm[:, 22, j:j + 1],
                                                                     in1=ctmp[:], op0=ALU.mult, op1=ALU.add),
                             reads=[cwrb, prmb, ctmpb], writes=[cfib])
                COS, cosb = sc.sb("COS", [128, S], F32)
                SIN, sinb = sc.sb("SIN", [128, S], F32)
                wre, wreb = sc.sb("wre", [128, 2, 512], F32)
                wim, wimb = sc.sb("wim", [128, 2, 512], F32)
                WK, wkb = sc.sb("WK", [128, 6, 512], F32)
                wk = [(WK[:, i, :], wkb) for i in range(6)]
                tb = WK[:, 0:4, :].rearrange("p (a b) c -> p a (b c)", a=2)
                tbb = wkb
                xb, _ = sc.sb("xb", [128, 2, 2, S], BF16)
                xbb = [[sc.buf() for _ in range(4)] for _ in range(2)]
                uBq, uBqb = sc.sb("uBq", [128, S], BF16)
                ysb, ysbb = sc.sb("ysb", [128, 512], F32)
                gw = [sc.sb("gw%d" % i, [128, 512], F32) for i in range(3)]
                k.op("dve", lambda e: e.memset(COS[:, 0:1], 1.0), writes=[cosb])
                k.op("dve", lambda e: e.memset(SIN[:, 0:1], 0.0), writes=[sinb])
                for j in range(NPAIR):
                    q, pp = j // 4, j % 4
                    for kk in range(11):
                        d = 1 << kk
                        cd, sd, nsd = CD[:, kk, j:j + 1], SD[:, kk, j:j + 1], NSD[:, kk, j:j + 1]
                        k.op("dve", lambda e: e.tensor_scalar(tb[:, 0, 0:d], SIN[:, 0:d], nsd, None, op0=ALU.mult),
                             reads=[sinb, prmb], writes=[tbb])
                        k.op("dve", lambda e: e.tensor_scalar(tb[:, 1, 0:d], COS[:, 0:d], sd, None, op0=ALU.mult),
                             reads=[cosb, prmb], writes=[tbb])
                        k.op("dve", lambda e: e.scalar_tensor_tensor(out=COS[:, d:2 * d], in0=COS[:, 0:d], scalar=cd, in1=tb[:, 0, 0:d],
                                                                     op0=ALU.mult, op1=ALU.add),
                             reads=[cosb, tbb, prmb], writes=[cosb])
                        k.op("dve", lambda e: e.scalar_tensor_tensor(out=SIN[:, d:2 * d], in0=SIN[:, 0:d], scalar=cd, in1=tb[:, 1, 0:d],
                                                                     op0=ALU.mult, op1=ALU.add),
                             reads=[sinb, tbb, prmb], writes=[sinb])
                    rcol = prm[:, 3, j:j + 1]
                    if pp == 0:
                        k.op("act", lambda e: e.activation(out=uBq[:], in_=uT[:, q, :], func=AF.Copy),
                             reads=ub[q], writes=[uBqb])
                    for nt in range(4):
                        sl = slice(nt * 512, (nt + 1) * 512)
                        rows = slice(64 * (pp // 2), 64 * (pp // 2) + 64)
                        k.mm(k.ps[0][:], k.psb[0], bwr[rows, q, pp, :], uBq[rows, sl], True, True, reads=[bwrb, uBqb])
                        k.mm(k.ps[1][:], k.psb[1], bwi[rows, q, pp, :], uBq[rows, sl], True, True, reads=[bwib, uBqb])
                        (t1, t1b), (t2, t2b), (t3, t3b), (t4, t4b), (pr, prb), (pi, pib) = [(a_[:, :] if False else a_, b_) for a_, b_ in wk]
                        C_, S_ = COS[:, sl], SIN[:, sl]
                        k.op("dve", lambda e: e.tensor_tensor(out=t1[:], in0=k.ps[0][:], in1=C_, op=ALU.mult), reads=[k.psb[0], cosb], writes=[t1b])
                        k.op("dve", lambda e: e.tensor_tensor(out=t2[:], in0=k.ps[1][:], in1=S_, op=ALU.mult), reads=[k.psb[1], sinb], writes=[t2b])
                        k.op("dve", lambda e: e.tensor_tensor(out=t3[:], in0=k.ps[1][:], in1=C_, op=ALU.mult), reads=[k.psb[1], cosb], writes=[t3b])
                        k.op("dve", lambda e: e.tensor_tensor(out=t4[:], in0=k.ps[0][:], in1=S_, op=ALU.mult), reads=[k.psb[0], sinb], writes=[t4b])
                        k.op("dve", lambda e: e.tensor_tensor(out=pr[:], in0=t1[:], in1=t2[:], op=ALU.add), reads=[t1b, t2b], writes=[prb])
                        k.op("dve", lambda e: e.tensor_tensor(out=pi[:], in0=t3[:], in1=t4[:], op=ALU.subtract), reads=[t3b, t4b], writes=[pib])
                        ini_r = 0.0 if nt == 0 else wre[:, (nt - 1) % 2, 511:512]
                        ini_i = 0.0 if nt == 0 else wim[:, (nt - 1) % 2, 511:512]
                        rb = rcol.to_broadcast([128, 512])
                        k.op("dve", lambda e: e.tensor_tensor_scan(out=wre[:, nt % 2, :], data0=rb, data1=pr[:], initial=ini_r, op0=ALU.mult, op1=ALU.add),
                             reads=[prb, prmb, wreb], writes=[wreb])
                        k.op("dve", lambda e: e.tensor_tensor_scan(out=wim[:, nt % 2, :], data0=rb, data1=pi[:], initial=ini_i, op0=ALU.mult, op1=ALU.add),
                             reads=[pib, prmb, wimb], writes=[wimb])
                        k.op("dve", lambda e: e.tensor_tensor(out=t1[:], in0=wre[:, nt % 2, :], in1=C_, op=ALU.mult), reads=[wreb, cosb], writes=[t1b])
                        k.op("dve", lambda e: e.tensor_tensor(out=t2[:], in0=wim[:, nt % 2, :], in1=S_, op=ALU.mult), reads=[wimb, sinb], writes=[t2b])
                        k.op("dve", lambda e: e.tensor_tensor(out=t3[:], in0=wre[:, nt % 2, :], in1=S_, op=ALU.mult), reads=[wreb, sinb], writes=[t3b])
                        k.op("dve", lambda e: e.tensor_tensor(out=t4[:], in0=wim[:, nt % 2, :], in1=C_, op=ALU.mult), reads=[wimb, cosb], writes=[t4b])
                        k.op("dve", lambda e: e.tensor_tensor(out=xb[:, j % 2, 0, sl], in0=t1[:], in1=t2[:], op=ALU.subtract), reads=[t1b, t2b], writes=[xbb[j % 2][nt]])
                        k.op("dve", lambda e: e.tensor_tensor(out=xb[:, j % 2, 1, sl], in0=t3[:], in1=t4[:], op=ALU.add), reads=[t3b, t4b], writes=[xbb[j % 2][nt]])
                        bk = 2 + nt
                        k.mm(k.ps[bk][:], k.psb[bk], cfr[:, j, :], xb[:, j % 2, 0, sl], pp == 0, False, reads=[cfrb, xbb[j % 2][nt]])
                        k.mm(k.ps[bk][:], k.psb[bk], cfi[:, j, :], xb[:, j % 2, 1, sl], False, pp == 3, reads=[cfib, xbb[j % 2][nt]])
                    if pp == 3:
                        for nt in range(4):
                            sl = slice(nt * 512, (nt + 1) * 512)
                            bk = 2 + nt
                            k.op("dve", lambda e: e.scalar_tensor_tensor(out=ysb[:], in0=uT[:, q, sl], scalar=dsk[:, q:q + 1], in1=k.ps[bk][:],
                                                                         op0=ALU.mult, op1=ALU.add),
                                 reads=[ub[q][nt], dskb, k.psb[bk]], writes=[ysbb])
                            (g1, g1b), (g2, g2b), (g3, g3b) = gw
                            k.op("act", lambda e: e.activation(out=g1[:], in_=ysb[:], func=AF.Square), reads=[ysbb], writes=[g1b])
                            k.op("pool", lambda e: e.tensor_scalar(g2[:], g1[:], 0.044715, 1.0, op0=ALU.mult, op1=ALU.add), reads=[g1b], writes=[g2b])
                            k.op("pool", lambda e: e.tensor_tensor(out=g3[:], in0=g2[:], in1=ysb[:], op=ALU.mult), reads=[g2b, ysbb], writes=[g3b])
                            k.op("act", lambda e: e.activation(out=g1[:], in_=g3[:], func=AF.Sigmoid, scale=1.5957691216057308), reads=[g3b], writes=[g1b])
                            k.op("pool", lambda e: e.tensor_tensor(out=gT[:, q, sl], in0=g1[:], in1=ysb[:], op=ALU.mult), reads=[g1b, ysbb], writes=[gb[q][nt]])
            with Scope(k) as sc2:
                oT, _ = sc2.sb("oT", [128, NQ, S], BF16)
                ob = [[sc2.buf() for _ in range(4)] for _ in range(NQ)]
                with Scope(k) as sc:
                    wg, wgb = sc.sb("wglu", [128, NQ, DS], BF16)
                    wv = A["l0_s5_w_glu"].rearrange("(c p) n -> p c n", p=128)
                    for i in range(2):
                        k.dma("pool", wg[:, 4 * i:4 * i + 4, :], wv[:, 4 * i:4 * i + 4, :], wgb, writes=[wgb])
                    bg, bgb = sc.sb("bglu", [128, NQ], F32)
                    k.dma("sp", bg[:], A["s5_b_glu"], bgb, writes=[bgb])
                    sg = [sc.sb("sg%d" % i, [128, 512], F32) for i in range(2)]
                    it = 0
                    for q in range(NQ):
                        for nt in range(4):
                            sl = slice(nt * 512, (nt + 1) * 512)
                            bk = it % 4
                            s_t, s_b = sg[it % 2]
                            it += 1
                            for c in range(NQ):
                                k.mm(k.ps[bk][:], k.psb[bk], wg[:, c, q * 128:(q + 1) * 128], gT[:, c, sl], c == 0, c == NQ - 1,
                                     reads=[wgb, gb[c][nt]])
                            k.op("act", lambda e: e.activation(out=s_t[:], in_=k.ps[bk][:], func=AF.Sigmoid, bias=bg[:, q:q + 1]),
                                 reads=[k.psb[bk], bgb], writes=[s_b])
                            k.op("dve", lambda e: e.tensor_tensor(out=oT[:, q, sl], in0=s_t[:], in1=gT[:, q, sl], op=ALU.mult),
                                 reads=[s_b, gb[q][nt]], writes=[ob[q][nt]])
                with Scope(k) as sc:
                    wo, wob = sc.sb("wout", [128, NQ, D], BF16)
                    wv = A["l0_s5_w_out"].rearrange("(c p) n -> p c n", p=128)
                    for i in range(4):
                        k.dma("pool", wo[:, 2 * i:2 * i + 2, :], wv[:, 2 * i:2 * i + 2, :], wob, writes=[wob])
                    xt = [sc.sb("xt", [128, D], F32) for _ in range(2)]
                    ot = [sc.sb("ot", [128, D], F32) for _ in range(2)]
                    for tt_ in range(NT):
                        x_t, x_b = xt[tt_ % 2]
                        o_t, o_b = ot[tt_ % 2]
                        k.dma("sp", x_t[:], x_in[tt_ * 128:(tt_ + 1) * 128, :], x_b, writes=[x_b])
                        nt = tt_ // 4
                        for n4 in range(4):
                            bk = (tt_ % 2) * 4 + n4
                            for c in range(NQ):
                                k.mm(k.ps[bk][:], k.psb[bk], oT[:, c, tt_ * 128:(tt_ + 1) * 128], wo[:, c, n4 * 512:(n4 + 1) * 512],
                                     c == 0, c == NQ - 1, reads=[ob[c][nt], wob])
                            k.op("dve", lambda e: e.tensor_tensor(out=o_t[:, n4 * 512:(n4 + 1) * 512], in0=k.ps[bk][:],
                                                                  in1=x_t[:, n4 * 512:(n4 + 1) * 512], op=ALU.add),
                                 reads=[k.psb[bk], x_b], writes=[o_b])
                        k.dma("sp", h_out[tt_ * 128:(tt_ + 1) * 128, :], o_t[:], o_b, reads=[o_b])


def prep_common(inp):
    f = np.float32
    out = {}
    for nm in ["l0_mix_norm", "l0_ffn_norm", "l1_mix_norm", "l1_ffn_norm", "final_norm"]:
        out[nm] = np.ascontiguousarray(np.broadcast_to(np.asarray(inp[nm], f)[None, :], (128, D)))
    lam_re = np.asarray(inp["l0_s5_lambda_re"], f)
    lam_im = np.asarray(inp["l0_s5_lambda_im"], f)
    lst = np.repeat(np.asarray(inp["l0_s5_log_step"], f), 64).reshape(64, 64)
    lam = np.stack([a.reshape(NPAIR, 128).T for a in (lam_re, lam_im, lst)], axis=1)
    out["s5_lam"] = np.ascontiguousarray(lam)
    b_re = np.asarray(inp["l0_s5_b_re"], f)
    b_im = np.asarray(inp["l0_s5_b_im"], f)
    c_re = np.asarray(inp["l0_s5_c_re"], f)
    c_im = np.asarray(inp["l0_s5_c_im"], f)
    bwr = np.zeros((128, NQ, 4, 128), f)
    bwi = np.zeros((128, NQ, 4, 128), f)
    cwr = np.zeros((128, NPAIR, 128), f)
    cwi = np.zeros((128, NPAIR, 128), f)
    for g in range(64):
        q, r0, gl, j = g // 8, (g % 8) * 16, g % 2, g // 2
        bwr[r0:r0 + 16, q, (g % 8) // 2, 64 * gl:64 * gl + 64] = b_re[g].T
        bwi[r0:r0 + 16, q, (g % 8) // 2, 64 * gl:64 * gl + 64] = b_im[g].T
        cwr[64 * gl:64 * gl + 64, j, r0:r0 + 16] = c_re[g].T
        cwi[64 * gl:64 * gl + 64, j, r0:r0 + 16] = c_im[g].T
    out["s5_bw_re"], out["s5_bw_im"], out["s5_cw_re"], out["s5_cw_im"] = bwr, bwi, cwr, cwi
    out["s5_d"] = np.ascontiguousarray(np.asarray(inp["l0_s5_d"], f).reshape(NQ, 128).T)
    out["s5_b_glu"] = np.ascontiguousarray(np.asarray(inp["l0_s5_b_glu"], f).reshape(NQ, 128).T)
    for nm in ["l0_s5_w_in", "l0_s5_w_glu", "l0_s5_w_out"]:
        out[nm] = np.asarray(inp[nm], f)
    out["l1_fox_w_in"] = np.asarray(inp["l1_fox_w_in"], f)
    out["l1_fox_w_out"] = np.asarray(inp["l1_fox_w_out"], f)
    out["l1_fox_b_forget"] = np.ascontiguousarray(np.asarray(inp["l1_fox_b_forget"], f).reshape(NH, 1))
    for L in ("l0", "l1"):
        out[L + "_moe_wr"] = np.ascontiguousarray(np.concatenate([np.asarray(inp[L + "_moe_w_group"], f), np.asarray(inp[L + "_moe_w_expert"], f)], axis=1))
        brow = np.concatenate([np.asarray(inp[L + "_moe_b_group"], f), np.asarray(inp[L + "_moe_b_expert"], f)])
        out[L + "_moe_br"] = np.ascontiguousarray(np.broadcast_to(brow[None, :], (128, 72)))
        for nm in ["_moe_w_gate", "_moe_w_up", "_moe_w_down"]:
            out[L + nm] = np.asarray(inp[L + nm], f)
    return out


def declare_inputs(k, arrays):
    A = {}
    for nm, a in arrays.items():
        A[nm] = k.nc.dram_tensor(nm, list(a.shape), F32, kind="ExternalInput").ap()
    return A


def phase_moe(k, A, L, h_in, h_out, yg, final_gain=None):
    with Scope(k) as sc0:
        cwb, cwbb = sc0.sb("cwb", [128, NT, NE], BF16)
        ohb, ohbb = sc0.sb("ohb", [128, NT, NG], BF16)
        ohf, ohfb = sc0.sb("ohf", [128, NT, NG], F32)
        rs, rsb = sc0.sb("rs", [128, NT, NG], F32)
        rankT, rankTb = sc0.sb("rankT", [NG, S], F32)
        (idb, idbb), (idf, idfb) = make_ident(k, sc0)
        iota_c, iocb = sc0.sb("iota_c", [128, CAP], F32)
        slot_idx, slib = sc0.sb("slot_idx", [128, NST], F32)
        tri, trib = sc0.sb("tri", [128, 128], BF16)
        ones, onesb = sc0.sb("ones", [128, 128], BF16)
        Eg, Egb = sc0.sb("Eg", [NG, NG, 128], F32)
        with Scope(k) as sct:
            ii, iib = sct.sb("ii", [128, 512], I32)
            k.op("pool", lambda e: e.iota(ii[:, 0:CAP], pattern=[[1, CAP]], base=0, channel_multiplier=0), writes=[iib])
            k.op("dve", lambda e: e.tensor_copy(out=iota_c[:], in_=ii[:, 0:CAP]), reads=[iib], writes=[iocb])
            k.op("pool", lambda e: e.iota(ii[:, 0:NST], pattern=[[128, NST]], base=0, channel_multiplier=1), reads=[iocb], writes=[iib])
            k.op("dve", lambda e: e.tensor_copy(out=slot_idx[:], in_=ii[:, 0:NST]), reads=[iib], writes=[slib])
            k.op("pool", lambda e: e.iota(ii[:, 0:128], pattern=[[1, 128]], base=0, channel_multiplier=-1), reads=[slib], writes=[iib])
            k.op("dve", lambda e: e.tensor_scalar(tri[:], ii[:, 0:128], 0, None, op0=ALU.is_gt), reads=[iib], writes=[trib])
            k.op("dve", lambda e: e.memset(ones[:], 1.0), writes=[onesb])
            i8, i8b = sct.sb("i8", [NG, NG, 128], I32)
            k.op("pool", lambda e: e.iota(i8[:], pattern=[[1, NG], [0, 128]], base=0, channel_multiplier=-1), writes=[i8b])
            k.op("dve", lambda e: e.tensor_scalar(Eg[:], i8[:], 0, None, op0=ALU.is_equal), reads=[i8b], writes=[Egb])
        scH = Scope(k)
        scH.__enter__()
        hnb, _ = scH.sb("hnb", [128, NT, D], BF16)
        hnbb = [scH.buf() for _ in range(NT)]
        if True:
            with Scope(k) as sc:
                gain, gainb = sc.sb("gain", [128, D], F32)
                k.dma("sp", gain[:], A[L + "_ffn_norm"], gainb, writes=[gainb])
                wr, wrb = sc.sb("wr", [128, NF, 72], F32)
                k.dma("sp", wr[:], A[L + "_moe_wr"].rearrange("(fc p) n -> p fc n", p=128), wrb, writes=[wrb])
                br, brb = sc.sb("br", [128, 72], F32)
                k.dma("sp", br[:], A[L + "_moe_br"], brb, writes=[brb])
                tmp = rms_tmp(sc)
                xt = [sc.sb("xt", [128, D], F32) for _ in range(2)]
                hnf = [sc.sb("hnf", [128, D], F32) for _ in range(2)]
                hnT = [sc.sb("hnTf", [128, NF, 128], F32) for _ in range(2)]
                rt, rtb = sc.sb("rt", [128, 320], F32)
                tt_, ts_, stt_, act_ = small_ops(k, rtb)
                for t in range(NT):
                    x_t, x_b = xt[t % 2]
                    f_t, f_b = hnf[t % 2]
                    T_t, T_b = hnT[t % 2]
                    k.dma("sp", x_t[:], h_in[t * 128:(t + 1) * 128, :], x_b, writes=[x_b])
                    rms_tile(k, sc, x_t[:], x_b, gain[:], gainb, f_t[:], f_b, tmp)
                    k.op("act", lambda e: e.activation(out=hnb[:, t, :], in_=f_t[:], func=AF.Copy), reads=[f_b], writes=[hnbb[t]])
                    for qd in range(4):
                        bk = qd
                        for j in range(4):
                            fc = qd * 4 + j
                            k.tr(k.ps[bk][:, j * 128:(j + 1) * 128], k.psb[bk], f_t[:, fc * 128:(fc + 1) * 128], idf[:], reads=[f_b, idfb])
                        src = k.ps[bk][:].rearrange("p (a b) -> p a b", a=4)
                        dst = T_t[:, qd * 4:qd * 4 + 4, :]
                        if qd % 2 == 0:
                            k.op("act", lambda e: e.activation(out=dst, in_=src, func=AF.Copy), reads=[k.psb[bk]], writes=[T_b])
                        else:
                            k.op("dve", lambda e: e.tensor_copy(out=dst, in_=src), reads=[k.psb[bk]], writes=[T_b])
                    bk = 4 + (t % 2)
                    for fc in range(NF):
                        k.mm(k.ps[bk][:, 0:72], k.psb[bk], T_t[:, fc, :], wr[:, fc, :], fc == 0, fc == NF - 1, reads=[T_b, wrb])
                    R = lambda a, b: rt[:, a:b]
                    lg, m8, ngm, eg, sg, pg = R(0, 72), R(72, 80), R(80, 81), R(81, 89), R(89, 90), R(90, 91)
                    pen, me, t8 = R(91, 99), R(99, 163), R(163, 171)
                    dl, ex, den, w1, w2, c1, c2 = R(171, 172), R(172, 173), R(173, 174), R(174, 175), R(175, 176), R(176, 240), R(240, 304)
                    tt_(lg, k.ps[bk][:, 0:72], br[:], ALU.add, extra_r=[k.psb[bk], brb])
                    k.op("dve", lambda e: e.max(out=m8, in_=rt[:, 0:8]), reads=[rtb], writes=[rtb])
                    oh_t = ohf[:, t, :]
                    k.op("dve", lambda e: e.tensor_scalar(oh_t, rt[:, 0:8], rt[:, 72:73], None, op0=ALU.is_equal), reads=[rtb], writes=[ohfb])
                    k.op("dve", lambda e: e.tensor_copy(out=ohb[:, t, :], in_=oh_t), reads=[ohfb], writes=[ohbb])
                    ts_(ngm, rt[:, 72:73], -1.0, None, ALU.mult)
                    k.op("act", lambda e: e.activation(out=eg, in_=rt[:, 0:8], func=AF.Exp, bias=rt[:, 80:81], scale=1.0, accum_out=sg),
                         reads=[rtb], writes=[rtb])
                    k.op("dve", lambda e: e.reciprocal(pg, sg), reads=[rtb], writes=[rtb])
                    ts_(pen, oh_t, -1.0, 1e30, ALU.add, ALU.mult, extra_r=[ohfb])
                    me3 = me.rearrange("p (a b) -> p a b", a=8)
                    el3 = rt[:, 8:72].rearrange("p (a b) -> p a b", a=8)
                    pen3 = pen.unsqueeze(2).to_broadcast([128, 8, 8])
                    tt_(me3, el3, pen3, ALU.add)
                    k.op("dve", lambda e: e.max(out=t8, in_=me), reads=[rtb], writes=[rtb])
                    tt_(dl, rt[:, 164:165], rt[:, 163:164], ALU.subtract)
                    act_(ex, dl, AF.Exp)
                    ts_(den, ex, 1.0, None, ALU.add)
                    k.op("dve", lambda e: e.reciprocal(w1, den), reads=[rtb], writes=[rtb])
                    tt_(w2, ex, w1, ALU.mult)
                    tt_(w1, w1, pg, ALU.mult)
                    tt_(w2, w2, pg, ALU.mult)
                    ts_(c1, me, rt[:, 163:164], rt[:, 174:175], ALU.is_equal, ALU.mult)
                    ts_(c2, me, rt[:, 164:165], rt[:, 175:176], ALU.is_equal, ALU.mult)
                    k.op("dve", lambda e: e.tensor_tensor(out=cwb[:, t, :], in0=c1, in1=c2, op=ALU.add), reads=[rtb], writes=[cwbb])
                for t in range(NT):
                    bk = 6 + (t % 2)
                    k.mm(k.ps[bk][:, 0:NG], k.psb[bk], tri[:], ohb[:, t, :], True, t == 0, reads=[trib, ohbb])
                    for t2 in range(t):
                        k.mm(k.ps[bk][:, 0:NG], k.psb[bk], ones[:], ohb[:, t2, :], False, t2 == t - 1, reads=[onesb, ohbb])
                    k.op("dve", lambda e: e.scalar_tensor_tensor(out=rs[:, t, :], in0=k.ps[bk][:, 0:NG], scalar=1.0, in1=ohf[:, t, :],
                                                                 op0=ALU.add, op1=ALU.mult), reads=[k.psb[bk], ohfb], writes=[rsb])
                    k.op("dve", lambda e: e.tensor_scalar(rs[:, t, :], rs[:, t, :], -1.0, None, op0=ALU.add), reads=[rsb], writes=[rsb])
                for t in range(NT):
                    bk = t % 2
                    k.tr(k.ps[bk][0:NG, 0:128], k.psb[bk], rs[:, t, :], idf[:], reads=[rsb, idfb])
                    k.op("dve", lambda e: e.tensor_copy(out=rankT[:, t * 128:(t + 1) * 128], in_=k.ps[bk][0:NG, 0:128]), reads=[k.psb[bk]], writes=[rankTb])
            with Scope(k) as sc:
                NU = 8
                ring, _ = sc.sb("ring", [128, NU, 4096], BF16)
                ringb = [sc.buf() for _ in range(NU)]
                sel, selb = sc.sb("sel", [128, NT, CAP], BF16)
                xg, xgb = sc.sb("xg", [128, NF, CAP], BF16)
                cwg, cwgb = sc.sb("cwg", [128, NST, EPG], F32)
                yacc, _ = sc.sb("yacc", [128, NST, D], F32)
                yaccb = [sc.buf() for _ in range(NST)]
                hid = [sc.sb("hid", [128, 4, CAP], BF16) for _ in range(2)]
                sgt = [sc.sb("sgt", [128, CAP], F32) for _ in range(2)]
                wg_v = A[L + "_moe_w_gate"]
                wu_v = A[L + "_moe_w_up"]
                wd_v = A[L + "_moe_w_down"]

                def unit_src(u):
                    e, j = u // 6, u % 6
                    if j < 4:
                        w = wg_v if j < 2 else wu_v
                        h = j % 2
                        return w[e].rearrange("(fc p) n -> p fc n", p=128)[:, h * 8:h * 8 + 8, :], "p (a b) -> p a b", 8
                    h = j - 4
                    return wd_v[e].rearrange("(c p) n -> p c n", p=128)[:, h * 2:h * 2 + 2, :], "p (a b) -> p a b", 2

                def issue_unit(u):
                    if u >= NE * 6:
                        return
                    src, pat, a = unit_src(u)
                    slot = u % NU
                    k.dma("pool", ring[:, slot, :].rearrange(pat, a=a), src, ringb[slot], writes=[ringb[slot]])

                for u in range(NU):
                    issue_unit(u)
                cnt_ev = 0
                for g in range(NG):
                    for t in range(NT):
                        eng = "dve" if t % 2 == 0 else "pool"
                        k.op(eng, lambda e: e.tensor_scalar(sel[:, t, :], iota_c[:], rs[:, t, g:g + 1], None, op0=ALU.is_equal),
                             reads=[iocb, rsb], writes=[selb])
                    for fc in range(NF):
                        bk = fc % 2
                        for t in range(NT):
                            k.mm(k.ps[bk][:, 0:CAP], k.psb[bk], hnb[:, t, fc * 128:(fc + 1) * 128], sel[:, t, :], t == 0, t == NT - 1,
                                 reads=[hnbb[t], selb])
                        if fc % 2 == 0:
                            k.op("act", lambda e: e.activation(out=xg[:, fc, :], in_=k.ps[bk][:, 0:CAP], func=AF.Copy), reads=[k.psb[bk]], writes=[xgb])
                        else:
                            k.op("dve", lambda e: e.tensor_copy(out=xg[:, fc, :], in_=k.ps[bk][:, 0:CAP]), reads=[k.psb[bk]], writes=[xgb])
                    for st in range(NST):
                        bk = st % 2
                        for t in range(NT):
                            k.mm(k.ps[bk][:, 0:EPG], k.psb[bk], sel[:, t, st * 128:(st + 1) * 128], cwb[:, t, g * EPG:(g + 1) * EPG],
                                 t == 0, t == NT - 1, reads=[selb, cwbb])
                        k.op("dve", lambda e: e.tensor_copy(out=cwg[:, st, :], in_=k.ps[bk][:, 0:EPG]), reads=[k.psb[bk]], writes=[cwgb])
                    for el in range(EPG):
                        e_id = g * EPG + el
                        u0 = e_id * 6
                        h_t, h_b = hid[e_id % 2]
                        for hc in range(4):
                            s_t, s_b = sgt[hc % 2]
                            bg, bu = 2 + (hc % 2), 4 + (hc % 2)
                            for fc in range(NF):
                                slot = (u0 + fc // 8) % NU
                                wv = ring[:, slot, :].rearrange("p (a b) -> p a b", a=8)
                                k.mm(k.ps[bg][:, 0:CAP], k.psb[bg], wv[:, fc % 8, hc * 128:(hc + 1) * 128], xg[:, fc, :], fc == 0, fc == NF - 1,
                                     reads=[ringb[slot], xgb])
                            for fc in range(NF):
                                slot = (u0 + 2 + fc // 8) % NU
                                wv = ring[:, slot, :].rearrange("p (a b) -> p a b", a=8)
                                k.mm(k.ps[bu][:, 0:CAP], k.psb[bu], wv[:, fc % 8, hc * 128:(hc + 1) * 128], xg[:, fc, :], fc == 0, fc == NF - 1,
                                     reads=[ringb[slot], xgb])
                            k.op("act", lambda e: e.activation(out=s_t[:], in_=k.ps[bg][:, 0:CAP], func=AF.Silu), reads=[k.psb[bg]], writes=[s_b])
                            k.op("dve", lambda e: e.tensor_tensor(out=h_t[:, hc, :], in0=s_t[:], in1=k.ps[bu][:, 0:CAP], op=ALU.mult),
                                 reads=[s_b, k.psb[bu]], writes=[h_b])
                        for j in range(4):
                            issue_unit(u0 + NU + j)
                        for st in range(NST):
                            for n4 in range(4):
                                bk = 6 + (cnt_ev % 2)
                                cnt_ev += 1
                                for hc in range(4):
                                    slot = (u0 + 4 + hc // 2) % NU
                                    wv = ring[:, slot, :].rearrange("p (a b) -> p a b", a=2)
                                    k.mm(k.ps[bk][:], k.psb[bk], h_t[:, hc, st * 128:(st + 1) * 128], wv[:, hc % 2, n4 * 512:(n4 + 1) * 512],
                                         hc == 0, hc == 3, reads=[h_b, ringb[slot]])
                                ya = yacc[:, st, n4 * 512:(n4 + 1) * 512]
                                if el == 0:
                                    k.op("dve", lambda e: e.tensor_scalar(ya, k.ps[bk][:], cwg[:, st, el:el + 1], None, op0=ALU.mult),
                                         reads=[k.psb[bk], cwgb], writes=[yaccb[st]])
                                else:
                                    k.op("dve", lambda e: e.scalar_tensor_tensor(out=ya, in0=k.ps[bk][:], scalar=cwg[:, st, el:el + 1], in1=ya,
                                                                                 op0=ALU.mult, op1=ALU.add),
                                         reads=[k.psb[bk], cwgb, yaccb[st]], writes=[yaccb[st]])
                        for j in range(4, 6):
                            issue_unit(u0 + NU + j)
                    for st in range(NST):
                        k.dma("sp", yg[g, st * 128:(st + 1) * 128, :], yacc[:, st, :], yaccb[st], reads=[yaccb[st]])
        scH.__exit__(None, None, None)
        with Scope(k) as sc:
            ygs, ygsb = sc.sb("ygs", [128, NG, NST, D], BF16)
            for g in range(NG):
                k.dma("pool", ygs[:, g, :, :], yg[g].rearrange("(st p) d -> p st d", p=128), ygsb, writes=[ygsb])
            selT = [sc.sb("selT", [128, NST, NG, 128], BF16) for _ in range(2)]
            xt = [sc.sb("xt", [128, D], F32) for _ in range(2)]
            ot = [sc.sb("ot", [128, D], F32) for _ in range(2)]
            if final_gain is not None:
                fg, fgb = sc.sb("fgain", [128, D], F32)
                k.dma("sp", fg[:], final_gain, fgb, writes=[fgb])
                tmp = rms_tmp(sc)
                ot2 = [sc.sb("ot2", [128, D], F32) for _ in range(2)]
            for t in range(NT):
                x_t, x_b = xt[t % 2]
                o_t, o_b = ot[t % 2]
                sT, sTb = selT[t % 2]
                k.dma("sp", x_t[:], h_in[t * 128:(t + 1) * 128, :], x_b, writes=[x_b])
                for half in range(2):
                    bk = half
                    for j in range(4):
                        g = half * 4 + j
                        k.mm(k.ps[bk][:, j * 128:(j + 1) * 128], k.psb[bk], Eg[:, g, :], rankT[:, t * 128:(t + 1) * 128], True, True,
                             reads=[Egb, rankTb])
                    for st in range(NST):
                        k.op("dve", lambda e: e.tensor_scalar(sT[:, st, half * 4:half * 4 + 4, :], k.ps[bk][:].rearrange("p (a b) -> p a b", a=4),
                                                              slot_idx[:, st:st + 1], None, op0=ALU.is_equal),
                             reads=[k.psb[bk], slib], writes=[sTb])
                for n4 in range(4):
                    bk = 2 + ((t * 4 + n4) % 6)
                    i = 0
                    for g in range(NG):
                        for st in range(NST):
                            k.mm(k.ps[bk][:], k.psb[bk], sT[:, st, g, :], ygs[:, g, st, n4 * 512:(n4 + 1) * 512], i == 0, i == NG * NST - 1,
                                 reads=[sTb, ygsb])
                            i += 1
                    k.op("dve", lambda e: e.tensor_tensor(out=o_t[:, n4 * 512:(n4 + 1) * 512], in0=k.ps[bk][:], in1=x_t[:, n4 * 512:(n4 + 1) * 512],
                                                          op=ALU.add), reads=[k.psb[bk], x_b], writes=[o_b])
                if final_gain is None:
                    k.dma("sp", h_out[t * 128:(t + 1) * 128, :], o_t[:], o_b, reads=[o_b])
                else:
                    o2, o2b = ot2[t % 2]
                    rms_tile(k, sc, o_t[:], o_b, fg[:], fgb, o2[:], o2b, tmp)
                    k.dma("sp", h_out[t * 128:(t + 1) * 128, :], o2[:], o2b, reads=[o2b])


def phase_fox(k, A, h_in, h_out, qTd, kTd, vd, sgd):
    SCALE = 128 ** -0.5
    with Scope(k) as sc0:
        (idb, idbb), (idf, idfb) = make_ident(k, sc0)
        cumT, cumTb = sc0.sb("cumT", [NH, S], F32)
        with Scope(k) as sc1:
            hnT, _ = sc1.sb("hnT", [128, NF, S], BF16)
            hnTb = [sc1.buf() for _ in range(NT)]
            with Scope(k) as sc:
                gain, gainb = sc.sb("gain", [128, D], F32)
                k.dma("sp", gain[:], A["l1_mix_norm"], gainb, writes=[gainb])
                tmp = rms_tmp(sc)
                xt = [sc.sb("xt", [128, D], F32) for _ in range(2)]
                hn = [sc.sb("hn", [128, D], BF16) for _ in range(2)]
                for t in range(NT):
                    x_t, x_b = xt[t % 2]
                    h_t, h_b = hn[t % 2]
                    k.dma("sp", x_t[:], h_in[t * 128:(t + 1) * 128, :], x_b, writes=[x_b])
                    rms_tile(k, sc, x_t[:], x_b, gain[:], gainb, h_t[:], h_b, tmp)
                    for half in range(2):
                        bk = (t % 2) * 2 + half
                        pv = k.ps[bk][:].bitcast(BF16)
                        for j in range(8):
                            fc = half * 8 + j
                            k.tr(pv[:, j * 128:(j + 1) * 128], k.psb[bk], h_t[:, fc * 128:(fc + 1) * 128], idb[:], reads=[h_b, idbb])
                        src = pv.rearrange("p (a b) -> p a b", a=8)
                        dst = hnT[:, half * 8:half * 8 + 8, t * 128:(t + 1) * 128]
                        if half == 0:
                            k.op("act", lambda e: e.activation(out=dst, in_=src, func=AF.Copy), reads=[k.psb[bk]], writes=[hnTb[t]])
                        else:
                            k.op("dve", lambda e: e.tensor_copy(out=dst, in_=src), reads=[k.psb[bk]], writes=[hnTb[t]])
            with Scope(k) as sc:
                NU = 4
                ring, _ = sc.sb("ring", [128, NU, NF, 512], BF16)
                ringb = [sc.buf() for _ in range(NU)]
                win = A["l1_fox_w_in"]

                def issue_unit(u):
                    if u >= 16:
                        return
                    kind, hg = u % 4, u // 4
                    c0 = kind * D + hg * 512
                    src = win[:, c0:c0 + 512].rearrange("(fc p) n -> p fc n", p=128)
                    sl = u % NU
                    for hf in range(2):
                        k.dma("pool", ring[:, sl, hf * 8:hf * 8 + 8, :], src[:, hf * 8:hf * 8 + 8, :], ringb[sl], writes=[ringb[sl]])

                for u in range(NU):
                    issue_unit(u)
                wf, wfb = sc.sb("wf", [128, NF, NH], BF16)
                k.dma("pool", wf[:], win[:, 4 * D:4 * D + NH].rearrange("(fc p) n -> p fc n", p=128), wfb, writes=[wfb])
                bf_, bfb = sc.sb("bfg", [NH, 2], F32)
                k.dma("sp", bf_[:, 0:1], A["l1_fox_b_forget"], bfb, writes=[bfb])
                k.op("dve", lambda e: e.tensor_scalar(bf_[:, 1:2], bf_[:, 0:1], -1.0, None, op0=ALU.mult), reads=[bfb], writes=[bfb])
                lf, lfb = sc.sb("lf", [NH, S], F32)
                ones16, o16b = sc.sb("ones16", [NH, 512], F32)
                k.op("dve", lambda e: e.memset(ones16[:], 1.0), writes=[o16b])
                for nt in range(4):
                    sl = slice(nt * 512, (nt + 1) * 512)
                    bk = nt
                    for fc in range(NF):
                        k.mm(k.ps[bk][0:NH, :], k.psb[bk], wf[:, fc, :], hnT[:, fc, sl], fc == 0, fc == NF - 1,
                             reads=[wfb] + hnTb[nt * 4:nt * 4 + 4])
                    k.op("act", lambda e: e.activation(out=lf[:, sl], in_=k.ps[bk][0:NH, :], func=AF.Exp, scale=-1.0, bias=bf_[:, 1:2]),
                         reads=[k.psb[bk], bfb], writes=[lfb])
                    k.op("act", lambda e: e.activation(out=lf[:, sl], in_=lf[:, sl], func=AF.Ln, bias=1.0, scale=1.0), reads=[lfb], writes=[lfb])
                    ini = 0.0 if nt == 0 else cumT[:, nt * 512 - 1:nt * 512]
                    k.op("dve", lambda e: e.tensor_tensor_scan(out=cumT[:, sl], data0=ones16[:], data1=lf[:, sl], initial=ini, op0=ALU.mult, op1=ALU.add),
                         reads=[lfb, o16b, cumTb], writes=[cumTb])
                qst = [sc.sb("qst", [128, S], BF16) for _ in range(2)]
                vst = [sc.sb("vst", [128, NT, 512], BF16) for _ in range(2)]
                nq = 0
                nb = 0
                for u in range(16):
                    kind, hg = u % 4, u // 4
                    sl_ = u % NU
                    if kind < 2:
                        dstd = qTd if kind == 0 else kTd
                        for hl in range(4):
                            h = hg * 4 + hl
                            q_t, q_b = qst[nq % 2]
                            nq += 1
                            for nt in range(4):
                                sl = slice(nt * 512, (nt + 1) * 512)
                                bk = nb % 4
                                nb += 1
                                for fc in range(NF):
                                    k.mm(k.ps[bk][:], k.psb[bk], ring[:, sl_, fc, hl * 128:(hl + 1) * 128], hnT[:, fc, sl], fc == 0, fc == NF - 1,
                                         reads=[ringb[sl_]] + hnTb[nt * 4:nt * 4 + 4])
                                sc_ = SCALE if kind == 0 else 1.0
                                if nt % 2 == 0:
                                    k.op("act", lambda e: e.activation(out=q_t[:, sl], in_=k.ps[bk][:], func=AF.Copy, scale=sc_), reads=[k.psb[bk]], writes=[q_b])
                                else:
                                    k.op("dve", lambda e: e.tensor_scalar(q_t[:, sl], k.ps[bk][:], sc_, None, op0=ALU.mult), reads=[k.psb[bk]], writes=[q_b])
                            k.dma("sp", dstd[h], q_t[:], q_b, reads=[q_b])
                    else:
                        dstd = vd if kind == 2 else sgd
                        v_t, v_b = vst[kind % 2]
                        for t in range(NT):
                            bk = 4 + (t % 4)
                            for fc in range(NF):
                                k.mm(k.ps[bk][:], k.psb[bk], hnT[:, fc, t * 128:(t + 1) * 128], ring[:, sl_, fc, :], fc == 0, fc == NF - 1,
                                     reads=[ringb[sl_], hnTb[t]])
                            if kind == 2:
                                if t % 2 == 0:
                                    k.op("act", lambda e: e.activation(out=v_t[:, t, :], in_=k.ps[bk][:], func=AF.Copy), reads=[k.psb[bk]], writes=[v_b])
                                else:
                                    k.op("dve", lambda e: e.tensor_copy(out=v_t[:, t, :], in_=k.ps[bk][:]), reads=[k.psb[bk]], writes=[v_b])
                            else:
                                k.op("act", lambda e: e.activation(out=v_t[:, t, :], in_=k.ps[bk][:], func=AF.Sigmoid), reads=[k.psb[bk]], writes=[v_b])
                        k.dma("sp", dstd[:, hg * 512:(hg + 1) * 512].rearrange("(t p) c -> p t c", p=128), v_t[:], v_b, reads=[v_b])
                    issue_unit(u + NU)
        with Scope(k) as sc2:
            ogT, _ = sc2.sb("ogT", [128, NH, S], BF16)
            ogTb = [sc2.buf() for _ in range(NT)]
            with Scope(k) as sc:
                E16, E16b = sc.sb("E16", [NH, NH, 128], F32)
                maskt, maskb = sc.sb("maskt", [128, 128], F32)
                with Scope(k) as sct:
                    i16, i16b = sct.sb("i16", [NH, NH, 128], I32)
                    k.op("pool", lambda e: e.iota(i16[:], pattern=[[1, NH], [0, 128]], base=0, channel_multiplier=-1), writes=[i16b])
                    k.op("dve", lambda e: e.tensor_scalar(E16[:], i16[:], 0, None, op0=ALU.is_equal), reads=[i16b], writes=[E16b])
                    im, imb = sct.sb("im", [128, 128], I32)
                    k.op("pool", lambda e: e.iota(im[:], pattern=[[1, 128]], base=0, channel_multiplier=-1), writes=[imb])
                    k.op("dve", lambda e: e.tensor_scalar(maskt[:], im[:], 0, -30000.0, op0=ALU.is_gt, op1=ALU.mult), reads=[imb], writes=[maskb])
                qh = [sc.sb("qh", [128, S], BF16) for _ in range(2)]
                kh = [sc.sb("kh", [128, S], BF16) for _ in range(2)]
                vg = [sc.sb("vg", [128, NT, 512], BF16) for _ in range(2)]
                sgg = [sc.sb("sgg", [128, NT, 512], BF16) for _ in range(2)]
                ssb = [sc.sb("ssb", [128, S], F32) for _ in range(2)]
                pb = [sc.sb("pb", [128, S], BF16) for _ in range(2)]
                ptb = [sc.sb("ptb", [128, NT, 128], BF16) for _ in range(2)]
                negck, negckb = sc.sb("negck", [128, S], F32)
                st4, st4b = sc.sb("st4", [128, 8], F32)
                ogt = [sc.sb("ogt", [128, 128], BF16) for _ in range(2)]

                def load_head(h):
                    k.dma("sp", qh[h % 2][0][:], qTd[h], qh[h % 2][1], writes=[qh[h % 2][1]])
                    k.dma("sp", kh[h % 2][0][:], kTd[h], kh[h % 2][1], writes=[kh[h % 2][1]])

                def load_group(hg):
                    k.dma("sp", vg[hg % 2][0][:], vd[:, hg * 512:(hg + 1) * 512].rearrange("(t p) c -> p t c", p=128), vg[hg % 2][1], writes=[vg[hg % 2][1]])
                    k.dma("sp", sgg[hg % 2][0][:], sgd[:, hg * 512:(hg + 1) * 512].rearrange("(t p) c -> p t c", p=128), sgg[hg % 2][1], writes=[sgg[hg % 2][1]])

                load_group(0)
                load_head(0)
                it = 0
                for h in range(NH):
                    hg, hl = h // 4, h % 4
                    if h + 1 < NH:
                        load_head(h + 1)
                    if hl == 0 and hg + 1 < 4:
                        load_group(hg + 1)
                    q_t, q_b = qh[h % 2]
                    k_t, k_b = kh[h % 2]
                    v_t, v_b = vg[hg % 2]
                    g_t, g_b = sgg[hg % 2]
                    for nt in range(4):
                        sl = slice(nt * 512, (nt + 1) * 512)
                        bk = 4 + (nt % 2)
                        k.mm(k.ps[bk][:], k.psb[bk], E16[:, h, :], cumT[:, sl], True, True, reads=[E16b, cumTb])
                        k.op("act", lambda e: e.activation(out=negck[:, sl], in_=k.ps[bk][:], func=AF.Copy), reads=[k.psb[bk]], writes=[negckb])
                    for qt in range(NT):
                        nk = (qt + 1) * 128
                        nb_ = (nk + 511) // 512
                        s_t, s_b = ssb[it % 2]
                        p_t, p_b = pb[it % 2]
                        pt_t, pt_b = ptb[it % 2]
                        o_t, o_b = ogt[it % 2]
                        it += 1
                        for b in range(nb_):
                            w = min(512, nk - b * 512)
                            k.mm(k.ps[b][:, 0:w], k.psb[b], q_t[:, qt * 128:(qt + 1) * 128], k_t[:, b * 512:b * 512 + w], True, True, reads=[q_b, k_b])
                            k.op("dve", lambda e: e.tensor_tensor(out=s_t[:, b * 512:b * 512 + w], in0=k.ps[b][:, 0:w], in1=negck[:, b * 512:b * 512 + w], op=ALU.add),
                                 reads=[k.psb[b], negckb], writes=[s_b])
                        k.op("pool", lambda e: e.tensor_tensor(out=s_t[:, nk - 128:nk], in0=s_t[:, nk - 128:nk], in1=maskt[:], op=ALU.add),
                             reads=[s_b, maskb], writes=[s_b])
                        k.op("dve", lambda e: e.reduce_max(out=st4[:, 0:1], in_=s_t[:, 0:nk], axis=AX.X), reads=[s_b, st4b], writes=[st4b])
                        k.op("dve", lambda e: e.tensor_scalar(st4[:, 1:2], st4[:, 0:1], -1.0, None, op0=ALU.mult), reads=[st4b], writes=[st4b])
                        k.op("act", lambda e: e.activation(out=p_t[:, 0:nk], in_=s_t[:, 0:nk], func=AF.Exp, bias=st4[:, 1:2], scale=1.0, accum_out=st4[:, 2:3]),
                             reads=[s_b, st4b], writes=[p_b, st4b])
                        k.op("dve", lambda e: e.reciprocal(st4[:, 3:4], st4[:, 2:3]), reads=[st4b], writes=[st4b])
                        for c8 in range((qt + 8) // 8):
                            bk = 4 + (c8 % 2)
                            pv = k.ps[bk][:].bitcast(BF16)
                            n8 = min(8, qt + 1 - c8 * 8)
                            for j in range(n8):
                                kt = c8 * 8 + j
                                k.tr(pv[:, j * 128:(j + 1) * 128], k.psb[bk], p_t[:, kt * 128:(kt + 1) * 128], idb[:], reads=[p_b, idbb])
                            k.op("act", lambda e: e.activation(out=pt_t[:, c8 * 8:c8 * 8 + n8, :], in_=pv[:, 0:n8 * 128].rearrange("p (a b) -> p a b", a=n8), func=AF.Copy),
                                 reads=[k.psb[bk]], writes=[pt_b])
                        for kt in range(qt + 1):
                            k.mm(k.ps[6][:, 0:128], k.psb[6], pt_t[:, kt, :], v_t[:, kt, hl * 128:(hl + 1) * 128], kt == 0, kt == qt, reads=[pt_b, v_b])
                        k.op("dve", lambda e: e.scalar_tensor_tensor(out=o_t[:], in0=k.ps[6][:, 0:128], scalar=st4[:, 3:4], in1=g_t[:, qt, hl * 128:(hl + 1) * 128],
                                                                     op0=ALU.mult, op1=ALU.mult), reads=[k.psb[6], st4b, g_b], writes=[o_b])
                        pv7 = k.ps[7][:].bitcast(BF16)
                        k.tr(pv7[:, 0:128], k.psb[7], o_t[:], idb[:], reads=[o_b, idbb])
                        k.op("act", lambda e: e.activation(out=ogT[:, h, qt * 128:(qt + 1) * 128], in_=pv7[:, 0:128], func=AF.Copy), reads=[k.psb[7]], writes=[ogTb[qt]])
            with Scope(k) as sc:
                wo, wob = sc.sb("wo", [128, NH, D], BF16)
                wv = A["l1_fox_w_out"].rearrange("(c p) n -> p c n", p=128)
                for i in range(4):
                    k.dma("pool", wo[:, 4 * i:4 * i + 4, :], wv[:, 4 * i:4 * i + 4, :], wob, writes=[wob])
                xt = [sc.sb("xt", [128, D], F32) for _ in range(2)]
                ot = [sc.sb("ot", [128, D], F32) for _ in range(2)]
                for t in range(NT):
                    x_t, x_b = xt[t % 2]
                    o_t, o_b = ot[t % 2]
                    k.dma("sp", x_t[:], h_in[t * 128:(t + 1) * 128, :], x_b, writes=[x_b])
                    for n4 in range(4):
                        bk = (t % 2) * 4 + n4
                        for c in range(NH):
                            k.mm(k.ps[bk][:], k.psb[bk], ogT[:, c, t * 128:(t + 1) * 128], wo[:, c, n4 * 512:(n4 + 1) * 512], c == 0, c == NH - 1,
                                 reads=[ogTb[t], wob])
                        k.op("dve", lambda e: e.tensor_tensor(out=o_t[:, n4 * 512:(n4 + 1) * 512], in0=k.ps[bk][:], in1=x_t[:, n4 * 512:(n4 + 1) * 512], op=ALU.add),
                             reads=[k.psb[bk], x_b], writes=[o_b])
                    k.dma("sp", h_out[t * 128:(t + 1) * 128, :], o_t[:], o_b, reads=[o_b])


_CACHE = {}


def build_full(arr_shapes):
    k = K()
    nc = k.nc
    A = {}
    for nm, shp in arr_shapes.items():
        A[nm] = nc.dram_tensor(nm, list(shp), F32, kind="ExternalInput").ap()
    out = nc.dram_tensor("out", [S, D], F32, kind="ExternalOutput").ap()
    hA = nc.dram_tensor("hA", [S, D], F32, kind="Internal").ap()
    hB = nc.dram_tensor("hB", [S, D], F32, kind="Internal").ap()
    yg = nc.dram_tensor("yg", [NG, CAP, D], F32, kind="Internal").ap()
    qTd = nc.dram_tensor("qTd", [NH, 128, S], BF16, kind="Internal").ap()
    kTd = nc.dram_tensor("kTd", [NH, 128, S], BF16, kind="Internal").ap()
    vd = nc.dram_tensor("vd", [S, D], BF16, kind="Internal").ap()
    sgd = nc.dram_tensor("sgd", [S, D], BF16, kind="Internal").ap()
    phase_s5(k, A, A["x"], hA)
    phase_moe(k, A, "l0", hA, hB, yg)
    phase_fox(k, A, hB, hA, qTd, kTd, vd, sgd)
    phase_moe(k, A, "l1", hA, out, yg, final_gain=A["final_norm"])
    k.barrier()
    return k


def kernel(**inputs):
    x = np.asarray(inputs["x"], np.float32)
    arrs = prep_common(inputs)
    shapes = {nm: a.shape for nm, a in arrs.items()}
    shapes["x"] = (S, D)
    k = build_full(shapes)
    in_maps = []
    for c in range(8):
        m = dict(arrs)
        m["x"] = np.ascontiguousarray(x[c])
        in_maps.append(m)
    res = run_bass_kernel_spmd(k.nc, in_maps, core_ids=list(range(8)))
    return np.stack([np.asarray(r["out"], np.float32) for r in res.results], axis=0)
```

```python
import numpy as np
from contextlib import ExitStack
import concourse.bass as bass
import concourse.mybir as mybir
from concourse.bass_utils import run_bass_kernel_spmd

F32 = mybir.dt.float32
BF16 = mybir.dt.bfloat16
I32 = mybir.dt.int32
AF = mybir.ActivationFunctionType
ALU = mybir.AluOpType
AX = mybir.AxisListType

D = 2048
S = 2048
NT = S // 128
NF = D // 128
DS = 1024
NQ = DS // 128
NPAIR = 32
NH = 16
EPS = 1e-6
NG = 8
EPG = 8
NE = 64
DE = 512
CAP = 384
NST = CAP // 128

SAME_SYNC = True


class Buf:
    __slots__ = ("w", "r", "ds", "name")

    def __init__(self, name=""):
        self.w = None
        self.r = {}
        self.ds = None
        self.name = name


class K:
    def __init__(self):
        nc = bass.Bass("TRN2", target_bir_lowering=False)
        self.nc = nc
        self.eng = dict(pe=nc.tensor, dve=nc.vector, act=nc.scalar, pool=nc.gpsimd, sp=nc.sync)
        self.sem = {k: nc.alloc_semaphore("s_" + k) for k in self.eng}
        self.cnt = {k: 0 for k in self.eng}
        self.seen = {k: {} for k in self.eng}
        self.dpool = []
        self.dall = []
        self.nbuf = 0
        self.ps = []
        self.psb = []
        for i in range(8):
            t = nc.alloc_psum_tensor("ps%d" % i, [128, 512], F32)
            self.ps.append(t)
            self.psb.append(Buf("ps%d" % i))

    def _dslot(self, b):
        if b.ds is None:
            if self.dpool:
                b.ds = self.dpool.pop()
            else:
                b.ds = [self.nc.alloc_semaphore("d%d" % len(self.dall)), 0]
                self.dall.append(b.ds)
        return b.ds

    def release(self, bufs):
        for b in bufs:
            if b.ds is not None:
                self.dpool.append(b.ds)
                b.ds = None

    def _waits(self, eng, reads, writes):
        need = {}

        def add(t):
            k, v = t
            kk = id(k) if isinstance(k, list) else k
            if kk not in need or need[kk][1] < v:
                need[kk] = (k, v)

        for b in reads:
            if b.w:
                add(b.w)
        for b in writes:
            if b.w:
                add(b.w)
            for t in b.r.values():
                add(t)
        e = self.eng[eng]
        for kk, (k, v) in need.items():
            if isinstance(k, list):
                v = max(v, k[1])
                h = k[0]
            else:
                if k == eng and (eng == "pe" or not SAME_SYNC):
                    continue
                h = self.sem[k]
            if self.seen[eng].get(kk, 0) >= v:
                continue
            e.wait_ge(h, v)
            self.seen[eng][kk] = v

    def op(self, eng, emit, reads=(), writes=()):
        self._waits(eng, reads, writes)
        ins = emit(self.eng[eng])
        self.cnt[eng] += 1
        ins.then_inc(self.sem[eng], 1)
        t = (eng, self.cnt[eng])
        for b in reads:
            b.r[eng] = t
        for b in writes:
            b.w = t
            b.r = {}
        return ins

    def dma(self, q, out, in_, sb, reads=(), writes=()):
        self._waits(q, reads, writes)
        ds = self._dslot(sb)
        ins = self.eng[q].dma_start(out=out, in_=in_)
        ds[1] += 16
        ins.then_inc(ds[0], 16)
        t = (ds, ds[1])
        for b in reads:
            b.r[id(ds)] = t
        for b in writes:
            b.w = t
            b.r = {}
        return ins

    def barrier(self):
        for e in self.eng:
            for k in self.eng:
                if k == e:
                    continue
                v = self.cnt[k]
                if v and self.seen[e].get(k, 0) < v:
                    self.eng[e].wait_ge(self.sem[k], v)
                    self.seen[e][k] = v
            for ds in self.dall:
                if ds[1] and self.seen[e].get(id(ds), 0) < ds[1]:
                    self.eng[e].wait_ge(ds[0], ds[1])
                    self.seen[e][id(ds)] = ds[1]

    def mm(self, bank_ap, bank_buf, lhsT, rhs, start, stop, reads):
        return self.op("pe", lambda e: e.matmul(bank_ap, lhsT, rhs, start=start, stop=stop),
                       reads=reads, writes=[bank_buf])

    def tr(self, out_ap, out_buf, in_ap, ident, reads):
        return self.op("pe", lambda e: e.transpose(out_ap, in_ap, ident), reads=reads, writes=[out_buf])


class Scope:
    def __init__(self, k):
        self.k = k
        self.es = ExitStack()
        self.bufs = []

    def __enter__(self):
        self.es.__enter__()
        return self

    def __exit__(self, *a):
        self.k.barrier()
        self.k.release(self.bufs)
        return self.es.__exit__(*a)

    def sb(self, name, shape, dt):
        self.k.nbuf += 1
        t = self.es.enter_context(self.k.nc.sbuf_tensor("%s_%d" % (name, self.k.nbuf), list(shape), dt))
        b = Buf(name)
        self.bufs.append(b)
        return t, b

    def buf(self, name=""):
        b = Buf(name)
        self.bufs.append(b)
        return b


def make_ident(k, sc):
    it, ib = sc.sb("iota_i", [128, 128], I32)
    k.op("pool", lambda e: e.iota(it[:], pattern=[[1, 128]], base=0, channel_multiplier=-1), writes=[ib])
    idb, idbb = sc.sb("ident_bf", [128, 128], BF16)
    idf, idfb = sc.sb("ident_f", [128, 128], F32)
    k.op("dve", lambda e: e.tensor_scalar(idb[:], it[:], 0, None, op0=ALU.is_equal), reads=[ib], writes=[idbb])
    k.op("dve", lambda e: e.tensor_scalar(idf[:], it[:], 0, None, op0=ALU.is_equal), reads=[ib], writes=[idfb])
    return (idb, idbb), (idf, idfb)


def rms_tile(k, sc, x_t, x_b, gain_t, gain_b, out_t, out_b, tmp):
    junk, junkb, ss, ssb = tmp["junk"], tmp["junkb"], tmp["ss"], tmp["ssb"]
    k.op("act", lambda e: e.activation(out=junk[:], in_=x_t, func=AF.Square, accum_out=ss[:, 0:1]),
         reads=[x_b], writes=[junkb, ssb])
    k.op("dve", lambda e: e.tensor_scalar(ss[:, 1:2], ss[:, 0:1], 1.0 / D, EPS, op0=ALU.mult, op1=ALU.add),
         reads=[ssb], writes=[ssb])
    k.op("act", lambda e: e.activation(out=ss[:, 2:3], in_=ss[:, 1:2], func=AF.Sqrt), reads=[ssb], writes=[ssb])
    k.op("dve", lambda e: e.reciprocal(ss[:, 3:4], ss[:, 2:3]), reads=[ssb], writes=[ssb])
    k.op("dve", lambda e: e.scalar_tensor_tensor(out=out_t, in0=x_t, scalar=ss[:, 3:4], in1=gain_t,
                                                 op0=ALU.mult, op1=ALU.mult),
         reads=[x_b, ssb, gain_b], writes=[out_b])


def rms_tmp(sc):
    junk, junkb = sc.sb("junk", [128, D], BF16)
    ss, ssb = sc.sb("ss", [128, 4], F32)
    return dict(junk=junk, junkb=junkb, ss=ss, ssb=ssb)


def small_ops(k, buf):
    def tt(out, a, b, op, extra_r=(), eng="dve"):
        k.op(eng, lambda e: e.tensor_tensor(out=out, in0=a, in1=b, op=op), reads=[buf, *extra_r], writes=[buf])

    def ts(out, a, s1, s2, op0, op1=None, extra_r=(), eng="dve"):
        if op1 is None:
            k.op(eng, lambda e: e.tensor_scalar(out, a, s1, None, op0=op0), reads=[buf, *extra_r], writes=[buf])
        else:
            k.op(eng, lambda e: e.tensor_scalar(out, a, s1, s2, op0=op0, op1=op1), reads=[buf, *extra_r], writes=[buf])

    def stt(out, a, sc, b, op0, op1, extra_r=()):
        k.op("dve", lambda e: e.scalar_tensor_tensor(out=out, in0=a, scalar=sc, in1=b, op0=op0, op1=op1),
             reads=[buf, *extra_r], writes=[buf])

    def act(out, a, func, scale=None, bias=None, extra_r=()):
        kw = {}
        if scale is not None:
            kw["scale"] = scale
        if bias is not None:
            kw["bias"] = bias
        k.op("act", lambda e: e.activation(out=out, in_=a, func=func, **kw), reads=[buf, *extra_r], writes=[buf])
    return tt, ts, stt, act


def phase_s5(k, A, x_in, h_out, gTd):
    with Scope(k) as sc0:
        uT, _ = sc0.sb("uT", [128, NQ, S], F32)
        ub = [[sc0.buf() for _ in range(4)] for _ in range(NQ)]
        (idb, idbb), _ = make_ident(k, sc0)
        with Scope(k) as sc:
            gain, gainb = sc.sb("gain", [128, D], F32)
            k.dma("sp", gain[:], A["l0_mix_norm"], gainb, writes=[gainb])
            win, winb = sc.sb("win", [128, NF, DS], BF16)
            wv = A["l0_s5_w_in"].rearrange("(fc p) n -> p fc n", p=128)
            for i in range(4):
                k.dma("pool", win[:, 4 * i:4 * i + 4, :], wv[:, 4 * i:4 * i + 4, :], winb, writes=[winb])
            tmp = rms_tmp(sc)
            xt = [sc.sb("xt", [128, D], F32) for _ in range(2)]
            hn = [sc.sb("hn", [128, D], BF16) for _ in range(2)]
            hnT = [sc.sb("hnT", [128, NF, 512], BF16) for _ in range(2)]
            for st in range(4):
                hT, hTb = hnT[st % 2]
                for tl in range(4):
                    tt_ = st * 4 + tl
                    x_t, x_b = xt[tt_ % 2]
                    h_t, h_b = hn[tt_ % 2]
                    k.dma("sp", x_t[:], x_in[tt_ * 128:(tt_ + 1) * 128, :], x_b, writes=[x_b])
                    rms_tile(k, sc, x_t[:], x_b, gain[:], gainb, h_t[:], h_b, tmp)
                    for half in range(2):
                        bk = (tt_ % 2) * 2 + half
                        pv = k.ps[bk][:].bitcast(BF16)
                        for j in range(8):
                            fc = half * 8 + j
                            k.tr(pv[:, j * 128:(j + 1) * 128], k.psb[bk], h_t[:, fc * 128:(fc + 1) * 128], idb[:],
                                 reads=[h_b, idbb])
                        src = pv.rearrange("p (a b) -> p a b", a=8)
                        dst = hT[:, half * 8:half * 8 + 8, tl * 128:(tl + 1) * 128]
                        if half == 0:
                            k.op("act", lambda e: e.activation(out=dst, in_=src, func=AF.Copy),
                                 reads=[k.psb[bk]], writes=[hTb])
                        else:
                            k.op("dve", lambda e: e.tensor_copy(out=dst, in_=src), reads=[k.psb[bk]], writes=[hTb])
                for q in range(NQ):
                    bk = 4 + (q % 4)
                    for fc in range(NF):
                        k.mm(k.ps[bk][:], k.psb[bk], win[:, fc, q * 128:(q + 1) * 128], hT[:, fc, :],
                             fc == 0, fc == NF - 1, reads=[winb, hTb])
                    k.op("act", lambda e: e.activation(out=uT[:, q, st * 512:(st + 1) * 512], in_=k.ps[bk][:], func=AF.Copy),
                         reads=[k.psb[bk]], writes=[ub[q][st]])
        with Scope(k) as sc1:
            with Scope(k) as sc:
                prm, prmb = sc.sb("prm", [128, 24, NPAIR], F32)
                lam, lamb = sc.sb("lam", [128, 3, NPAIR], F32)
                k.dma("sp", lam[:], A["s5_lam"], lamb, writes=[lamb])
                CD, _ = sc.sb("CD", [128, 11, NPAIR], F32)
                SD, _ = sc.sb("SD", [128, 11, NPAIR], F32)
                dsk, dskb = sc.sb("dsk", [128, NQ], F32)
                k.dma("sp", dsk[:], A["s5_d"], dskb, writes=[dskb])
                bwr, bwrb = sc.sb("bwr", [128, NQ, 4, 128], BF16)
                bwi, bwib = sc.sb("bwi", [128, NQ, 4, 128], BF16)
                k.dma("pool", bwr[:], A["s5_bw_re"], bwrb, writes=[bwrb])
                k.dma("pool", bwi[:], A["s5_bw_im"], bwib, writes=[bwib])
                cfr, cfrb = sc.sb("cfr", [128, NPAIR, 128], BF16)
                cfi, cfib = sc.sb("cfi", [128, NPAIR, 128], BF16)
                tt, ts, stt, act = small_ops(k, prmb)
                P = lambda i: prm[:, i, :]
                lr, li, lst = lam[:, 0, :], lam[:, 1, :], lam[:, 2, :]
                act(P(0), lst, AF.Exp, extra_r=[lamb])
                tt(P(1), lr, P(0), ALU.mult, extra_r=[lamb])
                tt(P(2), li, P(0), ALU.mult, extra_r=[lamb])
                act(P(3), P(1), AF.Exp)
                ts(P(4), P(2), 1.0 / 64, None, ALU.mult)
                tt(P(5), P(4), P(4), ALU.mult)
                ts(P(6), P(5), -1.0 / 42, 1.0, ALU.mult, ALU.add)
                tt(P(6), P(6), P(5), ALU.mult)
                ts(P(6), P(6), -1.0 / 20, 1.0, ALU.mult, ALU.add)
                tt(P(6), P(6), P(5), ALU.mult)
                ts(P(6), P(6), -1.0 / 6, 1.0, ALU.mult, ALU.add)
                tt(P(7), P(6), P(4), ALU.mult)
                ts(P(6), P(5), -1.0 / 56, 1.0, ALU.mult, ALU.add)
                tt(P(6), P(6), P(5), ALU.mult)
                ts(P(6), P(6), -1.0 / 30, 1.0, ALU.mult, ALU.add)
                tt(P(6), P(6), P(5), ALU.mult)
                ts(P(6), P(6), -1.0 / 12, 1.0, ALU.mult, ALU.add)
                tt(P(6), P(6), P(5), ALU.mult)
                ts(P(8), P(6), -0.5, 1.0, ALU.mult, ALU.add)
                ci, si = 8, 7
                free = [9, 10]
                for _ in range(6):
                    cn, sn = free
                    tt(P(11), P(ci), P(ci), ALU.mult)
                    tt(P(12), P(si), P(si), ALU.mult)
                    tt(P(cn), P(11), P(12), ALU.subtract)
                    stt(P(sn), P(ci), 2.0, P(si), ALU.mult, ALU.mult)
                    free = [ci, si]
                    ci, si = cn, sn
                k.op("dve", lambda e: e.tensor_copy(out=CD[:, 0, :], in_=P(ci)), reads=[prmb], writes=[prmb])
                k.op("dve", lambda e: e.tensor_copy(out=SD[:, 0, :], in_=P(si)), reads=[prmb], writes=[prmb])
                for kk in range(10):
                    tt(P(11), CD[:, kk, :], CD[:, kk, :], ALU.mult)
                    tt(P(12), SD[:, kk, :], SD[:, kk, :], ALU.mult)
                    tt(CD[:, kk + 1, :], P(11), P(12), ALU.subtract)
                    stt(SD[:, kk + 1, :], CD[:, kk, :], 2.0, SD[:, kk, :], ALU.mult, ALU.mult)
                NSD, _ = sc.sb("NSD", [128, 11, NPAIR], F32)
                ts(NSD[:], SD[:], -1.0, None, ALU.mult)
                tt(P(13), P(3), P(ci), ALU.mult)
                tt(P(14), P(3), P(si), ALU.mult)
                ts(P(15), P(13), -1.0, None, ALU.add)
                tt(P(16), lr, lr, ALU.mult, extra_r=[lamb])
                tt(P(17), li, li, ALU.mult, extra_r=[lamb])
                tt(P(16), P(16), P(17), ALU.add)
                k.op("dve", lambda e: e.reciprocal(P(17), P(16)), reads=[prmb], writes=[prmb])
                tt(P(18), P(15), lr, ALU.mult, extra_r=[lamb])
                tt(P(19), P(14), li, ALU.mult, extra_r=[lamb])
                tt(P(18), P(18), P(19), ALU.add)
                tt(P(20), P(18), P(17), ALU.mult)
                tt(P(18), P(14), lr, ALU.mult, extra_r=[lamb])
                tt(P(19), P(15), li, ALU.mult, extra_r=[lamb])
                tt(P(18), P(18), P(19), ALU.subtract)
                tt(P(21), P(18), P(17), ALU.mult)
                ts(P(22), P(21), -1.0, None, ALU.mult)
                with Scope(k) as scc:
                    cwr, cwrb = scc.sb("cwr", [128, NPAIR, 128], F32)
                    cwi, cwib = scc.sb("cwi", [128, NPAIR, 128], F32)
                    ctmp, ctmpb = scc.sb("ctmp", [128, 128], F32)
                    k.dma("sp", cwr[:], A["s5_cw_re"], cwrb, writes=[cwrb])
                    k.dma("sp", cwi[:], A["s5_cw_im"], cwib, writes=[cwib])
                    for j in range(NPAIR):
                        k.op("dve", lambda e: e.tensor_scalar(ctmp[:], cwi[:, j, :], prm[:, 22, j:j + 1], None, op0=ALU.mult),
                             reads=[cwib, prmb], writes=[ctmpb])
                        k.op("dve", lambda e: e.scalar_tensor_tensor(out=cfr[:, j, :], in0=cwr[:, j, :], scalar=prm[:, 20, j:j + 1],
                                                                     in1=ctmp[:], op0=ALU.mult, op1=ALU.add),
                             reads=[cwrb, prmb, ctmpb], writes=[cfrb])
                        k.op("dve", lambda e: e.tensor_scalar(ctmp[:], cwi[:, j, :], prm[:, 20, j:j + 1], -1.0, op0=ALU.mult, op1=ALU.mult),
                             reads=[cwib, prmb], writes=[ctmpb])
                        k.op("dve", lambda e: e.scalar_tensor_tensor(out=cfi[:, j, :], in0=cwr[:, j, :], scalar=prm[:, 22, j:j + 1],
                                                                     in1=ctmp[:], op0=ALU.mult, op1=ALU.add),
                             reads=[cwrb, prmb, ctmpb], writes=[cfib])
                COSb = [sc.sb("COS", [128, S], F32) for _ in range(2)]
                SINb = [sc.sb("SIN", [128, S], F32) for _ in range(2)]
                TA, TAb = sc.sb("TA", [128, 4, 512], F32)
                WK, wkb = sc.sb("WK", [128, 6, 512], F32)
                wre, wreb = sc.sb("wre", [128, 2, 512], F32)
                wim, wimb = sc.sb("wim", [128, 2, 512], F32)
                BR, BRb = sc.sb("BR", [128, 4, 512], F32)
                xb, _ = sc.sb("xb", [128, 2, 2, 512], BF16)
                xbb = [sc.buf() for _ in range(2)]
                uBq, uBqb = sc.sb("uBq", [128, S], BF16)
                ysb, ysbb = sc.sb("ysb", [128, 512], F32)
                gw = [sc.sb("gw%d" % i, [128, 512], F32) for i in range(3)]
                gst = [sc.sb("gst", [128, 512], BF16) for _ in range(2)]
                for i in range(2):
                    k.op("dve", lambda e: e.memset(COSb[i][0][:, 0:1], 1.0), writes=[COSb[i][1]])
                    k.op("dve", lambda e: e.memset(SINb[i][0][:, 0:1], 0.0), writes=[SINb[i][1]])

                def table_steps(j, kks):
                    C, Cb = COSb[j % 2]
                    S_, Sb = SINb[j % 2]
                    for kk in kks:
                        d = 1 << kk
                        cd, sd, nsd = CD[:, kk, j:j + 1], SD[:, kk, j:j + 1], NSD[:, kk, j:j + 1]
                        if kk < 7:
                            k.op("dve", lambda e: e.tensor_scalar(TA[:, 0, 0:d], S_[:, 0:d], nsd, None, op0=ALU.mult), reads=[Sb, prmb], writes=[TAb])
                            k.op("dve", lambda e: e.tensor_scalar(TA[:, 1, 0:d], C[:, 0:d], sd, None, op0=ALU.mult), reads=[Cb, prmb], writes=[TAb])
                            k.op("dve", lambda e: e.scalar_tensor_tensor(out=C[:, d:2 * d], in0=C[:, 0:d], scalar=cd, in1=TA[:, 0, 0:d], op0=ALU.mult, op1=ALU.add),
                                 reads=[Cb, TAb, prmb], writes=[Cb])
                            k.op("dve", lambda e: e.scalar_tensor_tensor(out=S_[:, d:2 * d], in0=S_[:, 0:d], scalar=cd, in1=TA[:, 1, 0:d], op0=ALU.mult, op1=ALU.add),
                                 reads=[Sb, TAb, prmb], writes=[Sb])
                        else:
                            for lo in range(0, d, 512):
                                n = min(512, d - lo)
                                hi = lo + n
                                k.op("act", lambda e: e.activation(out=TA[:, 0, 0:n], in_=C[:, lo:hi], func=AF.Copy, scale=cd), reads=[Cb, prmb], writes=[TAb])
                                k.op("act", lambda e: e.activation(out=TA[:, 1, 0:n], in_=S_[:, lo:hi], func=AF.Copy, scale=sd), reads=[Sb, prmb], writes=[TAb])
                                k.op("act", lambda e: e.activation(out=TA[:, 2, 0:n], in_=S_[:, lo:hi], func=AF.Copy, scale=cd), reads=[Sb, prmb], writes=[TAb])
                                k.op("act", lambda e: e.activation(out=TA[:, 3, 0:n], in_=C[:, lo:hi], func=AF.Copy, scale=sd), reads=[Cb, prmb], writes=[TAb])
                                k.op("pool", lambda e: e.tensor_tensor(out=C[:, d + lo:d + hi], in0=TA[:, 0, 0:n], in1=TA[:, 1, 0:n], op=ALU.subtract), reads=[TAb], writes=[Cb])
                                k.op("pool", lambda e: e.tensor_tensor(out=S_[:, d + lo:d + hi], in0=TA[:, 2, 0:n], in1=TA[:, 3, 0:n], op=ALU.add), reads=[TAb], writes=[Sb])

                table_steps(0, range(11))
                sched = [list(range(0, 8)), [8], [9], [10]]
                BR2, _ = sc.sb("BR2", [128, 4, 512], F32)
                BRs = [(BR, BRb), (BR2, sc.buf())]

                def stage_A(idx, j, nt):
                    q, pp = j // 4, j % 4
                    B_, Bb_ = BRs[idx % 2]
                    xi = idx % 2
                    k.op("dve", lambda e: e.tensor_tensor(out=xb[:, xi, 0, :], in0=B_[:, 0, :], in1=B_[:, 1, :], op=ALU.subtract), reads=[Bb_], writes=[xbb[xi]])
                    k.op("dve", lambda e: e.tensor_tensor(out=xb[:, xi, 1, :], in0=B_[:, 2, :], in1=B_[:, 3, :], op=ALU.add), reads=[Bb_], writes=[xbb[xi]])
                    bk = 2 + nt
                    k.mm(k.ps[bk][:], k.psb[bk], cfr[:, j, :], xb[:, xi, 0, :], pp == 0, False, reads=[cfrb, xbb[xi]])
                    k.mm(k.ps[bk][:], k.psb[bk], cfi[:, j, :], xb[:, xi, 1, :], False, pp == 3, reads=[cfib, xbb[xi]])
                    if pp == 3 and nt == 3:
                        for n2 in range(4):
                            sl2 = slice(n2 * 512, (n2 + 1) * 512)
                            bk2 = 2 + n2
                            k.op("dve", lambda e: e.scalar_tensor_tensor(out=ysb[:], in0=uT[:, q, sl2], scalar=dsk[:, q:q + 1], in1=k.ps[bk2][:],
                                                                         op0=ALU.mult, op1=ALU.add),
                                 reads=[ub[q][n2], dskb, k.psb[bk2]], writes=[ysbb])
                            (g1, g1b), (g2, g2b), (g3, g3b) = gw
                            go, gob = gst[n2 % 2]
                            k.op("act", lambda e: e.activation(out=g1[:], in_=ysb[:], func=AF.Square), reads=[ysbb], writes=[g1b])
                            k.op("pool", lambda e: e.tensor_scalar(g2[:], g1[:], 0.044715, 1.0, op0=ALU.mult, op1=ALU.add), reads=[g1b], writes=[g2b])
                            k.op("pool", lambda e: e.tensor_tensor(out=g3[:], in0=g2[:], in1=ysb[:], op=ALU.mult), reads=[g2b, ysbb], writes=[g3b])
                            k.op("act", lambda e: e.activation(out=g1[:], in_=g3[:], func=AF.Sigmoid, scale=1.5957691216057308), reads=[g3b], writes=[g1b])
                            k.op("pool", lambda e: e.tensor_tensor(out=go[:], in0=g1[:], in1=ysb[:], op=ALU.mult), reads=[g1b, ysbb], writes=[gob])
                            k.dma("sp", gTd[q, :, sl2], go[:], gob, reads=[gob])

                items = [(j, nt) for j in range(NPAIR) for nt in range(4)]
                for idx, (j, nt) in enumerate(items):
                    q, pp = j // 4, j % 4
                    COS, cosb = COSb[j % 2]
                    SIN, sinb = SINb[j % 2]
                    rcol = prm[:, 3, j:j + 1]
                    if pp == 0 and nt == 0:
                        k.op("act", lambda e: e.activation(out=uBq[:], in_=uT[:, q, :], func=AF.Copy), reads=ub[q], writes=[uBqb])
                    sl = slice(nt * 512, (nt + 1) * 512)
                    rows = slice(64 * (pp // 2), 64 * (pp // 2) + 64)
                    b0, b1 = (0, 1) if nt % 2 == 0 else (6, 7)
                    k.mm(k.ps[b0][:], k.psb[b0], bwr[rows, q, pp, :], uBq[rows, sl], True, True, reads=[bwrb, uBqb])
                    k.mm(k.ps[b1][:], k.psb[b1], bwi[rows, q, pp, :], uBq[rows, sl], True, True, reads=[bwib, uBqb])
                    t1, t2, t3, t4, pr, pi = [WK[:, i, :] for i in range(6)]
                    C_, S_ = COS[:, sl], SIN[:, sl]
                    k.op("dve", lambda e: e.tensor_tensor(out=t1, in0=k.ps[b0][:], in1=C_, op=ALU.mult), reads=[k.psb[b0], cosb], writes=[wkb])
                    k.op("dve", lambda e: e.tensor_tensor(out=t2, in0=k.ps[b1][:], in1=S_, op=ALU.mult), reads=[k.psb[b1], sinb], writes=[wkb])
                    k.op("dve", lambda e: e.tensor_tensor(out=t3, in0=k.ps[b1][:], in1=C_, op=ALU.mult), reads=[k.psb[b1], cosb], writes=[wkb])
                    k.op("dve", lambda e: e.tensor_tensor(out=t4, in0=k.ps[b0][:], in1=S_, op=ALU.mult), reads=[k.psb[b0], sinb], writes=[wkb])
                    k.op("dve", lambda e: e.tensor_tensor(out=pr, in0=t1, in1=t2, op=ALU.add), reads=[wkb], writes=[wkb])
                    k.op("dve", lambda e: e.tensor_tensor(out=pi, in0=t3, in1=t4, op=ALU.subtract), reads=[wkb], writes=[wkb])
                    ini_r = 0.0 if nt == 0 else wre[:, (nt - 1) % 2, 511:512]
                    ini_i = 0.0 if nt == 0 else wim[:, (nt - 1) % 2, 511:512]
                    rb = rcol.to_broadcast([128, 512])
                    k.op("dve", lambda e: e.tensor_tensor_scan(out=wre[:, nt % 2, :], data0=rb, data1=pr, initial=ini_r, op0=ALU.mult, op1=ALU.add),
                         reads=[wkb, prmb, wreb], writes=[wreb])
                    k.op("dve", lambda e: e.tensor_tensor_scan(out=wim[:, nt % 2, :], data0=rb, data1=pi, initial=ini_i, op0=ALU.mult, op1=ALU.add),
                         reads=[wkb, prmb, wimb], writes=[wimb])
                    B_, Bb_ = BRs[idx % 2]
                    k.op("pool", lambda e: e.tensor_tensor(out=B_[:, 0, :], in0=wre[:, nt % 2, :], in1=C_, op=ALU.mult), reads=[wreb, cosb], writes=[Bb_])
                    k.op("pool", lambda e: e.tensor_tensor(out=B_[:, 1, :], in0=wim[:, nt % 2, :], in1=S_, op=ALU.mult), reads=[wimb, sinb], writes=[Bb_])
                    k.op("pool", lambda e: e.tensor_tensor(out=B_[:, 2, :], in0=wre[:, nt % 2, :], in1=S_, op=ALU.mult), reads=[wreb, sinb], writes=[Bb_])
                    k.op("pool", lambda e: e.tensor_tensor(out=B_[:, 3, :], in0=wim[:, nt % 2, :], in1=C_, op=ALU.mult), reads=[wimb, cosb], writes=[Bb_])
                    if j + 1 < NPAIR:
                        table_steps(j + 1, sched[nt])
                    if idx > 0:
                        stage_A(idx - 1, *items[idx - 1])
                stage_A(len(items) - 1, *items[-1])
            with Scope(k) as sc2:
                oT, _ = sc2.sb("oT", [128, NQ, S], BF16)
                ob = [[sc2.buf() for _ in range(4)] for _ in range(NQ)]
                gT, gTb = sc2.sb("gT", [128, NQ, S], BF16)
                for q_ in range(NQ):
                    k.dma("sp", gT[:, q_, :], gTd[q_], gTb, writes=[gTb])
                with Scope(k) as sc:
                    wg, wgb = sc.sb("wglu", [128, NQ, DS], BF16)
                    wv = A["l0_s5_w_glu"].rearrange("(c p) n -> p c n", p=128)
                    for i in range(2):
                        k.dma("pool", wg[:, 4 * i:4 * i + 4, :], wv[:, 4 * i:4 * i + 4, :], wgb, writes=[wgb])
                    bg, bgb = sc.sb("bglu", [128, NQ], F32)
                    k.dma("sp", bg[:], A["s5_b_glu"], bgb, writes=[bgb])
                    sg = [sc.sb("sg%d" % i, [128, 512], F32) for i in range(2)]
                    it = 0
                    for q in range(NQ):
                        for nt in range(4):
                            sl = slice(nt * 512, (nt + 1) * 512)
                            bk = it % 4
                            s_t, s_b = sg[it % 2]
                            it += 1
                            for c in range(NQ):
                                k.mm(k.ps[bk][:], k.psb[bk], wg[:, c, q * 128:(q + 1) * 128], gT[:, c, sl], c == 0, c == NQ - 1,
                                     reads=[wgb, gTb])
                            k.op("act", lambda e: e.activation(out=s_t[:], in_=k.ps[bk][:], func=AF.Sigmoid, bias=bg[:, q:q + 1]),
                                 reads=[k.psb[bk], bgb], writes=[s_b])
                            k.op("dve", lambda e: e.tensor_tensor(out=oT[:, q, sl], in0=s_t[:], in1=gT[:, q, sl], op=ALU.mult),
                                 reads=[s_b, gTb], writes=[ob[q][nt]])
                with Scope(k) as sc:
                    wo, wob = sc.sb("wout", [128, NQ, D], BF16)
                    wv = A["l0_s5_w_out"].rearrange("(c p) n -> p c n", p=128)
                    for i in range(4):
                        k.dma("pool", wo[:, 2 * i:2 * i + 2, :], wv[:, 2 * i:2 * i + 2, :], wob, writes=[wob])
                    xt = [sc.sb("xt", [128, D], F32) for _ in range(2)]
                    ot = [sc.sb("ot", [128, D], F32) for _ in range(2)]
                    for tt_ in range(NT):
                        x_t, x_b = xt[tt_ % 2]
                        o_t, o_b = ot[tt_ % 2]
                        k.dma("sp", x_t[:], x_in[tt_ * 128:(tt_ + 1) * 128, :], x_b, writes=[x_b])
                        nt = tt_ // 4
                        for n4 in range(4):
                            bk = (tt_ % 2) * 4 + n4
                            for c in range(NQ):
                                k.mm(k.ps[bk][:], k.psb[bk], oT[:, c, tt_ * 128:(tt_ + 1) * 128], wo[:, c, n4 * 512:(n4 + 1) * 512],
                                     c == 0, c == NQ - 1, reads=[ob[c][nt], wob])
                            k.op("dve", lambda e: e.tensor_tensor(out=o_t[:, n4 * 512:(n4 + 1) * 512], in0=k.ps[bk][:],
                                                                  in1=x_t[:, n4 * 512:(n4 + 1) * 512], op=ALU.add),
                                 reads=[k.psb[bk], x_b], writes=[o_b])
                        k.dma("sp", h_out[tt_ * 128:(tt_ + 1) * 128, :], o_t[:], o_b, reads=[o_b])


def prep_common(inp):
    f = np.float32
    out = {}
    for nm in ["l0_mix_norm", "l0_ffn_norm", "l1_mix_norm", "l1_ffn_norm", "final_norm"]:
        out[nm] = np.ascontiguousarray(np.broadcast_to(np.asarray(inp[nm], f)[None, :], (128, D)))
    lam_re = np.asarray(inp["l0_s5_lambda_re"], f)
    lam_im = np.asarray(inp["l0_s5_lambda_im"], f)
    lst = np.repeat(np.asarray(inp["l0_s5_log_step"], f), 64).reshape(64, 64)
    lam = np.stack([a.reshape(NPAIR, 128).T for a in (lam_re, lam_im, lst)], axis=1)
    out["s5_lam"] = np.ascontiguousarray(lam)
    b_re = np.asarray(inp["l0_s5_b_re"], f)
    b_im = np.asarray(inp["l0_s5_b_im"], f)
    c_re = np.asarray(inp["l0_s5_c_re"], f)
    c_im = np.asarray(inp["l0_s5_c_im"], f)
    bwr = np.zeros((128, NQ, 4, 128), f)
    bwi = np.zeros((128, NQ, 4, 128), f)
    cwr = np.zeros((128, NPAIR, 128), f)
    cwi = np.zeros((128, NPAIR, 128), f)
    for g in range(64):
        q, r0, gl, j = g // 8, (g % 8) * 16, g % 2, g // 2
        bwr[r0:r0 + 16, q, (g % 8) // 2, 64 * gl:64 * gl + 64] = b_re[g].T
        bwi[r0:r0 + 16, q, (g % 8) // 2, 64 * gl:64 * gl + 64] = b_im[g].T
        cwr[64 * gl:64 * gl + 64, j, r0:r0 + 16] = c_re[g].T
        cwi[64 * gl:64 * gl + 64, j, r0:r0 + 16] = c_im[g].T
    out["s5_bw_re"], out["s5_bw_im"], out["s5_cw_re"], out["s5_cw_im"] = bwr, bwi, cwr, cwi
    out["s5_d"] = np.ascontiguousarray(np.asarray(inp["l0_s5_d"], f).reshape(NQ, 128).T)
    out["s5_b_glu"] = np.ascontiguousarray(np.asarray(inp["l0_s5_b_glu"], f).reshape(NQ, 128).T)
    for nm in ["l0_s5_w_in", "l0_s5_w_glu", "l0_s5_w_out"]:
        out[nm] = np.asarray(inp[nm], f)
    out["l1_fox_w_in"] = np.asarray(inp["l1_fox_w_in"], f)
    out["l1_fox_w_out"] = np.asarray(inp["l1_fox_w_out"], f)
    out["l1_fox_b_forget"] = np.ascontiguousarray(np.asarray(inp["l1_fox_b_forget"], f).reshape(NH, 1))
    for L in ("l0", "l1"):
        out[L + "_moe_wr"] = np.ascontiguousarray(np.concatenate([np.asarray(inp[L + "_moe_w_group"], f), np.asarray(inp[L + "_moe_w_expert"], f)], axis=1))
        brow = np.concatenate([np.asarray(inp[L + "_moe_b_group"], f), np.asarray(inp[L + "_moe_b_expert"], f)])
        out[L + "_moe_br"] = np.ascontiguousarray(np.broadcast_to(brow[None, :], (128, 72)))
        for nm in ["_moe_w_gate", "_moe_w_up", "_moe_w_down"]:
            out[L + nm] = np.asarray(inp[L + nm], f)
    return out


def declare_inputs(k, arrays):
    A = {}
    for nm, a in arrays.items():
        A[nm] = k.nc.dram_tensor(nm, list(a.shape), F32, kind="ExternalInput").ap()
    return A


def phase_moe(k, A, L, h_in, h_out, yg, final_gain=None):
    with Scope(k) as sc0:
        cwb, cwbb = sc0.sb("cwb", [128, NT, NE], BF16)
        ohb, ohbb = sc0.sb("ohb", [128, NT, NG], BF16)
        ohf, ohfb = sc0.sb("ohf", [128, NT, NG], F32)
        rs, rsb = sc0.sb("rs", [128, NT, NG], F32)
        rankT, rankTb = sc0.sb("rankT", [NG, S], F32)
        (idb, idbb), (idf, idfb) = make_ident(k, sc0)
        iota_c, iocb = sc0.sb("iota_c", [128, CAP], F32)
        slot_idx, slib = sc0.sb("slot_idx", [128, NST], F32)
        tri, trib = sc0.sb("tri", [128, 128], BF16)
        ones, onesb = sc0.sb("ones", [128, 128], BF16)
        Eg, Egb = sc0.sb("Eg", [NG, NG, 128], F32)
        with Scope(k) as sct:
            ii, iib = sct.sb("ii", [128, 512], I32)
            k.op("pool", lambda e: e.iota(ii[:, 0:CAP], pattern=[[1, CAP]], base=0, channel_multiplier=0), writes=[iib])
            k.op("dve", lambda e: e.tensor_copy(out=iota_c[:], in_=ii[:, 0:CAP]), reads=[iib], writes=[iocb])
            k.op("pool", lambda e: e.iota(ii[:, 0:NST], pattern=[[128, NST]], base=0, channel_multiplier=1), reads=[iocb], writes=[iib])
            k.op("dve", lambda e: e.tensor_copy(out=slot_idx[:], in_=ii[:, 0:NST]), reads=[iib], writes=[slib])
            k.op("pool", lambda e: e.iota(ii[:, 0:128], pattern=[[1, 128]], base=0, channel_multiplier=-1), reads=[slib], writes=[iib])
            k.op("dve", lambda e: e.tensor_scalar(tri[:], ii[:, 0:128], 0, None, op0=ALU.is_gt), reads=[iib], writes=[trib])
            k.op("dve", lambda e: e.memset(ones[:], 1.0), writes=[onesb])
            i8, i8b = sct.sb("i8", [NG, NG, 128], I32)
            k.op("pool", lambda e: e.iota(i8[:], pattern=[[1, NG], [0, 128]], base=0, channel_multiplier=-1), writes=[i8b])
            k.op("dve", lambda e: e.tensor_scalar(Eg[:], i8[:], 0, None, op0=ALU.is_equal), reads=[i8b], writes=[Egb])
        scH = Scope(k)
        scH.__enter__()
        hnb, _ = scH.sb("hnb", [128, NT, D], BF16)
        hnbb = [scH.buf() for _ in range(NT)]
        if True:
            with Scope(k) as sc:
                gain, gainb = sc.sb("gain", [128, D], F32)
                k.dma("sp", gain[:], A[L + "_ffn_norm"], gainb, writes=[gainb])
                wr, wrb = sc.sb("wr", [128, NF, 72], F32)
                k.dma("sp", wr[:], A[L + "_moe_wr"].rearrange("(fc p) n -> p fc n", p=128), wrb, writes=[wrb])
                br, brb = sc.sb("br", [128, 72], F32)
                k.dma("sp", br[:], A[L + "_moe_br"], brb, writes=[brb])
                tmp = rms_tmp(sc)
                xt = [sc.sb("xt", [128, D], F32) for _ in range(2)]
                hnf = [sc.sb("hnf", [128, D], F32) for _ in range(2)]
                hnT = [sc.sb("hnTf", [128, NF, 128], F32) for _ in range(2)]
                rt, rtb = sc.sb("rt", [128, 320], F32)
                tt_, ts_, stt_, act_ = small_ops(k, rtb)
                for t in range(NT):
                    x_t, x_b = xt[t % 2]
                    f_t, f_b = hnf[t % 2]
                    T_t, T_b = hnT[t % 2]
                    k.dma("sp", x_t[:], h_in[t * 128:(t + 1) * 128, :], x_b, writes=[x_b])
                    rms_tile(k, sc, x_t[:], x_b, gain[:], gainb, f_t[:], f_b, tmp)
                    k.op("act", lambda e: e.activation(out=hnb[:, t, :], in_=f_t[:], func=AF.Copy), reads=[f_b], writes=[hnbb[t]])
                    for qd in range(4):
                        bk = qd
                        for j in range(4):
                            fc = qd * 4 + j
                            k.tr(k.ps[bk][:, j * 128:(j + 1) * 128], k.psb[bk], f_t[:, fc * 128:(fc + 1) * 128], idf[:], reads=[f_b, idfb])
                        src = k.ps[bk][:].rearrange("p (a b) -> p a b", a=4)
                        dst = T_t[:, qd * 4:qd * 4 + 4, :]
                        if qd % 2 == 0:
                            k.op("act", lambda e: e.activation(out=dst, in_=src, func=AF.Copy), reads=[k.psb[bk]], writes=[T_b])
                        else:
                            k.op("dve", lambda e: e.tensor_copy(out=dst, in_=src), reads=[k.psb[bk]], writes=[T_b])
                    bk = 4 + (t % 2)
                    for fc in range(NF):
                        k.mm(k.ps[bk][:, 0:72], k.psb[bk], T_t[:, fc, :], wr[:, fc, :], fc == 0, fc == NF - 1, reads=[T_b, wrb])
                    R = lambda a, b: rt[:, a:b]
                    lg, m8, ngm, eg, sg, pg = R(0, 72), R(72, 80), R(80, 81), R(81, 89), R(89, 90), R(90, 91)
                    pen, me, t8 = R(91, 99), R(99, 163), R(163, 171)
                    dl, ex, den, w1, w2, c1, c2 = R(171, 172), R(172, 173), R(173, 174), R(174, 175), R(175, 176), R(176, 240), R(240, 304)
                    tt_(lg, k.ps[bk][:, 0:72], br[:], ALU.add, extra_r=[k.psb[bk], brb])
                    k.op("dve", lambda e: e.max(out=m8, in_=rt[:, 0:8]), reads=[rtb], writes=[rtb])
                    oh_t = ohf[:, t, :]
                    k.op("dve", lambda e: e.tensor_scalar(oh_t, rt[:, 0:8], rt[:, 72:73], None, op0=ALU.is_equal), reads=[rtb], writes=[ohfb])
                    k.op("dve", lambda e: e.tensor_copy(out=ohb[:, t, :], in_=oh_t), reads=[ohfb], writes=[ohbb])
                    ts_(ngm, rt[:, 72:73], -1.0, None, ALU.mult)
                    k.op("act", lambda e: e.activation(out=eg, in_=rt[:, 0:8], func=AF.Exp, bias=rt[:, 80:81], scale=1.0, accum_out=sg),
                         reads=[rtb], writes=[rtb])
                    k.op("dve", lambda e: e.reciprocal(pg, sg), reads=[rtb], writes=[rtb])
                    ts_(pen, oh_t, -1.0, 1e30, ALU.add, ALU.mult, extra_r=[ohfb])
                    me3 = me.rearrange("p (a b) -> p a b", a=8)
                    el3 = rt[:, 8:72].rearrange("p (a b) -> p a b", a=8)
                    pen3 = pen.unsqueeze(2).to_broadcast([128, 8, 8])
                    tt_(me3, el3, pen3, ALU.add)
                    k.op("dve", lambda e: e.max(out=t8, in_=me), reads=[rtb], writes=[rtb])
                    tt_(dl, rt[:, 164:165], rt[:, 163:164], ALU.subtract)
                    act_(ex, dl, AF.Exp)
                    ts_(den, ex, 1.0, None, ALU.add)
                    k.op("dve", lambda e: e.reciprocal(w1, den), reads=[rtb], writes=[rtb])
                    tt_(w2, ex, w1, ALU.mult)
                    tt_(w1, w1, pg, ALU.mult)
                    tt_(w2, w2, pg, ALU.mult)
                    ts_(c1, me, rt[:, 163:164], rt[:, 174:175], ALU.is_equal, ALU.mult)
                    ts_(c2, me, rt[:, 164:165], rt[:, 175:176], ALU.is_equal, ALU.mult)
                    k.op("dve", lambda e: e.tensor_tensor(out=cwb[:, t, :], in0=c1, in1=c2, op=ALU.add), reads=[rtb], writes=[cwbb])
                for t in range(NT):
                    bk = 6 + (t % 2)
                    k.mm(k.ps[bk][:, 0:NG], k.psb[bk], tri[:], ohb[:, t, :], True, t == 0, reads=[trib, ohbb])
                    for t2 in range(t):
                        k.mm(k.ps[bk][:, 0:NG], k.psb[bk], ones[:], ohb[:, t2, :], False, t2 == t - 1, reads=[onesb, ohbb])
                    k.op("dve", lambda e: e.scalar_tensor_tensor(out=rs[:, t, :], in0=k.ps[bk][:, 0:NG], scalar=1.0, in1=ohf[:, t, :],
                                                                 op0=ALU.add, op1=ALU.mult), reads=[k.psb[bk], ohfb], writes=[rsb])
                    k.op("dve", lambda e: e.tensor_scalar(rs[:, t, :], rs[:, t, :], -1.0, None, op0=ALU.add), reads=[rsb], writes=[rsb])
                for t in range(NT):
                    bk = t % 2
                    k.tr(k.ps[bk][0:NG, 0:128], k.psb[bk], rs[:, t, :], idf[:], reads=[rsb, idfb])
                    k.op("dve", lambda e: e.tensor_copy(out=rankT[:, t * 128:(t + 1) * 128], in_=k.ps[bk][0:NG, 0:128]), reads=[k.psb[bk]], writes=[rankTb])
            with Scope(k) as sc:
                NU = 8
                ring, _ = sc.sb("ring", [128, NU, 4096], BF16)
                ringb = [sc.buf() for _ in range(NU)]
                sel, selb = sc.sb("sel", [128, NT, CAP], BF16)
                xg, xgb = sc.sb("xg", [128, NF, CAP], BF16)
                cwg, cwgb = sc.sb("cwg", [128, NST, EPG], F32)
                yacc, _ = sc.sb("yacc", [128, NST, D], F32)
                yaccb = [sc.buf() for _ in range(NST)]
                hid = [sc.sb("hid", [128, 4, CAP], BF16) for _ in range(2)]
                sgt = [sc.sb("sgt", [128, CAP], F32) for _ in range(2)]
                wg_v = A[L + "_moe_w_gate"]
                wu_v = A[L + "_moe_w_up"]
                wd_v = A[L + "_moe_w_down"]

                def unit_src(u):
                    e, j = u // 6, u % 6
                    if j < 4:
                        w = wg_v if j < 2 else wu_v
                        h = j % 2
                        return w[e].rearrange("(fc p) n -> p fc n", p=128)[:, h * 8:h * 8 + 8, :], "p (a b) -> p a b", 8
                    h = j - 4
                    return wd_v[e].rearrange("(c p) n -> p c n", p=128)[:, h * 2:h * 2 + 2, :], "p (a b) -> p a b", 2

                def issue_unit(u):
                    if u >= NE * 6:
                        return
                    src, pat, a = unit_src(u)
                    slot = u % NU
                    k.dma("pool", ring[:, slot, :].rearrange(pat, a=a), src, ringb[slot], writes=[ringb[slot]])

                for u in range(NU):
                    issue_unit(u)
                cnt_ev = 0
                for g in range(NG):
                    for t in range(NT):
                        eng = "dve" if t % 2 == 0 else "pool"
                        k.op(eng, lambda e: e.tensor_scalar(sel[:, t, :], iota_c[:], rs[:, t, g:g + 1], None, op0=ALU.is_equal),
                             reads=[iocb, rsb], writes=[selb])
                    for fc in range(NF):
                        bk = fc % 2
                        for t in range(NT):
                            k.mm(k.ps[bk][:, 0:CAP], k.psb[bk], hnb[:, t, fc * 128:(fc + 1) * 128], sel[:, t, :], t == 0, t == NT - 1,
                                 reads=[hnbb[t], selb])
                        if fc % 2 == 0:
                            k.op("act", lambda e: e.activation(out=xg[:, fc, :], in_=k.ps[bk][:, 0:CAP], func=AF.Copy), reads=[k.psb[bk]], writes=[xgb])
                        else:
                            k.op("dve", lambda e: e.tensor_copy(out=xg[:, fc, :], in_=k.ps[bk][:, 0:CAP]), reads=[k.psb[bk]], writes=[xgb])
                    for st in range(NST):
                        bk = st % 2
                        for t in range(NT):
                            k.mm(k.ps[bk][:, 0:EPG], k.psb[bk], sel[:, t, st * 128:(st + 1) * 128], cwb[:, t, g * EPG:(g + 1) * EPG],
                                 t == 0, t == NT - 1, reads=[selb, cwbb])
                        k.op("dve", lambda e: e.tensor_copy(out=cwg[:, st, :], in_=k.ps[bk][:, 0:EPG]), reads=[k.psb[bk]], writes=[cwgb])
                    for el in range(EPG):
                        e_id = g * EPG + el
                        u0 = e_id * 6
                        h_t, h_b = hid[e_id % 2]
                        for hc in range(4):
                            s_t, s_b = sgt[hc % 2]
                            bg, bu = 2 + (hc % 2), 4 + (hc % 2)
                            for fc in range(NF):
                                slot = (u0 + fc // 8) % NU
                                wv = ring[:, slot, :].rearrange("p (a b) -> p a b", a=8)
                                k.mm(k.ps[bg][:, 0:CAP], k.psb[bg], wv[:, fc % 8, hc * 128:(hc + 1) * 128], xg[:, fc, :], fc == 0, fc == NF - 1,
                                     reads=[ringb[slot], xgb])
                            for fc in range(NF):
                                slot = (u0 + 2 + fc // 8) % NU
                                wv = ring[:, slot, :].rearrange("p (a b) -> p a b", a=8)
                                k.mm(k.ps[bu][:, 0:CAP], k.psb[bu], wv[:, fc % 8, hc * 128:(hc + 1) * 128], xg[:, fc, :], fc == 0, fc == NF - 1,
                                     reads=[ringb[slot], xgb])
                            k.op("act", lambda e: e.activation(out=s_t[:], in_=k.ps[bg][:, 0:CAP], func=AF.Silu), reads=[k.psb[bg]], writes=[s_b])
                            k.op("dve", lambda e: e.tensor_tensor(out=h_t[:, hc, :], in0=s_t[:], in1=k.ps[bu][:, 0:CAP], op=ALU.mult),
                                 reads=[s_b, k.psb[bu]], writes=[h_b])
                        for j in range(4):
                            issue_unit(u0 + NU + j)
                        for st in range(NST):
                            for n4 in range(4):
                                bk = 6 + (cnt_ev % 2)
                                cnt_ev += 1
                                for hc in range(4):
                                    slot = (u0 + 4 + hc // 2) % NU
                                    wv = ring[:, slot, :].rearrange("p (a b) -> p a b", a=2)
                                    k.mm(k.ps[bk][:], k.psb[bk], h_t[:, hc, st * 128:(st + 1) * 128], wv[:, hc % 2, n4 * 512:(n4 + 1) * 512],
                                         hc == 0, hc == 3, reads=[h_b, ringb[slot]])
                                ya = yacc[:, st, n4 * 512:(n4 + 1) * 512]
                                if el == 0:
                                    k.op("dve", lambda e: e.tensor_scalar(ya, k.ps[bk][:], cwg[:, st, el:el + 1], None, op0=ALU.mult),
                                         reads=[k.psb[bk], cwgb], writes=[yaccb[st]])
                                else:
                                    k.op("dve", lambda e: e.scalar_tensor_tensor(out=ya, in0=k.ps[bk][:], scalar=cwg[:, st, el:el + 1], in1=ya,
                                                                                 op0=ALU.mult, op1=ALU.add),
                                         reads=[k.psb[bk], cwgb, yaccb[st]], writes=[yaccb[st]])
                        for j in range(4, 6):
                            issue_unit(u0 + NU + j)
                    for st in range(NST):
                        k.dma("sp", yg[g, st * 128:(st + 1) * 128, :], yacc[:, st, :], yaccb[st], reads=[yaccb[st]])
        scH.__exit__(None, None, None)
        with Scope(k) as sc:
            ygs, ygsb = sc.sb("ygs", [128, NG, NST, D], BF16)
            for g in range(NG):
                k.dma("pool", ygs[:, g, :, :], yg[g].rearrange("(st p) d -> p st d", p=128), ygsb, writes=[ygsb])
            selT = [sc.sb("selT", [128, NST, NG, 128], BF16) for _ in range(2)]
            xt = [sc.sb("xt", [128, D], F32) for _ in range(2)]
            ot = [sc.sb("ot", [128, D], F32) for _ in range(2)]
            if final_gain is not None:
                fg, fgb = sc.sb("fgain", [128, D], F32)
                k.dma("sp", fg[:], final_gain, fgb, writes=[fgb])
                tmp = rms_tmp(sc)
                ot2 = [sc.sb("ot2", [128, D], F32) for _ in range(2)]
            for t in range(NT):
                x_t, x_b = xt[t % 2]
                o_t, o_b = ot[t % 2]
                sT, sTb = selT[t % 2]
                k.dma("sp", x_t[:], h_in[t * 128:(t + 1) * 128, :], x_b, writes=[x_b])
                for half in range(2):
                    bk = half
                    for j in range(4):
                        g = half * 4 + j
                        k.mm(k.ps[bk][:, j * 128:(j + 1) * 128], k.psb[bk], Eg[:, g, :], rankT[:, t * 128:(t + 1) * 128], True, True,
                             reads=[Egb, rankTb])
                    for st in range(NST):
                        k.op("dve", lambda e: e.tensor_scalar(sT[:, st, half * 4:half * 4 + 4, :], k.ps[bk][:].rearrange("p (a b) -> p a b", a=4),
                                                              slot_idx[:, st:st + 1], None, op0=ALU.is_equal),
                             reads=[k.psb[bk], slib], writes=[sTb])
                for n4 in range(4):
                    bk = 2 + ((t * 4 + n4) % 6)
                    i = 0
                    for g in range(NG):
                        for st in range(NST):
                            k.mm(k.ps[bk][:], k.psb[bk], sT[:, st, g, :], ygs[:, g, st, n4 * 512:(n4 + 1) * 512], i == 0, i == NG * NST - 1,
                                 reads=[sTb, ygsb])
                            i += 1
                    k.op("dve", lambda e: e.tensor_tensor(out=o_t[:, n4 * 512:(n4 + 1) * 512], in0=k.ps[bk][:], in1=x_t[:, n4 * 512:(n4 + 1) * 512],
                                                          op=ALU.add), reads=[k.psb[bk], x_b], writes=[o_b])
                if final_gain is None:
                    k.dma("sp", h_out[t * 128:(t + 1) * 128, :], o_t[:], o_b, reads=[o_b])
                else:
                    o2, o2b = ot2[t % 2]
                    rms_tile(k, sc, o_t[:], o_b, fg[:], fgb, o2[:], o2b, tmp)
                    k.dma("sp", h_out[t * 128:(t + 1) * 128, :], o2[:], o2b, reads=[o2b])


def phase_fox(k, A, h_in, h_out, qTd, kTd, vd, sgd):
    SCALE = 128 ** -0.5
    with Scope(k) as sc0:
        (idb, idbb), (idf, idfb) = make_ident(k, sc0)
        cumT, cumTb = sc0.sb("cumT", [NH, S], F32)
        with Scope(k) as sc1:
            hnT, _ = sc1.sb("hnT", [128, NF, S], BF16)
            hnTb = [sc1.buf() for _ in range(NT)]
            with Scope(k) as sc:
                gain, gainb = sc.sb("gain", [128, D], F32)
                k.dma("sp", gain[:], A["l1_mix_norm"], gainb, writes=[gainb])
                tmp = rms_tmp(sc)
                xt = [sc.sb("xt", [128, D], F32) for _ in range(2)]
                hn = [sc.sb("hn", [128, D], BF16) for _ in range(2)]
                for t in range(NT):
                    x_t, x_b = xt[t % 2]
                    h_t, h_b = hn[t % 2]
                    k.dma("sp", x_t[:], h_in[t * 128:(t + 1) * 128, :], x_b, writes=[x_b])
                    rms_tile(k, sc, x_t[:], x_b, gain[:], gainb, h_t[:], h_b, tmp)
                    for half in range(2):
                        bk = (t % 2) * 2 + half
                        pv = k.ps[bk][:].bitcast(BF16)
                        for j in range(8):
                            fc = half * 8 + j
                            k.tr(pv[:, j * 128:(j + 1) * 128], k.psb[bk], h_t[:, fc * 128:(fc + 1) * 128], idb[:], reads=[h_b, idbb])
                        src = pv.rearrange("p (a b) -> p a b", a=8)
                        dst = hnT[:, half * 8:half * 8 + 8, t * 128:(t + 1) * 128]
                        if half == 0:
                            k.op("act", lambda e: e.activation(out=dst, in_=src, func=AF.Copy), reads=[k.psb[bk]], writes=[hnTb[t]])
                        else:
                            k.op("dve", lambda e: e.tensor_copy(out=dst, in_=src), reads=[k.psb[bk]], writes=[hnTb[t]])
            with Scope(k) as sc:
                NU = 4
                ring, _ = sc.sb("ring", [128, NU, NF, 512], BF16)
                ringb = [sc.buf() for _ in range(NU)]
                win = A["l1_fox_w_in"]

                def issue_unit(u):
                    if u >= 16:
                        return
                    kind, hg = u % 4, u // 4
                    c0 = kind * D + hg * 512
                    src = win[:, c0:c0 + 512].rearrange("(fc p) n -> p fc n", p=128)
                    sl = u % NU
                    for hf in range(2):
                        k.dma("pool", ring[:, sl, hf * 8:hf * 8 + 8, :], src[:, hf * 8:hf * 8 + 8, :], ringb[sl], writes=[ringb[sl]])

                for u in range(NU):
                    issue_unit(u)
                wf, wfb = sc.sb("wf", [128, NF, NH], BF16)
                k.dma("pool", wf[:], win[:, 4 * D:4 * D + NH].rearrange("(fc p) n -> p fc n", p=128), wfb, writes=[wfb])
                bf_, bfb = sc.sb("bfg", [NH, 2], F32)
                k.dma("sp", bf_[:, 0:1], A["l1_fox_b_forget"], bfb, writes=[bfb])
                k.op("dve", lambda e: e.tensor_scalar(bf_[:, 1:2], bf_[:, 0:1], -1.0, None, op0=ALU.mult), reads=[bfb], writes=[bfb])
                lf, lfb = sc.sb("lf", [NH, S], F32)
                ones16, o16b = sc.sb("ones16", [NH, 512], F32)
                k.op("dve", lambda e: e.memset(ones16[:], 1.0), writes=[o16b])
                for nt in range(4):
                    sl = slice(nt * 512, (nt + 1) * 512)
                    bk = nt
                    for fc in range(NF):
                        k.mm(k.ps[bk][0:NH, :], k.psb[bk], wf[:, fc, :], hnT[:, fc, sl], fc == 0, fc == NF - 1,
                             reads=[wfb] + hnTb[nt * 4:nt * 4 + 4])
                    k.op("act", lambda e: e.activation(out=lf[:, sl], in_=k.ps[bk][0:NH, :], func=AF.Exp, scale=-1.0, bias=bf_[:, 1:2]),
                         reads=[k.psb[bk], bfb], writes=[lfb])
                    k.op("act", lambda e: e.activation(out=lf[:, sl], in_=lf[:, sl], func=AF.Ln, bias=1.0, scale=1.0), reads=[lfb], writes=[lfb])
                    ini = 0.0 if nt == 0 else cumT[:, nt * 512 - 1:nt * 512]
                    k.op("dve", lambda e: e.tensor_tensor_scan(out=cumT[:, sl], data0=ones16[:], data1=lf[:, sl], initial=ini, op0=ALU.mult, op1=ALU.add),
                         reads=[lfb, o16b, cumTb], writes=[cumTb])
                qst = [sc.sb("qst", [128, S], BF16) for _ in range(2)]
                vst = [sc.sb("vst", [128, NT, 512], BF16) for _ in range(2)]
                nq = 0
                nb = 0
                for u in range(16):
                    kind, hg = u % 4, u // 4
                    sl_ = u % NU
                    if kind < 2:
                        dstd = qTd if kind == 0 else kTd
                        for hl in range(4):
                            h = hg * 4 + hl
                            q_t, q_b = qst[nq % 2]
                            nq += 1
                            for nt in range(4):
                                sl = slice(nt * 512, (nt + 1) * 512)
                                bk = nb % 4
                                nb += 1
                                for fc in range(NF):
                                    k.mm(k.ps[bk][:], k.psb[bk], ring[:, sl_, fc, hl * 128:(hl + 1) * 128], hnT[:, fc, sl], fc == 0, fc == NF - 1,
                                         reads=[ringb[sl_]] + hnTb[nt * 4:nt * 4 + 4])
                                sc_ = SCALE if kind == 0 else 1.0
                                if nt % 2 == 0:
                                    k.op("act", lambda e: e.activation(out=q_t[:, sl], in_=k.ps[bk][:], func=AF.Copy, scale=sc_), reads=[k.psb[bk]], writes=[q_b])
                                else:
                                    k.op("dve", lambda e: e.tensor_scalar(q_t[:, sl], k.ps[bk][:], sc_, None, op0=ALU.mult), reads=[k.psb[bk]], writes=[q_b])
                            k.dma("sp", dstd[h], q_t[:], q_b, reads=[q_b])
                    else:
                        dstd = vd if kind == 2 else sgd
                        v_t, v_b = vst[kind % 2]
                        for t in range(NT):
                            bk = 4 + (t % 4)
                            for fc in range(NF):
                                k.mm(k.ps[bk][:], k.psb[bk], hnT[:, fc, t * 128:(t + 1) * 128], ring[:, sl_, fc, :], fc == 0, fc == NF - 1,
                                     reads=[ringb[sl_], hnTb[t]])
                            if kind == 2:
                                if t % 2 == 0:
                                    k.op("act", lambda e: e.activation(out=v_t[:, t, :], in_=k.ps[bk][:], func=AF.Copy), reads=[k.psb[bk]], writes=[v_b])
                                else:
                                    k.op("dve", lambda e: e.tensor_copy(out=v_t[:, t, :], in_=k.ps[bk][:]), reads=[k.psb[bk]], writes=[v_b])
                            else:
                                k.op("act", lambda e: e.activation(out=v_t[:, t, :], in_=k.ps[bk][:], func=AF.Sigmoid), reads=[k.psb[bk]], writes=[v_b])
                        k.dma("sp", dstd[:, hg * 512:(hg + 1) * 512].rearrange("(t p) c -> p t c", p=128), v_t[:], v_b, reads=[v_b])
                    issue_unit(u + NU)
        with Scope(k) as sc2:
            ogT, _ = sc2.sb("ogT", [128, NH, S], BF16)
            ogTb = [sc2.buf() for _ in range(NT)]
            with Scope(k) as sc:
                E16, E16b = sc.sb("E16", [NH, NH, 128], F32)
                maskt, maskb = sc.sb("maskt", [128, 128], F32)
                with Scope(k) as sct:
                    i16, i16b = sct.sb("i16", [NH, NH, 128], I32)
                    k.op("pool", lambda e: e.iota(i16[:], pattern=[[1, NH], [0, 128]], base=0, channel_multiplier=-1), writes=[i16b])
                    k.op("dve", lambda e: e.tensor_scalar(E16[:], i16[:], 0, None, op0=ALU.is_equal), reads=[i16b], writes=[E16b])
                    im, imb = sct.sb("im", [128, 128], I32)
                    k.op("pool", lambda e: e.iota(im[:], pattern=[[1, 128]], base=0, channel_multiplier=-1), writes=[imb])
                    k.op("dve", lambda e: e.tensor_scalar(maskt[:], im[:], 0, -30000.0, op0=ALU.is_gt, op1=ALU.mult), reads=[imb], writes=[maskb])
                qh = [sc.sb("qh", [128, S], BF16) for _ in range(2)]
                kh = [sc.sb("kh", [128, S], BF16) for _ in range(2)]
                vg = [sc.sb("vg", [128, NT, 512], BF16) for _ in range(1)]
                sgg = [sc.sb("sgg", [128, NT, 512], BF16) for _ in range(1)]
                ssb = [sc.sb("ssb", [128, S], F32) for _ in range(3)]
                pb = [sc.sb("pb", [128, S], BF16) for _ in range(3)]
                maskbf, maskbfb = sc.sb("maskbf", [128, 128], BF16)
                k.op("dve", lambda e: e.tensor_copy(out=maskbf[:], in_=maskt[:]), reads=[maskb], writes=[maskbfb])
                ptb = [sc.sb("ptb", [128, NT, 128], BF16) for _ in range(2)]
                negck, negckb = sc.sb("negck", [128, S], F32)
                ogt = [sc.sb("ogt", [128, 128], BF16) for _ in range(2)]

                def load_head(h):
                    k.dma("sp", qh[h % 2][0][:], qTd[h], qh[h % 2][1], writes=[qh[h % 2][1]])
                    k.dma("sp", kh[h % 2][0][:], kTd[h], kh[h % 2][1], writes=[kh[h % 2][1]])

                def load_group(hg):
                    k.dma("sp", vg[0][0][:], vd[:, hg * 512:(hg + 1) * 512].rearrange("(t p) c -> p t c", p=128), vg[0][1], writes=[vg[0][1]])
                    k.dma("sp", sgg[0][0][:], sgd[:, hg * 512:(hg + 1) * 512].rearrange("(t p) c -> p t c", p=128), sgg[0][1], writes=[sgg[0][1]])

                st4s = [sc.sb("st4", [128, 8], F32) for _ in range(3)]
                load_head(0)

                def head_setup(h):
                    for nt in range(4):
                        sl = slice(nt * 512, (nt + 1) * 512)
                        bk = 4 + (nt % 2)
                        k.mm(k.ps[bk][:], k.psb[bk], E16[:, h, :], cumT[:, sl], True, True, reads=[E16b, cumTb])
                        k.op("act", lambda e: e.activation(out=negck[:, sl], in_=k.ps[bk][:], func=AF.Copy), reads=[k.psb[bk]], writes=[negckb])

                def stage_A(it, h, qt):
                    if qt == 0:
                        head_setup(h)
                    q_t, q_b = qh[h % 2]
                    k_t, k_b = kh[h % 2]
                    nk = (qt + 1) * 128
                    nb_ = (nk + 511) // 512
                    s_t, s_b = ssb[it % 3]
                    p_t, p_b = pb[it % 3]
                    s4, s4b = st4s[it % 3]
                    for b in range(nb_):
                        w = min(512, nk - b * 512)
                        last = (b == nb_ - 1)
                        k.mm(k.ps[b][:, 0:w], k.psb[b], q_t[:, qt * 128:(qt + 1) * 128], k_t[:, b * 512:b * 512 + w], True, not last, reads=[q_b, k_b])
                        if last:
                            k.mm(k.ps[b][:, w - 128:w], k.psb[b], idb[:], maskbf[:], False, True, reads=[idbb, maskbfb])
                        k.op("dve", lambda e: e.tensor_tensor(out=s_t[:, b * 512:b * 512 + w], in0=k.ps[b][:, 0:w], in1=negck[:, b * 512:b * 512 + w], op=ALU.add),
                             reads=[k.psb[b], negckb], writes=[s_b])
                    k.op("dve", lambda e: e.tensor_reduce(out=s4[:, 1:2], in_=s_t[:, 0:nk], axis=AX.X, op=ALU.max, negate=True), reads=[s_b, s4b], writes=[s4b])
                    k.op("act", lambda e: e.activation(out=p_t[:, 0:nk], in_=s_t[:, 0:nk], func=AF.Exp, bias=s4[:, 1:2], scale=1.0, accum_out=s4[:, 2:3]),
                         reads=[s_b, s4b], writes=[p_b, s4b])

                pvb = [Buf("pv6"), Buf("pv7")]
                tgb = [Buf("tg6"), Buf("tg7")]

                def stage_B(it, h, qt):
                    hg, hl = h // 4, h % 4
                    if qt == 0:
                        if h + 1 < NH:
                            load_head(h + 1)
                    p_t, p_b = pb[it % 3]
                    pt_t, pt_b = ptb[it % 2]
                    for c8 in range((qt + 8) // 8):
                        bk = 4 + (c8 % 2)
                        pv = k.ps[bk][:].bitcast(BF16)
                        n8 = min(8, qt + 1 - c8 * 8)
                        for j in range(n8):
                            kt = c8 * 8 + j
                            k.tr(pv[:, j * 128:(j + 1) * 128], k.psb[bk], p_t[:, kt * 128:(kt + 1) * 128], idb[:], reads=[p_b, idbb])
                        k.op("act", lambda e: e.activation(out=pt_t[:, c8 * 8:c8 * 8 + n8, :], in_=pv[:, 0:n8 * 128].rearrange("p (a b) -> p a b", a=n8), func=AF.Copy),
                             reads=[k.psb[bk]], writes=[pt_b])

                def stage_C1(it, h, qt):
                    hg, hl = h // 4, h % 4
                    if qt == 0 and hl == 0:
                        load_group(hg)
                    v_t, v_b = vg[0]
                    g_t, g_b = sgg[0]
                    pt_t, pt_b = ptb[it % 2]
                    o_t, o_b = ogt[it % 2]
                    s4, s4b = st4s[it % 3]
                    po = k.ps[6 + (it % 2)][:, 0:128]
                    for kt in range(qt + 1):
                        k.mm(po, k.psb[6 + (it % 2)], pt_t[:, kt, :], v_t[:, kt, hl * 128:(hl + 1) * 128], kt == 0, kt == qt, reads=[pt_b, v_b])
                    k.op("dve", lambda e: e.reciprocal(s4[:, 3:4], s4[:, 2:3]), reads=[s4b], writes=[s4b])
                    k.op("dve", lambda e: e.scalar_tensor_tensor(out=o_t[:], in0=po, scalar=s4[:, 3:4], in1=g_t[:, qt, hl * 128:(hl + 1) * 128],
                                                                 op0=ALU.mult, op1=ALU.mult), reads=[k.psb[6 + (it % 2)], s4b, g_b], writes=[o_b])

                def stage_C2(it, h, qt):
                    o_t, o_b = ogt[it % 2]
                    pv7 = k.ps[6 + (it % 2)][:].bitcast(BF16)
                    to = pv7[:, 512:640]
                    k.tr(to, k.psb[6 + (it % 2)], o_t[:], idb[:], reads=[o_b, idbb])
                    k.op("act", lambda e: e.activation(out=ogT[:, h, qt * 128:(qt + 1) * 128], in_=to, func=AF.Copy), reads=[k.psb[6 + (it % 2)]], writes=[ogTb[qt]])

                aitems = [(h, qt) for h in range(NH) for qt in range(NT)]
                NI = len(aitems)
                for n in range(-3, NI):
                    if 0 <= n + 3 < NI:
                        stage_A(n + 3, *aitems[n + 3])
                    if 0 <= n + 2 < NI:
                        stage_B(n + 2, *aitems[n + 2])
                    if 0 <= n + 1 < NI:
                        stage_C1(n + 1, *aitems[n + 1])
                    if 0 <= n < NI:
                        stage_C2(n, *aitems[n])
            with Scope(k) as sc:
                wo, wob = sc.sb("wo", [128, NH, D], BF16)
                wv = A["l1_fox_w_out"].rearrange("(c p) n -> p c n", p=128)
                for i in range(4):
                    k.dma("pool", wo[:, 4 * i:4 * i + 4, :], wv[:, 4 * i:4 * i + 4, :], wob, writes=[wob])
                xt = [sc.sb("xt", [128, D], F32) for _ in range(2)]
                ot = [sc.sb("ot", [128, D], F32) for _ in range(2)]
                for t in range(NT):
                    x_t, x_b = xt[t % 2]
                    o_t, o_b = ot[t % 2]
                    k.dma("sp", x_t[:], h_in[t * 128:(t + 1) * 128, :], x_b, writes=[x_b])
                    for n4 in range(4):
                        bk = (t % 2) * 4 + n4
                        for c in range(NH):
                            k.mm(k.ps[bk][:], k.psb[bk], ogT[:, c, t * 128:(t + 1) * 128], wo[:, c, n4 * 512:(n4 + 1) * 512], c == 0, c == NH - 1,
                                 reads=[ogTb[t], wob])
                        k.op("dve", lambda e: e.tensor_tensor(out=o_t[:, n4 * 512:(n4 + 1) * 512], in0=k.ps[bk][:], in1=x_t[:, n4 * 512:(n4 + 1) * 512], op=ALU.add),
                             reads=[k.psb[bk], x_b], writes=[o_b])
                    k.dma("sp", h_out[t * 128:(t + 1) * 128, :], o_t[:], o_b, reads=[o_b])


_CACHE = {}


def build_full(arr_shapes):
    k = K()
    nc = k.nc
    A = {}
    for nm, shp in arr_shapes.items():
        A[nm] = nc.dram_tensor(nm, list(shp), F32, kind="ExternalInput").ap()
    out = nc.dram_tensor("out", [S, D], F32, kind="ExternalOutput").ap()
    hA = nc.dram_tensor("hA", [S, D], F32, kind="Internal").ap()
    hB = nc.dram_tensor("hB", [S, D], F32, kind="Internal").ap()
    yg = nc.dram_tensor("yg", [NG, CAP, D], F32, kind="Internal").ap()
    qTd = nc.dram_tensor("qTd", [NH, 128, S], BF16, kind="Internal").ap()
    kTd = nc.dram_tensor("kTd", [NH, 128, S], BF16, kind="Internal").ap()
    vd = nc.dram_tensor("vd", [S, D], BF16, kind="Internal").ap()
    sgd = nc.dram_tensor("sgd", [S, D], BF16, kind="Internal").ap()
    gTd = nc.dram_tensor("gTd", [NQ, 128, S], BF16, kind="Internal").ap()
    phase_s5(k, A, A["x"], hA, gTd)
    phase_moe(k, A, "l0", hA, hB, yg)
    phase_fox(k, A, hB, hA, qTd, kTd, vd, sgd)
    phase_moe(k, A, "l1", hA, out, yg, final_gain=A["final_norm"])
    k.barrier()
    return k


def kernel(**inputs):
    x = np.asarray(inputs["x"], np.float32)
    arrs = prep_common(inputs)
    shapes = {nm: a.shape for nm, a in arrs.items()}
    shapes["x"] = (S, D)
    k = build_full(shapes)
    in_maps = []
    for c in range(8):
        m = dict(arrs)
        m["x"] = np.ascontiguousarray(x[c])
        in_maps.append(m)
    res = run_bass_kernel_spmd(k.nc, in_maps, core_ids=list(range(8)))
    return np.stack([np.asarray(r["out"], np.float32) for r in res.results], axis=0)
```

```python
import numpy as np
from contextlib import ExitStack
import concourse.bass as bass
import concourse.mybir as mybir
from concourse.bass_utils import run_bass_kernel_spmd

F32 = mybir.dt.float32
BF16 = mybir.dt.bfloat16
I32 = mybir.dt.int32
AF = mybir.ActivationFunctionType
ALU = mybir.AluOpType
AX = mybir.AxisListType

D = 2048
S = 2048
NT = S // 128
NF = D // 128
DS = 1024
NQ = DS // 128
NPAIR = 32
NH = 16
EPS = 1e-6
NG = 8
EPG = 8
NE = 64
DE = 512
CAP = 384
NST = CAP // 128

SAME_SYNC = True


class Buf:
    __slots__ = ("w", "r", "ds", "name")

    def __init__(self, name=""):
        self.w = None
        self.r = {}
        self.ds = None
        self.name = name


class K:
    def __init__(self):
        nc = bass.Bass("TRN2", target_bir_lowering=False)
        self.nc = nc
        self.eng = dict(pe=nc.tensor, dve=nc.vector, act=nc.scalar, pool=nc.gpsimd, sp=nc.sync)
        self.sem = {k: nc.alloc_semaphore("s_" + k) for k in self.eng}
        self.cnt = {k: 0 for k in self.eng}
        self.seen = {k: {} for k in self.eng}
        self.dpool = []
        self.dall = []
        self.nbuf = 0
        self.ps = []
        self.psb = []
        for i in range(8):
            t = nc.alloc_psum_tensor("ps%d" % i, [128, 512], F32)
            self.ps.append(t)
            self.psb.append(Buf("ps%d" % i))

    def _dslot(self, b):
        if b.ds is None:
            if self.dpool:
                b.ds = self.dpool.pop()
            else:
                b.ds = [self.nc.alloc_semaphore("d%d" % len(self.dall)), 0]
                self.dall.append(b.ds)
        return b.ds

    def release(self, bufs):
        for b in bufs:
            if b.ds is not None:
                self.dpool.append(b.ds)
                b.ds = None

    def _waits(self, eng, reads, writes):
        need = {}

        def add(t):
            k, v = t
            kk = id(k) if isinstance(k, list) else k
            if kk not in need or need[kk][1] < v:
                need[kk] = (k, v)

        for b in reads:
            if b.w:
                add(b.w)
        for b in writes:
            if b.w:
                add(b.w)
            for t in b.r.values():
                add(t)
        e = self.eng[eng]
        for kk, (k, v) in need.items():
            if isinstance(k, list):
                v = max(v, k[1])
                h = k[0]
            else:
                if k == eng and (eng == "pe" or not SAME_SYNC):
                    continue
                h = self.sem[k]
            if self.seen[eng].get(kk, 0) >= v:
                continue
            e.wait_ge(h, v)
            self.seen[eng][kk] = v

    def op(self, eng, emit, reads=(), writes=()):
        self._waits(eng, reads, writes)
        ins = emit(self.eng[eng])
        self.cnt[eng] += 1
        ins.then_inc(self.sem[eng], 1)
        t = (eng, self.cnt[eng])
        for b in reads:
            b.r[eng] = t
        for b in writes:
            b.w = t
            b.r = {}
        return ins

    def dma(self, q, out, in_, sb, reads=(), writes=()):
        self._waits(q, reads, writes)
        ds = self._dslot(sb)
        ins = self.eng[q].dma_start(out=out, in_=in_)
        ds[1] += 16
        ins.then_inc(ds[0], 16)
        t = (ds, ds[1])
        for b in reads:
            b.r[id(ds)] = t
        for b in writes:
            b.w = t
            b.r = {}
        return ins

    def barrier(self):
        for e in self.eng:
            for k in self.eng:
                if k == e:
                    continue
                v = self.cnt[k]
                if v and self.seen[e].get(k, 0) < v:
                    self.eng[e].wait_ge(self.sem[k], v)
                    self.seen[e][k] = v
            for ds in self.dall:
                if ds[1] and self.seen[e].get(id(ds), 0) < ds[1]:
                    self.eng[e].wait_ge(ds[0], ds[1])
                    self.seen[e][id(ds)] = ds[1]

    def mm(self, bank_ap, bank_buf, lhsT, rhs, start, stop, reads):
        return self.op("pe", lambda e: e.matmul(bank_ap, lhsT, rhs, start=start, stop=stop),
                       reads=reads, writes=[bank_buf])

    def tr(self, out_ap, out_buf, in_ap, ident, reads):
        return self.op("pe", lambda e: e.transpose(out_ap, in_ap, ident), reads=reads, writes=[out_buf])


class Scope:
    def __init__(self, k):
        self.k = k
        self.es = ExitStack()
        self.bufs = []

    def __enter__(self):
        self.es.__enter__()
        return self

    def __exit__(self, *a):
        self.k.barrier()
        self.k.release(self.bufs)
        return self.es.__exit__(*a)

    def sb(self, name, shape, dt):
        self.k.nbuf += 1
        t = self.es.enter_context(self.k.nc.sbuf_tensor("%s_%d" % (name, self.k.nbuf), list(shape), dt))
        b = Buf(name)
        self.bufs.append(b)
        return t, b

    def buf(self, name=""):
        b = Buf(name)
        self.bufs.append(b)
        return b


def make_ident(k, sc):
    it, ib = sc.sb("iota_i", [128, 128], I32)
    k.op("pool", lambda e: e.iota(it[:], pattern=[[1, 128]], base=0, channel_multiplier=-1), writes=[ib])
    idb, idbb = sc.sb("ident_bf", [128, 128], BF16)
    idf, idfb = sc.sb("ident_f", [128, 128], F32)
    k.op("dve", lambda e: e.tensor_scalar(idb[:], it[:], 0, None, op0=ALU.is_equal), reads=[ib], writes=[idbb])
    k.op("dve", lambda e: e.tensor_scalar(idf[:], it[:], 0, None, op0=ALU.is_equal), reads=[ib], writes=[idfb])
    return (idb, idbb), (idf, idfb)


def rms_tile(k, sc, x_t, x_b, gain_t, gain_b, out_t, out_b, tmp):
    junk, junkb, ss, ssb = tmp["junk"], tmp["junkb"], tmp["ss"], tmp["ssb"]
    k.op("act", lambda e: e.activation(out=junk[:], in_=x_t, func=AF.Square, accum_out=ss[:, 0:1]),
         reads=[x_b], writes=[junkb, ssb])
    k.op("dve", lambda e: e.tensor_scalar(ss[:, 1:2], ss[:, 0:1], 1.0 / D, EPS, op0=ALU.mult, op1=ALU.add),
         reads=[ssb], writes=[ssb])
    k.op("act", lambda e: e.activation(out=ss[:, 2:3], in_=ss[:, 1:2], func=AF.Sqrt), reads=[ssb], writes=[ssb])
    k.op("dve", lambda e: e.reciprocal(ss[:, 3:4], ss[:, 2:3]), reads=[ssb], writes=[ssb])
    k.op("dve", lambda e: e.scalar_tensor_tensor(out=out_t, in0=x_t, scalar=ss[:, 3:4], in1=gain_t,
                                                 op0=ALU.mult, op1=ALU.mult),
         reads=[x_b, ssb, gain_b], writes=[out_b])


def rms_tmp(sc):
    junk, junkb = sc.sb("junk", [128, D], BF16)
    ss, ssb = sc.sb("ss", [128, 4], F32)
    return dict(junk=junk, junkb=junkb, ss=ss, ssb=ssb)


def small_ops(k, buf):
    def tt(out, a, b, op, extra_r=(), eng="dve"):
        k.op(eng, lambda e: e.tensor_tensor(out=out, in0=a, in1=b, op=op), reads=[buf, *extra_r], writes=[buf])

    def ts(out, a, s1, s2, op0, op1=None, extra_r=(), eng="dve"):
        if op1 is None:
            k.op(eng, lambda e: e.tensor_scalar(out, a, s1, None, op0=op0), reads=[buf, *extra_r], writes=[buf])
        else:
            k.op(eng, lambda e: e.tensor_scalar(out, a, s1, s2, op0=op0, op1=op1), reads=[buf, *extra_r], writes=[buf])

    def stt(out, a, sc, b, op0, op1, extra_r=()):
        k.op("dve", lambda e: e.scalar_tensor_tensor(out=out, in0=a, scalar=sc, in1=b, op0=op0, op1=op1),
             reads=[buf, *extra_r], writes=[buf])

    def act(out, a, func, scale=None, bias=None, extra_r=()):
        kw = {}
        if scale is not None:
            kw["scale"] = scale
        if bias is not None:
            kw["bias"] = bias
        k.op("act", lambda e: e.activation(out=out, in_=a, func=func, **kw), reads=[buf, *extra_r], writes=[buf])
    return tt, ts, stt, act


def phase_s5(k, A, x_in, h_out, gTd):
    with Scope(k) as sc0:
        uT, _ = sc0.sb("uT", [128, NQ, S], F32)
        ub = [[sc0.buf() for _ in range(4)] for _ in range(NQ)]
        (idb, idbb), _ = make_ident(k, sc0)
        with Scope(k) as sc:
            gain, gainb = sc.sb("gain", [128, D], F32)
            k.dma("sp", gain[:], A["l0_mix_norm"], gainb, writes=[gainb])
            win, winb = sc.sb("win", [128, NF, DS], BF16)
            wv = A["l0_s5_w_in"].rearrange("(fc p) n -> p fc n", p=128)
            for i in range(4):
                k.dma("pool", win[:, 4 * i:4 * i + 4, :], wv[:, 4 * i:4 * i + 4, :], winb, writes=[winb])
            tmp = rms_tmp(sc)
            xt = [sc.sb("xt", [128, D], F32) for _ in range(2)]
            hn = [sc.sb("hn", [128, D], BF16) for _ in range(2)]
            hnT = [sc.sb("hnT", [128, NF, 512], BF16) for _ in range(2)]
            for st in range(4):
                hT, hTb = hnT[st % 2]
                for tl in range(4):
                    tt_ = st * 4 + tl
                    x_t, x_b = xt[tt_ % 2]
                    h_t, h_b = hn[tt_ % 2]
                    k.dma("sp", x_t[:], x_in[tt_ * 128:(tt_ + 1) * 128, :], x_b, writes=[x_b])
                    rms_tile(k, sc, x_t[:], x_b, gain[:], gainb, h_t[:], h_b, tmp)
                    for half in range(2):
                        bk = (tt_ % 2) * 2 + half
                        pv = k.ps[bk][:].bitcast(BF16)
                        for j in range(8):
                            fc = half * 8 + j
                            k.tr(pv[:, j * 128:(j + 1) * 128], k.psb[bk], h_t[:, fc * 128:(fc + 1) * 128], idb[:],
                                 reads=[h_b, idbb])
                        src = pv.rearrange("p (a b) -> p a b", a=8)
                        dst = hT[:, half * 8:half * 8 + 8, tl * 128:(tl + 1) * 128]
                        if half == 0:
                            k.op("act", lambda e: e.activation(out=dst, in_=src, func=AF.Copy),
                                 reads=[k.psb[bk]], writes=[hTb])
                        else:
                            k.op("dve", lambda e: e.tensor_copy(out=dst, in_=src), reads=[k.psb[bk]], writes=[hTb])
                for q in range(NQ):
                    bk = 4 + (q % 4)
                    for fc in range(NF):
                        k.mm(k.ps[bk][:], k.psb[bk], win[:, fc, q * 128:(q + 1) * 128], hT[:, fc, :],
                             fc == 0, fc == NF - 1, reads=[winb, hTb])
                    k.op("act", lambda e: e.activation(out=uT[:, q, st * 512:(st + 1) * 512], in_=k.ps[bk][:], func=AF.Copy),
                         reads=[k.psb[bk]], writes=[ub[q][st]])
        with Scope(k) as sc1:
            with Scope(k) as sc:
                prm, prmb = sc.sb("prm", [128, 24, NPAIR], F32)
                lam, lamb = sc.sb("lam", [128, 3, NPAIR], F32)
                k.dma("sp", lam[:], A["s5_lam"], lamb, writes=[lamb])
                CD, _ = sc.sb("CD", [128, 11, NPAIR], F32)
                SD, _ = sc.sb("SD", [128, 11, NPAIR], F32)
                dsk, dskb = sc.sb("dsk", [128, NQ], F32)
                k.dma("sp", dsk[:], A["s5_d"], dskb, writes=[dskb])
                bwr, bwrb = sc.sb("bwr", [128, NQ, 4, 128], BF16)
                bwi, bwib = sc.sb("bwi", [128, NQ, 4, 128], BF16)
                k.dma("pool", bwr[:], A["s5_bw_re"], bwrb, writes=[bwrb])
                k.dma("pool", bwi[:], A["s5_bw_im"], bwib, writes=[bwib])
                cfr, cfrb = sc.sb("cfr", [128, NPAIR, 128], BF16)
                cfi, cfib = sc.sb("cfi", [128, NPAIR, 128], BF16)
                tt, ts, stt, act = small_ops(k, prmb)
                P = lambda i: prm[:, i, :]
                lr, li, lst = lam[:, 0, :], lam[:, 1, :], lam[:, 2, :]
                act(P(0), lst, AF.Exp, extra_r=[lamb])
                tt(P(1), lr, P(0), ALU.mult, extra_r=[lamb])
                tt(P(2), li, P(0), ALU.mult, extra_r=[lamb])
                act(P(3), P(1), AF.Exp)
                ts(P(4), P(2), 1.0 / 64, None, ALU.mult)
                tt(P(5), P(4), P(4), ALU.mult)
                ts(P(6), P(5), -1.0 / 42, 1.0, ALU.mult, ALU.add)
                tt(P(6), P(6), P(5), ALU.mult)
                ts(P(6), P(6), -1.0 / 20, 1.0, ALU.mult, ALU.add)
                tt(P(6), P(6), P(5), ALU.mult)
                ts(P(6), P(6), -1.0 / 6, 1.0, ALU.mult, ALU.add)
                tt(P(7), P(6), P(4), ALU.mult)
                ts(P(6), P(5), -1.0 / 56, 1.0, ALU.mult, ALU.add)
                tt(P(6), P(6), P(5), ALU.mult)
                ts(P(6), P(6), -1.0 / 30, 1.0, ALU.mult, ALU.add)
                tt(P(6), P(6), P(5), ALU.mult)
                ts(P(6), P(6), -1.0 / 12, 1.0, ALU.mult, ALU.add)
                tt(P(6), P(6), P(5), ALU.mult)
                ts(P(8), P(6), -0.5, 1.0, ALU.mult, ALU.add)
                ci, si = 8, 7
                free = [9, 10]
                for _ in range(6):
                    cn, sn = free
                    tt(P(11), P(ci), P(ci), ALU.mult)
                    tt(P(12), P(si), P(si), ALU.mult)
                    tt(P(cn), P(11), P(12), ALU.subtract)
                    stt(P(sn), P(ci), 2.0, P(si), ALU.mult, ALU.mult)
                    free = [ci, si]
                    ci, si = cn, sn
                k.op("dve", lambda e: e.tensor_copy(out=CD[:, 0, :], in_=P(ci)), reads=[prmb], writes=[prmb])
                k.op("dve", lambda e: e.tensor_copy(out=SD[:, 0, :], in_=P(si)), reads=[prmb], writes=[prmb])
                for kk in range(10):
                    tt(P(11), CD[:, kk, :], CD[:, kk, :], ALU.mult)
                    tt(P(12), SD[:, kk, :], SD[:, kk, :], ALU.mult)
                    tt(CD[:, kk + 1, :], P(11), P(12), ALU.subtract)
                    stt(SD[:, kk + 1, :], CD[:, kk, :], 2.0, SD[:, kk, :], ALU.mult, ALU.mult)
                NSD, _ = sc.sb("NSD", [128, 11, NPAIR], F32)
                ts(NSD[:], SD[:], -1.0, None, ALU.mult)
                tt(P(13), P(3), P(ci), ALU.mult)
                tt(P(14), P(3), P(si), ALU.mult)
                ts(P(15), P(13), -1.0, None, ALU.add)
                tt(P(16), lr, lr, ALU.mult, extra_r=[lamb])
                tt(P(17), li, li, ALU.mult, extra_r=[lamb])
                tt(P(16), P(16), P(17), ALU.add)
                k.op("dve", lambda e: e.reciprocal(P(17), P(16)), reads=[prmb], writes=[prmb])
                tt(P(18), P(15), lr, ALU.mult, extra_r=[lamb])
                tt(P(19), P(14), li, ALU.mult, extra_r=[lamb])
                tt(P(18), P(18), P(19), ALU.add)
                tt(P(20), P(18), P(17), ALU.mult)
                tt(P(18), P(14), lr, ALU.mult, extra_r=[lamb])
                tt(P(19), P(15), li, ALU.mult, extra_r=[lamb])
                tt(P(18), P(18), P(19), ALU.subtract)
                tt(P(21), P(18), P(17), ALU.mult)
                ts(P(22), P(21), -1.0, None, ALU.mult)
                with Scope(k) as scc:
                    cwr, cwrb = scc.sb("cwr", [128, NPAIR, 128], F32)
                    cwi, cwib = scc.sb("cwi", [128, NPAIR, 128], F32)
                    ctmp, ctmpb = scc.sb("ctmp", [128, 128], F32)
                    k.dma("sp", cwr[:], A["s5_cw_re"], cwrb, writes=[cwrb])
                    k.dma("sp", cwi[:], A["s5_cw_im"], cwib, writes=[cwib])
                    for j in range(NPAIR):
                        k.op("dve", lambda e: e.tensor_scalar(ctmp[:], cwi[:, j, :], prm[:, 22, j:j + 1], None, op0=ALU.mult),
                             reads=[cwib, prmb], writes=[ctmpb])
                        k.op("dve", lambda e: e.scalar_tensor_tensor(out=cfr[:, j, :], in0=cwr[:, j, :], scalar=prm[:, 20, j:j + 1],
                                                                     in1=ctmp[:], op0=ALU.mult, op1=ALU.add),
                             reads=[cwrb, prmb, ctmpb], writes=[cfrb])
                        k.op("dve", lambda e: e.tensor_scalar(ctmp[:], cwi[:, j, :], prm[:, 20, j:j + 1], -1.0, op0=ALU.mult, op1=ALU.mult),
                             reads=[cwib, prmb], writes=[ctmpb])
                        k.op("dve", lambda e: e.scalar_tensor_tensor(out=cfi[:, j, :], in0=cwr[:, j, :], scalar=prm[:, 22, j:j + 1],
                                                                     in1=ctmp[:], op0=ALU.mult, op1=ALU.add),
                             reads=[cwrb, prmb, ctmpb], writes=[cfib])
                COSb = [sc.sb("COS", [128, S], F32) for _ in range(2)]
                SINb = [sc.sb("SIN", [128, S], F32) for _ in range(2)]
                TA, TAb = sc.sb("TA", [128, 4, 512], F32)
                WK, wkb = sc.sb("WK", [128, 6, 512], F32)
                wre, wreb = sc.sb("wre", [128, 2, 512], F32)
                wim, wimb = sc.sb("wim", [128, 2, 512], F32)
                BR, BRb = sc.sb("BR", [128, 4, 512], BF16)
                xb, _ = sc.sb("xb", [128, 2, 2, 512], BF16)
                xbb = [sc.buf() for _ in range(2)]
                uBq, uBqb = sc.sb("uBq", [128, S], BF16)
                ysb, ysbb = sc.sb("ysb", [128, 512], F32)
                gw = [sc.sb("gw%d" % i, [128, 512], F32) for i in range(3)]
                gst = [sc.sb("gst", [128, 512], BF16) for _ in range(2)]
                cq = [[sc.buf() for _ in range(3)] for _ in range(2)]
                sq = [[sc.buf() for _ in range(3)] for _ in range(2)]
                QMAP = [0, 1, 2, 2]
                for i in range(2):
                    k.op("dve", lambda e: e.memset(COSb[i][0][:, 0:1], 1.0), writes=[cq[i][0]])
                    k.op("dve", lambda e: e.memset(SINb[i][0][:, 0:1], 0.0), writes=[sq[i][0]])

                def table_steps(j, kks):
                    C = COSb[j % 2][0]
                    S_ = SINb[j % 2][0]
                    for kk in kks:
                        d = 1 << kk
                        cd, sd = CD[:, kk, j:j + 1], SD[:, kk, j:j + 1]
                        for lo in range(0, d, 512):
                            n = min(512, d - lo)
                            hi = lo + n
                            rq = lo // 512
                            wq = 0 if kk <= 8 else (1 if kk == 9 else 2)
                            Cr, Sr = cq[j % 2][rq], sq[j % 2][rq]
                            Cw, Sw = cq[j % 2][wq], sq[j % 2][wq]
                            k.op("act", lambda e: e.activation(out=TA[:, 0, 0:n], in_=C[:, lo:hi], func=AF.Copy, scale=cd), reads=[Cr, prmb], writes=[TAb])
                            k.op("act", lambda e: e.activation(out=TA[:, 1, 0:n], in_=S_[:, lo:hi], func=AF.Copy, scale=sd), reads=[Sr, prmb], writes=[TAb])
                            k.op("act", lambda e: e.activation(out=TA[:, 2, 0:n], in_=S_[:, lo:hi], func=AF.Copy, scale=cd), reads=[Sr, prmb], writes=[TAb])
                            k.op("act", lambda e: e.activation(out=TA[:, 3, 0:n], in_=C[:, lo:hi], func=AF.Copy, scale=sd), reads=[Cr, prmb], writes=[TAb])
                            k.op("pool", lambda e: e.tensor_tensor(out=C[:, d + lo:d + hi], in0=TA[:, 0, 0:n], in1=TA[:, 1, 0:n], op=ALU.subtract), reads=[TAb], writes=[Cw])
                            k.op("pool", lambda e: e.tensor_tensor(out=S_[:, d + lo:d + hi], in0=TA[:, 2, 0:n], in1=TA[:, 3, 0:n], op=ALU.add), reads=[TAb], writes=[Sw])

                table_steps(0, range(11))
                sched = [list(range(0, 7)), [7, 8], [9], [10]]
                BR2, _ = sc.sb("BR2", [128, 4, 512], BF16)
                BRs = [(BR, BRb), (BR2, sc.buf())]

                def stage_A(idx, j, nt):
                    q, pp = j // 4, j % 4
                    B_, Bb_ = BRs[idx % 2]
                    xi = idx % 2
                    k.op("dve", lambda e: e.tensor_tensor(out=xb[:, xi, 0, :], in0=B_[:, 0, :], in1=B_[:, 1, :], op=ALU.subtract), reads=[Bb_], writes=[xbb[xi]])
                    k.op("dve", lambda e: e.tensor_tensor(out=xb[:, xi, 1, :], in0=B_[:, 2, :], in1=B_[:, 3, :], op=ALU.add), reads=[Bb_], writes=[xbb[xi]])
                    bk = 2 + nt
                    k.mm(k.ps[bk][:], k.psb[bk], cfr[:, j, :], xb[:, xi, 0, :], pp == 0, False, reads=[cfrb, xbb[xi]])
                    k.mm(k.ps[bk][:], k.psb[bk], cfi[:, j, :], xb[:, xi, 1, :], False, pp == 3, reads=[cfib, xbb[xi]])
                    if pp == 3 and nt == 3:
                        for n2 in range(4):
                            sl2 = slice(n2 * 512, (n2 + 1) * 512)
                            bk2 = 2 + n2
                            k.op("dve", lambda e: e.scalar_tensor_tensor(out=ysb[:], in0=uT[:, q, sl2], scalar=dsk[:, q:q + 1], in1=k.ps[bk2][:],
                                                                         op0=ALU.mult, op1=ALU.add),
                                 reads=[ub[q][n2], dskb, k.psb[bk2]], writes=[ysbb])
                            (g1, g1b), (g2, g2b), (g3, g3b) = gw
                            go, gob = gst[n2 % 2]
                            k.op("act", lambda e: e.activation(out=g1[:], in_=ysb[:], func=AF.Square), reads=[ysbb], writes=[g1b])
                            k.op("pool", lambda e: e.tensor_scalar(g2[:], g1[:], 0.044715, 1.0, op0=ALU.mult, op1=ALU.add), reads=[g1b], writes=[g2b])
                            k.op("pool", lambda e: e.tensor_tensor(out=g3[:], in0=g2[:], in1=ysb[:], op=ALU.mult), reads=[g2b, ysbb], writes=[g3b])
                            k.op("act", lambda e: e.activation(out=g1[:], in_=g3[:], func=AF.Sigmoid, scale=1.5957691216057308), reads=[g3b], writes=[g1b])
                            k.op("pool", lambda e: e.tensor_tensor(out=go[:], in0=g1[:], in1=ysb[:], op=ALU.mult), reads=[g1b, ysbb], writes=[gob])
                            k.dma("sp", gTd[q, :, sl2], go[:], gob, reads=[gob])

                items = [(j, nt) for j in range(NPAIR) for nt in range(4)]
                for idx, (j, nt) in enumerate(items):
                    q, pp = j // 4, j % 4
                    COS = COSb[j % 2][0]
                    SIN = SINb[j % 2][0]
                    cosb = cq[j % 2][QMAP[nt]]
                    sinb = sq[j % 2][QMAP[nt]]
                    rcol = prm[:, 3, j:j + 1]
                    if pp == 0 and nt == 0:
                        k.op("act", lambda e: e.activation(out=uBq[:], in_=uT[:, q, :], func=AF.Copy), reads=ub[q], writes=[uBqb])
                    sl = slice(nt * 512, (nt + 1) * 512)
                    rows = slice(64 * (pp // 2), 64 * (pp // 2) + 64)
                    b0, b1 = (0, 1) if nt % 2 == 0 else (6, 7)
                    k.mm(k.ps[b0][:], k.psb[b0], bwr[rows, q, pp, :], uBq[rows, sl], True, True, reads=[bwrb, uBqb])
                    k.mm(k.ps[b1][:], k.psb[b1], bwi[rows, q, pp, :], uBq[rows, sl], True, True, reads=[bwib, uBqb])
                    t1, t2, t3, t4, pr, pi = [WK[:, i, :] for i in range(6)]
                    C_, S_ = COS[:, sl], SIN[:, sl]
                    k.op("dve", lambda e: e.tensor_tensor(out=t1, in0=k.ps[b0][:], in1=C_, op=ALU.mult), reads=[k.psb[b0], cosb], writes=[wkb])
                    k.op("dve", lambda e: e.tensor_tensor(out=t2, in0=k.ps[b1][:], in1=S_, op=ALU.mult), reads=[k.psb[b1], sinb], writes=[wkb])
                    k.op("dve", lambda e: e.tensor_tensor(out=t3, in0=k.ps[b1][:], in1=C_, op=ALU.mult), reads=[k.psb[b1], cosb], writes=[wkb])
                    k.op("dve", lambda e: e.tensor_tensor(out=t4, in0=k.ps[b0][:], in1=S_, op=ALU.mult), reads=[k.psb[b0], sinb], writes=[wkb])
                    k.op("dve", lambda e: e.tensor_tensor(out=pr, in0=t1, in1=t2, op=ALU.add), reads=[wkb], writes=[wkb])
                    k.op("dve", lambda e: e.tensor_tensor(out=pi, in0=t3, in1=t4, op=ALU.subtract), reads=[wkb], writes=[wkb])
                    ini_r = 0.0 if nt == 0 else wre[:, (nt - 1) % 2, 511:512]
                    ini_i = 0.0 if nt == 0 else wim[:, (nt - 1) % 2, 511:512]
                    rb = rcol.to_broadcast([128, 512])
                    k.op("dve", lambda e: e.tensor_tensor_scan(out=wre[:, nt % 2, :], data0=rb, data1=pr, initial=ini_r, op0=ALU.mult, op1=ALU.add),
                         reads=[wkb, prmb, wreb], writes=[wreb])
                    k.op("dve", lambda e: e.tensor_tensor_scan(out=wim[:, nt % 2, :], data0=rb, data1=pi, initial=ini_i, op0=ALU.mult, op1=ALU.add),
                         reads=[wkb, prmb, wimb], writes=[wimb])
                    B_, Bb_ = BRs[idx % 2]
                    k.op("pool", lambda e: e.tensor_tensor(out=B_[:, 0, :], in0=wre[:, nt % 2, :], in1=C_, op=ALU.mult), reads=[wreb, cosb], writes=[Bb_])
                    k.op("pool", lambda e: e.tensor_tensor(out=B_[:, 1, :], in0=wim[:, nt % 2, :], in1=S_, op=ALU.mult), reads=[wimb, sinb], writes=[Bb_])
                    k.op("pool", lambda e: e.tensor_tensor(out=B_[:, 2, :], in0=wre[:, nt % 2, :], in1=S_, op=ALU.mult), reads=[wreb, sinb], writes=[Bb_])
                    k.op("pool", lambda e: e.tensor_tensor(out=B_[:, 3, :], in0=wim[:, nt % 2, :], in1=C_, op=ALU.mult), reads=[wimb, cosb], writes=[Bb_])
                    if j + 1 < NPAIR:
                        table_steps(j + 1, sched[nt])
                    if idx > 0:
                        stage_A(idx - 1, *items[idx - 1])
                stage_A(len(items) - 1, *items[-1])
            with Scope(k) as sc2:
                oT, _ = sc2.sb("oT", [128, NQ, S], BF16)
                ob = [[sc2.buf() for _ in range(4)] for _ in range(NQ)]
                gT, gTb = sc2.sb("gT", [128, NQ, S], BF16)
                for q_ in range(NQ):
                    k.dma("sp", gT[:, q_, :], gTd[q_], gTb, writes=[gTb])
                with Scope(k) as sc:
                    wg, wgb = sc.sb("wglu", [128, NQ, DS], BF16)
                    wv = A["l0_s5_w_glu"].rearrange("(c p) n -> p c n", p=128)
                    for i in range(2):
                        k.dma("pool", wg[:, 4 * i:4 * i + 4, :], wv[:, 4 * i:4 * i + 4, :], wgb, writes=[wgb])
                    bg, bgb = sc.sb("bglu", [128, NQ], F32)
                    k.dma("sp", bg[:], A["s5_b_glu"], bgb, writes=[bgb])
                    sg = [sc.sb("sg%d" % i, [128, 512], F32) for i in range(2)]
                    it = 0
                    for q in range(NQ):
                        for nt in range(4):
                            sl = slice(nt * 512, (nt + 1) * 512)
                            bk = it % 4
                            s_t, s_b = sg[it % 2]
                            it += 1
                            for c in range(NQ):
                                k.mm(k.ps[bk][:], k.psb[bk], wg[:, c, q * 128:(q + 1) * 128], gT[:, c, sl], c == 0, c == NQ - 1,
                                     reads=[wgb, gTb])
                            k.op("act", lambda e: e.activation(out=s_t[:], in_=k.ps[bk][:], func=AF.Sigmoid, bias=bg[:, q:q + 1]),
                                 reads=[k.psb[bk], bgb], writes=[s_b])
                            k.op("dve", lambda e: e.tensor_tensor(out=oT[:, q, sl], in0=s_t[:], in1=gT[:, q, sl], op=ALU.mult),
                                 reads=[s_b, gTb], writes=[ob[q][nt]])
                with Scope(k) as sc:
                    wo, wob = sc.sb("wout", [128, NQ, D], BF16)
                    wv = A["l0_s5_w_out"].rearrange("(c p) n -> p c n", p=128)
                    for i in range(4):
                        k.dma("pool", wo[:, 2 * i:2 * i + 2, :], wv[:, 2 * i:2 * i + 2, :], wob, writes=[wob])
                    xt = [sc.sb("xt", [128, D], F32) for _ in range(2)]
                    ot = [sc.sb("ot", [128, D], F32) for _ in range(2)]
                    for tt_ in range(NT):
                        x_t, x_b = xt[tt_ % 2]
                        o_t, o_b = ot[tt_ % 2]
                        k.dma("sp", x_t[:], x_in[tt_ * 128:(tt_ + 1) * 128, :], x_b, writes=[x_b])
                        nt = tt_ // 4
                        for n4 in range(4):
                            bk = (tt_ % 2) * 4 + n4
                            for c in range(NQ):
                                k.mm(k.ps[bk][:], k.psb[bk], oT[:, c, tt_ * 128:(tt_ + 1) * 128], wo[:, c, n4 * 512:(n4 + 1) * 512],
                                     c == 0, c == NQ - 1, reads=[ob[c][nt], wob])
                            k.op("dve", lambda e: e.tensor_tensor(out=o_t[:, n4 * 512:(n4 + 1) * 512], in0=k.ps[bk][:],
                                                                  in1=x_t[:, n4 * 512:(n4 + 1) * 512], op=ALU.add),
                                 reads=[k.psb[bk], x_b], writes=[o_b])
                        k.dma("sp", h_out[tt_ * 128:(tt_ + 1) * 128, :], o_t[:], o_b, reads=[o_b])


def prep_common(inp):
    f = np.float32
    out = {}
    for nm in ["l0_mix_norm", "l0_ffn_norm", "l1_mix_norm", "l1_ffn_norm", "final_norm"]:
        out[nm] = np.ascontiguousarray(np.broadcast_to(np.asarray(inp[nm], f)[None, :], (128, D)))
    lam_re = np.asarray(inp["l0_s5_lambda_re"], f)
    lam_im = np.asarray(inp["l0_s5_lambda_im"], f)
    lst = np.repeat(np.asarray(inp["l0_s5_log_step"], f), 64).reshape(64, 64)
    lam = np.stack([a.reshape(NPAIR, 128).T for a in (lam_re, lam_im, lst)], axis=1)
    out["s5_lam"] = np.ascontiguousarray(lam)
    b_re = np.asarray(inp["l0_s5_b_re"], f)
    b_im = np.asarray(inp["l0_s5_b_im"], f)
    c_re = np.asarray(inp["l0_s5_c_re"], f)
    c_im = np.asarray(inp["l0_s5_c_im"], f)
    bwr = np.zeros((128, NQ, 4, 128), f)
    bwi = np.zeros((128, NQ, 4, 128), f)
    cwr = np.zeros((128, NPAIR, 128), f)
    cwi = np.zeros((128, NPAIR, 128), f)
    for g in range(64):
        q, r0, gl, j = g // 8, (g % 8) * 16, g % 2, g // 2
        bwr[r0:r0 + 16, q, (g % 8) // 2, 64 * gl:64 * gl + 64] = b_re[g].T
        bwi[r0:r0 + 16, q, (g % 8) // 2, 64 * gl:64 * gl + 64] = b_im[g].T
        cwr[64 * gl:64 * gl + 64, j, r0:r0 + 16] = c_re[g].T
        cwi[64 * gl:64 * gl + 64, j, r0:r0 + 16] = c_im[g].T
    out["s5_bw_re"], out["s5_bw_im"], out["s5_cw_re"], out["s5_cw_im"] = bwr, bwi, cwr, cwi
    out["s5_d"] = np.ascontiguousarray(np.asarray(inp["l0_s5_d"], f).reshape(NQ, 128).T)
    out["s5_b_glu"] = np.ascontiguousarray(np.asarray(inp["l0_s5_b_glu"], f).reshape(NQ, 128).T)
    for nm in ["l0_s5_w_in", "l0_s5_w_glu", "l0_s5_w_out"]:
        out[nm] = np.asarray(inp[nm], f)
    out["l1_fox_w_in"] = np.asarray(inp["l1_fox_w_in"], f)
    out["l1_fox_w_out"] = np.asarray(inp["l1_fox_w_out"], f)
    out["l1_fox_b_forget"] = np.ascontiguousarray(np.asarray(inp["l1_fox_b_forget"], f).reshape(NH, 1))
    for L in ("l0", "l1"):
        out[L + "_moe_wr"] = np.ascontiguousarray(np.concatenate([np.asarray(inp[L + "_moe_w_group"], f), np.asarray(inp[L + "_moe_w_expert"], f)], axis=1))
        brow = np.concatenate([np.asarray(inp[L + "_moe_b_group"], f), np.asarray(inp[L + "_moe_b_expert"], f)])
        out[L + "_moe_br"] = np.ascontiguousarray(np.broadcast_to(brow[None, :], (128, 72)))
        for nm in ["_moe_w_gate", "_moe_w_up", "_moe_w_down"]:
            out[L + nm] = np.asarray(inp[L + nm], f)
    return out


def declare_inputs(k, arrays):
    A = {}
    for nm, a in arrays.items():
        A[nm] = k.nc.dram_tensor(nm, list(a.shape), F32, kind="ExternalInput").ap()
    return A


def phase_moe(k, A, L, h_in, h_out, yg, final_gain=None):
    with Scope(k) as sc0:
        cwb, cwbb = sc0.sb("cwb", [128, NT, NE], BF16)
        ohb, ohbb = sc0.sb("ohb", [128, NT, NG], BF16)
        ohf, ohfb = sc0.sb("ohf", [128, NT, NG], F32)
        rs, rsb = sc0.sb("rs", [128, NT, NG], F32)
        rankT, rankTb = sc0.sb("rankT", [NG, S], F32)
        (idb, idbb), (idf, idfb) = make_ident(k, sc0)
        iota_c, iocb = sc0.sb("iota_c", [128, CAP], F32)
        slot_idx, slib = sc0.sb("slot_idx", [128, NST], F32)
        tri, trib = sc0.sb("tri", [128, 128], BF16)
        ones, onesb = sc0.sb("ones", [128, 128], BF16)
        Eg, Egb = sc0.sb("Eg", [NG, NG, 128], F32)
        with Scope(k) as sct:
            ii, iib = sct.sb("ii", [128, 512], I32)
            k.op("pool", lambda e: e.iota(ii[:, 0:CAP], pattern=[[1, CAP]], base=0, channel_multiplier=0), writes=[iib])
            k.op("dve", lambda e: e.tensor_copy(out=iota_c[:], in_=ii[:, 0:CAP]), reads=[iib], writes=[iocb])
            k.op("pool", lambda e: e.iota(ii[:, 0:NST], pattern=[[128, NST]], base=0, channel_multiplier=1), reads=[iocb], writes=[iib])
            k.op("dve", lambda e: e.tensor_copy(out=slot_idx[:], in_=ii[:, 0:NST]), reads=[iib], writes=[slib])
            k.op("pool", lambda e: e.iota(ii[:, 0:128], pattern=[[1, 128]], base=0, channel_multiplier=-1), reads=[slib], writes=[iib])
            k.op("dve", lambda e: e.tensor_scalar(tri[:], ii[:, 0:128], 0, None, op0=ALU.is_gt), reads=[iib], writes=[trib])
            k.op("dve", lambda e: e.memset(ones[:], 1.0), writes=[onesb])
            i8, i8b = sct.sb("i8", [NG, NG, 128], I32)
            k.op("pool", lambda e: e.iota(i8[:], pattern=[[1, NG], [0, 128]], base=0, channel_multiplier=-1), writes=[i8b])
            k.op("dve", lambda e: e.tensor_scalar(Eg[:], i8[:], 0, None, op0=ALU.is_equal), reads=[i8b], writes=[Egb])
        scH = Scope(k)
        scH.__enter__()
        hnb, _ = scH.sb("hnb", [128, NT, D], BF16)
        hnbb = [scH.buf() for _ in range(NT)]
        if True:
            with Scope(k) as sc:
                gain, gainb = sc.sb("gain", [128, D], F32)
                k.dma("sp", gain[:], A[L + "_ffn_norm"], gainb, writes=[gainb])
                wr, wrb = sc.sb("wr", [128, NF, 72], F32)
                k.dma("sp", wr[:], A[L + "_moe_wr"].rearrange("(fc p) n -> p fc n", p=128), wrb, writes=[wrb])
                br, brb = sc.sb("br", [128, 72], F32)
                k.dma("sp", br[:], A[L + "_moe_br"], brb, writes=[brb])
                tmp = rms_tmp(sc)
                xt = [sc.sb("xt", [128, D], F32) for _ in range(2)]
                hnf = [sc.sb("hnf", [128, D], F32) for _ in range(2)]
                hnT = [sc.sb("hnTf", [128, NF, 128], F32) for _ in range(2)]
                rt, rtb = sc.sb("rt", [128, 320], F32)
                tt_, ts_, stt_, act_ = small_ops(k, rtb)
                def stage1(t):
                    x_t, x_b = xt[t % 2]
                    f_t, f_b = hnf[t % 2]
                    T_t, T_b = hnT[t % 2]
                    k.dma("sp", x_t[:], h_in[t * 128:(t + 1) * 128, :], x_b, writes=[x_b])
                    rms_tile(k, sc, x_t[:], x_b, gain[:], gainb, f_t[:], f_b, tmp)
                    k.op("act", lambda e: e.activation(out=hnb[:, t, :], in_=f_t[:], func=AF.Copy), reads=[f_b], writes=[hnbb[t]])
                    for qd in range(4):
                        bk = qd
                        for j in range(4):
                            fc = qd * 4 + j
                            k.tr(k.ps[bk][:, j * 128:(j + 1) * 128], k.psb[bk], f_t[:, fc * 128:(fc + 1) * 128], idf[:], reads=[f_b, idfb])
                        src = k.ps[bk][:].rearrange("p (a b) -> p a b", a=4)
                        dst = T_t[:, qd * 4:qd * 4 + 4, :]
                        if qd % 2 == 0:
                            k.op("act", lambda e: e.activation(out=dst, in_=src, func=AF.Copy), reads=[k.psb[bk]], writes=[T_b])
                        else:
                            k.op("dve", lambda e: e.tensor_copy(out=dst, in_=src), reads=[k.psb[bk]], writes=[T_b])
                    bk = 4 + (t % 2)
                    for fc in range(NF):
                        k.mm(k.ps[bk][:, 0:72], k.psb[bk], T_t[:, fc, :], wr[:, fc, :], fc == 0, fc == NF - 1, reads=[T_b, wrb])

                def stage2(t):
                    bk = 4 + (t % 2)
                    R = lambda a, b: rt[:, a:b]
                    lg, m8, ngm, eg, sg, pg = R(0, 72), R(72, 80), R(80, 81), R(81, 89), R(89, 90), R(90, 91)
                    pen, me, t8 = R(91, 99), R(99, 163), R(163, 171)
                    dl, ex, den, w1, w2, c1, c2 = R(171, 172), R(172, 173), R(173, 174), R(174, 175), R(175, 176), R(176, 240), R(240, 304)
                    tt_(lg, k.ps[bk][:, 0:72], br[:], ALU.add, extra_r=[k.psb[bk], brb])
                    k.op("dve", lambda e: e.max(out=m8, in_=rt[:, 0:8]), reads=[rtb], writes=[rtb])
                    oh_t = ohf[:, t, :]
                    k.op("dve", lambda e: e.tensor_scalar(oh_t, rt[:, 0:8], rt[:, 72:73], None, op0=ALU.is_equal), reads=[rtb], writes=[ohfb])
                    k.op("dve", lambda e: e.tensor_copy(out=ohb[:, t, :], in_=oh_t), reads=[ohfb], writes=[ohbb])
                    ts_(ngm, rt[:, 72:73], -1.0, None, ALU.mult)
                    k.op("act", lambda e: e.activation(out=eg, in_=rt[:, 0:8], func=AF.Exp, bias=rt[:, 80:81], scale=1.0, accum_out=sg),
                         reads=[rtb], writes=[rtb])
                    k.op("dve", lambda e: e.reciprocal(pg, sg), reads=[rtb], writes=[rtb])
                    ts_(pen, oh_t, -1.0, 1e30, ALU.add, ALU.mult, extra_r=[ohfb])
                    me3 = me.rearrange("p (a b) -> p a b", a=8)
                    el3 = rt[:, 8:72].rearrange("p (a b) -> p a b", a=8)
                    pen3 = pen.unsqueeze(2).to_broadcast([128, 8, 8])
                    tt_(me3, el3, pen3, ALU.add)
                    k.op("dve", lambda e: e.max(out=t8, in_=me), reads=[rtb], writes=[rtb])
                    tt_(dl, rt[:, 164:165], rt[:, 163:164], ALU.subtract)
                    act_(ex, dl, AF.Exp)
                    ts_(den, ex, 1.0, None, ALU.add)
                    k.op("dve", lambda e: e.reciprocal(w1, den), reads=[rtb], writes=[rtb])
                    tt_(w2, ex, w1, ALU.mult)
                    tt_(w1, w1, pg, ALU.mult)
                    tt_(w2, w2, pg, ALU.mult)
                    ts_(c1, me, rt[:, 163:164], rt[:, 174:175], ALU.is_equal, ALU.mult)
                    ts_(c2, me, rt[:, 164:165], rt[:, 175:176], ALU.is_equal, ALU.mult)
                    k.op("dve", lambda e: e.tensor_tensor(out=cwb[:, t, :], in0=c1, in1=c2, op=ALU.add), reads=[rtb], writes=[cwbb])

                stage1(0)
                for t in range(NT):
                    if t + 1 < NT:
                        stage1(t + 1)
                    stage2(t)
                for t in range(NT):
                    bk = 6 + (t % 2)
                    k.mm(k.ps[bk][:, 0:NG], k.psb[bk], tri[:], ohb[:, t, :], True, t == 0, reads=[trib, ohbb])
                    for t2 in range(t):
                        k.mm(k.ps[bk][:, 0:NG], k.psb[bk], ones[:], ohb[:, t2, :], False, t2 == t - 1, reads=[onesb, ohbb])
                    k.op("dve", lambda e: e.scalar_tensor_tensor(out=rs[:, t, :], in0=k.ps[bk][:, 0:NG], scalar=1.0, in1=ohf[:, t, :],
                                                                 op0=ALU.add, op1=ALU.mult), reads=[k.psb[bk], ohfb], writes=[rsb])
                    k.op("dve", lambda e: e.tensor_scalar(rs[:, t, :], rs[:, t, :], -1.0, None, op0=ALU.add), reads=[rsb], writes=[rsb])
                for t in range(NT):
                    bk = t % 2
                    k.tr(k.ps[bk][0:NG, 0:128], k.psb[bk], rs[:, t, :], idf[:], reads=[rsb, idfb])
                    k.op("dve", lambda e: e.tensor_copy(out=rankT[:, t * 128:(t + 1) * 128], in_=k.ps[bk][0:NG, 0:128]), reads=[k.psb[bk]], writes=[rankTb])
            with Scope(k) as sc:
                NU = 8
                ring, _ = sc.sb("ring", [128, NU, 4096], BF16)
                ringb = [sc.buf() for _ in range(NU)]
                sel, selb = sc.sb("sel", [128, NT, CAP], BF16)
                xg, xgb = sc.sb("xg", [128, NF, CAP], BF16)
                cwg, cwgb = sc.sb("cwg", [128, NST, EPG], F32)
                yacc, _ = sc.sb("yacc", [128, NST, D], F32)
                yaccb = [sc.buf() for _ in range(NST)]
                hid = [sc.sb("hid", [128, 4, CAP], BF16) for _ in range(2)]
                sgt = [sc.sb("sgt", [128, CAP], F32) for _ in range(2)]
                wg_v = A[L + "_moe_w_gate"]
                wu_v = A[L + "_moe_w_up"]
                wd_v = A[L + "_moe_w_down"]

                def unit_src(u):
                    e, j = u // 6, u % 6
                    if j < 4:
                        w = wg_v if j < 2 else wu_v
                        h = j % 2
                        return w[e].rearrange("(fc p) n -> p fc n", p=128)[:, h * 8:h * 8 + 8, :], "p (a b) -> p a b", 8
                    h = j - 4
                    return wd_v[e].rearrange("(c p) n -> p c n", p=128)[:, h * 2:h * 2 + 2, :], "p (a b) -> p a b", 2

                def issue_unit(u):
                    if u >= NE * 6:
                        return
                    src, pat, a = unit_src(u)
                    slot = u % NU
                    k.dma("pool", ring[:, slot, :].rearrange(pat, a=a), src, ringb[slot], writes=[ringb[slot]])

                for u in range(NU):
                    issue_unit(u)
                cnt_ev = 0
                for g in range(NG):
                    for t in range(NT):
                        eng = "dve" if t % 2 == 0 else "pool"
                        k.op(eng, lambda e: e.tensor_scalar(sel[:, t, :], iota_c[:], rs[:, t, g:g + 1], None, op0=ALU.is_equal),
                             reads=[iocb, rsb], writes=[selb])
                    for fc in range(NF):
                        bk = fc % 2
                        for t in range(NT):
                            k.mm(k.ps[bk][:, 0:CAP], k.psb[bk], hnb[:, t, fc * 128:(fc + 1) * 128], sel[:, t, :], t == 0, t == NT - 1,
                                 reads=[hnbb[t], selb])
                        if fc % 2 == 0:
                            k.op("act", lambda e: e.activation(out=xg[:, fc, :], in_=k.ps[bk][:, 0:CAP], func=AF.Copy), reads=[k.psb[bk]], writes=[xgb])
                        else:
                            k.op("dve", lambda e: e.tensor_copy(out=xg[:, fc, :], in_=k.ps[bk][:, 0:CAP]), reads=[k.psb[bk]], writes=[xgb])
                    for st in range(NST):
                        bk = st % 2
                        for t in range(NT):
                            k.mm(k.ps[bk][:, 0:EPG], k.psb[bk], sel[:, t, st * 128:(st + 1) * 128], cwb[:, t, g * EPG:(g + 1) * EPG],
                                 t == 0, t == NT - 1, reads=[selb, cwbb])
                        k.op("dve", lambda e: e.tensor_copy(out=cwg[:, st, :], in_=k.ps[bk][:, 0:EPG]), reads=[k.psb[bk]], writes=[cwgb])
                    for el in range(EPG):
                        e_id = g * EPG + el
                        u0 = e_id * 6
                        h_t, h_b = hid[e_id % 2]
                        for hc in range(4):
                            s_t, s_b = sgt[hc % 2]
                            bg, bu = 2 + (hc % 2), 4 + (hc % 2)
                            for fc in range(NF):
                                slot = (u0 + fc // 8) % NU
                                wv = ring[:, slot, :].rearrange("p (a b) -> p a b", a=8)
                                k.mm(k.ps[bg][:, 0:CAP], k.psb[bg], wv[:, fc % 8, hc * 128:(hc + 1) * 128], xg[:, fc, :], fc == 0, fc == NF - 1,
                                     reads=[ringb[slot], xgb])
                            for fc in range(NF):
                                slot = (u0 + 2 + fc // 8) % NU
                                wv = ring[:, slot, :].rearrange("p (a b) -> p a b", a=8)
                                k.mm(k.ps[bu][:, 0:CAP], k.psb[bu], wv[:, fc % 8, hc * 128:(hc + 1) * 128], xg[:, fc, :], fc == 0, fc == NF - 1,
                                     reads=[ringb[slot], xgb])
                            k.op("act", lambda e: e.activation(out=s_t[:], in_=k.ps[bg][:, 0:CAP], func=AF.Silu), reads=[k.psb[bg]], writes=[s_b])
                            k.op("dve", lambda e: e.tensor_tensor(out=h_t[:, hc, :], in0=s_t[:], in1=k.ps[bu][:, 0:CAP], op=ALU.mult),
                                 reads=[s_b, k.psb[bu]], writes=[h_b])
                        for j in range(4):
                            issue_unit(u0 + NU + j)
                        for st in range(NST):
                            for n4 in range(4):
                                bk = 6 + (cnt_ev % 2)
                                cnt_ev += 1
                                for hc in range(4):
                                    slot = (u0 + 4 + hc // 2) % NU
                                    wv = ring[:, slot, :].rearrange("p (a b) -> p a b", a=2)
                                    k.mm(k.ps[bk][:], k.psb[bk], h_t[:, hc, st * 128:(st + 1) * 128], wv[:, hc % 2, n4 * 512:(n4 + 1) * 512],
                                         hc == 0, hc == 3, reads=[h_b, ringb[slot]])
                                ya = yacc[:, st, n4 * 512:(n4 + 1) * 512]
                                if el == 0:
                                    k.op("dve", lambda e: e.tensor_scalar(ya, k.ps[bk][:], cwg[:, st, el:el + 1], None, op0=ALU.mult),
                                         reads=[k.psb[bk], cwgb], writes=[yaccb[st]])
                                else:
                                    k.op("dve", lambda e: e.scalar_tensor_tensor(out=ya, in0=k.ps[bk][:], scalar=cwg[:, st, el:el + 1], in1=ya,
                                                                                 op0=ALU.mult, op1=ALU.add),
                                         reads=[k.psb[bk], cwgb, yaccb[st]], writes=[yaccb[st]])
                        for j in range(4, 6):
                            issue_unit(u0 + NU + j)
                    for st in range(NST):
                        k.dma("sp", yg[g, st * 128:(st + 1) * 128, :], yacc[:, st, :], yaccb[st], reads=[yaccb[st]])
        scH.__exit__(None, None, None)
        with Scope(k) as sc:
            ygs, ygsb = sc.sb("ygs", [128, NG, NST, D], BF16)
            for g in range(NG):
                k.dma("pool", ygs[:, g, :, :], yg[g].rearrange("(st p) d -> p st d", p=128), ygsb, writes=[ygsb])
            selT = [sc.sb("selT", [128, NST, NG, 128], BF16) for _ in range(2)]
            xt = [sc.sb("xt", [128, D], F32) for _ in range(2)]
            ot = [sc.sb("ot", [128, D], F32) for _ in range(2)]
            if final_gain is not None:
                fg, fgb = sc.sb("fgain", [128, D], F32)
                k.dma("sp", fg[:], final_gain, fgb, writes=[fgb])
                tmp = rms_tmp(sc)
                ot2 = [sc.sb("ot2", [128, D], F32) for _ in range(2)]
            for t in range(NT):
                x_t, x_b = xt[t % 2]
                o_t, o_b = ot[t % 2]
                sT, sTb = selT[t % 2]
                k.dma("sp", x_t[:], h_in[t * 128:(t + 1) * 128, :], x_b, writes=[x_b])
                for half in range(2):
                    bk = half
                    for j in range(4):
                        g = half * 4 + j
                        k.mm(k.ps[bk][:, j * 128:(j + 1) * 128], k.psb[bk], Eg[:, g, :], rankT[:, t * 128:(t + 1) * 128], True, True,
                             reads=[Egb, rankTb])
                    for st in range(NST):
                        k.op("dve", lambda e: e.tensor_scalar(sT[:, st, half * 4:half * 4 + 4, :], k.ps[bk][:].rearrange("p (a b) -> p a b", a=4),
                                                              slot_idx[:, st:st + 1], None, op0=ALU.is_equal),
                             reads=[k.psb[bk], slib], writes=[sTb])
                for n4 in range(4):
                    bk = 2 + ((t * 4 + n4) % 6)
                    i = 0
                    for g in range(NG):
                        for st in range(NST):
                            k.mm(k.ps[bk][:], k.psb[bk], sT[:, st, g, :], ygs[:, g, st, n4 * 512:(n4 + 1) * 512], i == 0, i == NG * NST - 1,
                                 reads=[sTb, ygsb])
                            i += 1
                    k.op("dve", lambda e: e.tensor_tensor(out=o_t[:, n4 * 512:(n4 + 1) * 512], in0=k.ps[bk][:], in1=x_t[:, n4 * 512:(n4 + 1) * 512],
                                                          op=ALU.add), reads=[k.psb[bk], x_b], writes=[o_b])
                if final_gain is None:
                    k.dma("sp", h_out[t * 128:(t + 1) * 128, :], o_t[:], o_b, reads=[o_b])
                else:
                    o2, o2b = ot2[t % 2]
                    rms_tile(k, sc, o_t[:], o_b, fg[:], fgb, o2[:], o2b, tmp)
                    k.dma("sp", h_out[t * 128:(t + 1) * 128, :], o2[:], o2b, reads=[o2b])


def phase_fox(k, A, h_in, h_out, qTd, kTd, vd, sgd):
    SCALE = 128 ** -0.5
    with Scope(k) as sc0:
        (idb, idbb), (idf, idfb) = make_ident(k, sc0)
        cumT, cumTb = sc0.sb("cumT", [NH, S], F32)
        with Scope(k) as sc1:
            hnT, _ = sc1.sb("hnT", [128, NF, S], BF16)
            hnTb = [sc1.buf() for _ in range(NT)]
            with Scope(k) as sc:
                gain, gainb = sc.sb("gain", [128, D], F32)
                k.dma("sp", gain[:], A["l1_mix_norm"], gainb, writes=[gainb])
                tmp = rms_tmp(sc)
                xt = [sc.sb("xt", [128, D], F32) for _ in range(2)]
                hn = [sc.sb("hn", [128, D], BF16) for _ in range(2)]
                for t in range(NT):
                    x_t, x_b = xt[t % 2]
                    h_t, h_b = hn[t % 2]
                    k.dma("sp", x_t[:], h_in[t * 128:(t + 1) * 128, :], x_b, writes=[x_b])
                    rms_tile(k, sc, x_t[:], x_b, gain[:], gainb, h_t[:], h_b, tmp)
                    for half in range(2):
                        bk = (t % 2) * 2 + half
                        pv = k.ps[bk][:].bitcast(BF16)
                        for j in range(8):
                            fc = half * 8 + j
                            k.tr(pv[:, j * 128:(j + 1) * 128], k.psb[bk], h_t[:, fc * 128:(fc + 1) * 128], idb[:], reads=[h_b, idbb])
                        src = pv.rearrange("p (a b) -> p a b", a=8)
                        dst = hnT[:, half * 8:half * 8 + 8, t * 128:(t + 1) * 128]
                        if half == 0:
                            k.op("act", lambda e: e.activation(out=dst, in_=src, func=AF.Copy), reads=[k.psb[bk]], writes=[hnTb[t]])
                        else:
                            k.op("dve", lambda e: e.tensor_copy(out=dst, in_=src), reads=[k.psb[bk]], writes=[hnTb[t]])
            with Scope(k) as sc:
                NU = 4
                ring, _ = sc.sb("ring", [128, NU, NF, 512], BF16)
                ringb = [sc.buf() for _ in range(NU)]
                win = A["l1_fox_w_in"]

                def issue_unit(u):
                    if u >= 16:
                        return
                    kind, hg = u % 4, u // 4
                    c0 = kind * D + hg * 512
                    src = win[:, c0:c0 + 512].rearrange("(fc p) n -> p fc n", p=128)
                    sl = u % NU
                    for hf in range(2):
                        k.dma("pool", ring[:, sl, hf * 8:hf * 8 + 8, :], src[:, hf * 8:hf * 8 + 8, :], ringb[sl], writes=[ringb[sl]])

                for u in range(NU):
                    issue_unit(u)
                wf, wfb = sc.sb("wf", [128, NF, NH], BF16)
                k.dma("pool", wf[:], win[:, 4 * D:4 * D + NH].rearrange("(fc p) n -> p fc n", p=128), wfb, writes=[wfb])
                bf_, bfb = sc.sb("bfg", [NH, 2], F32)
                k.dma("sp", bf_[:, 0:1], A["l1_fox_b_forget"], bfb, writes=[bfb])
                k.op("dve", lambda e: e.tensor_scalar(bf_[:, 1:2], bf_[:, 0:1], -1.0, None, op0=ALU.mult), reads=[bfb], writes=[bfb])
                lf, lfb = sc.sb("lf", [NH, S], F32)
                ones16, o16b = sc.sb("ones16", [NH, 512], F32)
                k.op("dve", lambda e: e.memset(ones16[:], 1.0), writes=[o16b])
                for nt in range(4):
                    sl = slice(nt * 512, (nt + 1) * 512)
                    bk = nt
                    for fc in range(NF):
                        k.mm(k.ps[bk][0:NH, :], k.psb[bk], wf[:, fc, :], hnT[:, fc, sl], fc == 0, fc == NF - 1,
                             reads=[wfb] + hnTb[nt * 4:nt * 4 + 4])
                    k.op("act", lambda e: e.activation(out=lf[:, sl], in_=k.ps[bk][0:NH, :], func=AF.Exp, scale=-1.0, bias=bf_[:, 1:2]),
                         reads=[k.psb[bk], bfb], writes=[lfb])
                    k.op("act", lambda e: e.activation(out=lf[:, sl], in_=lf[:, sl], func=AF.Ln, bias=1.0, scale=1.0), reads=[lfb], writes=[lfb])
                    ini = 0.0 if nt == 0 else cumT[:, nt * 512 - 1:nt * 512]
                    k.op("dve", lambda e: e.tensor_tensor_scan(out=cumT[:, sl], data0=ones16[:], data1=lf[:, sl], initial=ini, op0=ALU.mult, op1=ALU.add),
                         reads=[lfb, o16b, cumTb], writes=[cumTb])
                qst = [sc.sb("qst", [128, S], BF16) for _ in range(2)]
                vst = [sc.sb("vst", [128, NT, 512], BF16) for _ in range(2)]
                nq = 0
                nb = 0
                for u in range(16):
                    kind, hg = u % 4, u // 4
                    sl_ = u % NU
                    if kind < 2:
                        dstd = qTd if kind == 0 else kTd
                        for hl in range(4):
                            h = hg * 4 + hl
                            q_t, q_b = qst[nq % 2]
                            nq += 1
                            for nt in range(4):
                                sl = slice(nt * 512, (nt + 1) * 512)
                                bk = nb % 4
                                nb += 1
                                for fc in range(NF):
                                    k.mm(k.ps[bk][:], k.psb[bk], ring[:, sl_, fc, hl * 128:(hl + 1) * 128], hnT[:, fc, sl], fc == 0, fc == NF - 1,
                                         reads=[ringb[sl_]] + hnTb[nt * 4:nt * 4 + 4])
                                sc_ = SCALE if kind == 0 else 1.0
                                if nt % 2 == 0:
                                    k.op("act", lambda e: e.activation(out=q_t[:, sl], in_=k.ps[bk][:], func=AF.Copy, scale=sc_), reads=[k.psb[bk]], writes=[q_b])
                                else:
                                    k.op("dve", lambda e: e.tensor_scalar(q_t[:, sl], k.ps[bk][:], sc_, None, op0=ALU.mult), reads=[k.psb[bk]], writes=[q_b])
                            k.dma("sp", dstd[h], q_t[:], q_b, reads=[q_b])
                    else:
                        dstd = vd if kind == 2 else sgd
                        v_t, v_b = vst[kind % 2]
                        for t in range(NT):
                            bk = 4 + (t % 4)
                            for fc in range(NF):
                                k.mm(k.ps[bk][:], k.psb[bk], hnT[:, fc, t * 128:(t + 1) * 128], ring[:, sl_, fc, :], fc == 0, fc == NF - 1,
                                     reads=[ringb[sl_], hnTb[t]])
                            if kind == 2:
                                if t % 2 == 0:
                                    k.op("act", lambda e: e.activation(out=v_t[:, t, :], in_=k.ps[bk][:], func=AF.Copy), reads=[k.psb[bk]], writes=[v_b])
                                else:
                                    k.op("dve", lambda e: e.tensor_copy(out=v_t[:, t, :], in_=k.ps[bk][:]), reads=[k.psb[bk]], writes=[v_b])
                            else:
                                k.op("act", lambda e: e.activation(out=v_t[:, t, :], in_=k.ps[bk][:], func=AF.Sigmoid), reads=[k.psb[bk]], writes=[v_b])
                        k.dma("sp", dstd[:, hg * 512:(hg + 1) * 512].rearrange("(t p) c -> p t c", p=128), v_t[:], v_b, reads=[v_b])
                    issue_unit(u + NU)
        with Scope(k) as sc2:
            ogT, _ = sc2.sb("ogT", [128, NH, S], BF16)
            ogTb = [sc2.buf() for _ in range(NT)]
            with Scope(k) as sc:
                E16, E16b = sc.sb("E16", [NH, NH, 128], F32)
                maskt, maskb = sc.sb("maskt", [128, 128], F32)
                with Scope(k) as sct:
                    i16, i16b = sct.sb("i16", [NH, NH, 128], I32)
                    k.op("pool", lambda e: e.iota(i16[:], pattern=[[1, NH], [0, 128]], base=0, channel_multiplier=-1), writes=[i16b])
                    k.op("dve", lambda e: e.tensor_scalar(E16[:], i16[:], 0, None, op0=ALU.is_equal), reads=[i16b], writes=[E16b])
                    im, imb = sct.sb("im", [128, 128], I32)
                    k.op("pool", lambda e: e.iota(im[:], pattern=[[1, 128]], base=0, channel_multiplier=-1), writes=[imb])
                    k.op("dve", lambda e: e.tensor_scalar(maskt[:], im[:], 0, -30000.0, op0=ALU.is_gt, op1=ALU.mult), reads=[imb], writes=[maskb])
                qh = [sc.sb("qh", [128, S], BF16) for _ in range(2)]
                kh = [sc.sb("kh", [128, S], BF16) for _ in range(2)]
                vg = [sc.sb("vg", [128, NT, 512], BF16) for _ in range(1)]
                sgg = [sc.sb("sgg", [128, NT, 512], BF16) for _ in range(1)]
                ssb = [sc.sb("ssb", [128, S], F32) for _ in range(3)]
                pb = [sc.sb("pb", [128, S], BF16) for _ in range(3)]
                maskbf, maskbfb = sc.sb("maskbf", [128, 128], BF16)
                k.op("dve", lambda e: e.tensor_copy(out=maskbf[:], in_=maskt[:]), reads=[maskb], writes=[maskbfb])
                ptb = [sc.sb("ptb", [128, NT, 128], BF16) for _ in range(2)]
                negck, negckb = sc.sb("negck", [128, S], F32)
                ogt = [sc.sb("ogt", [128, 128], BF16) for _ in range(2)]

                def load_head(h):
                    k.dma("sp", qh[h % 2][0][:], qTd[h], qh[h % 2][1], writes=[qh[h % 2][1]])
                    k.dma("sp", kh[h % 2][0][:], kTd[h], kh[h % 2][1], writes=[kh[h % 2][1]])

                def load_group(hg):
                    k.dma("sp", vg[0][0][:], vd[:, hg * 512:(hg + 1) * 512].rearrange("(t p) c -> p t c", p=128), vg[0][1], writes=[vg[0][1]])
                    k.dma("sp", sgg[0][0][:], sgd[:, hg * 512:(hg + 1) * 512].rearrange("(t p) c -> p t c", p=128), sgg[0][1], writes=[sgg[0][1]])

                st4s = [sc.sb("st4", [128, 8], F32) for _ in range(3)]
                load_head(0)

                def head_setup(h):
                    for nt in range(4):
                        sl = slice(nt * 512, (nt + 1) * 512)
                        bk = 4 + (nt % 2)
                        k.mm(k.ps[bk][:], k.psb[bk], E16[:, h, :], cumT[:, sl], True, True, reads=[E16b, cumTb])
                        k.op("act", lambda e: e.activation(out=negck[:, sl], in_=k.ps[bk][:], func=AF.Copy), reads=[k.psb[bk]], writes=[negckb])

                def stage_A(it, h, qt):
                    if qt == 0:
                        head_setup(h)
                    q_t, q_b = qh[h % 2]
                    k_t, k_b = kh[h % 2]
                    nk = (qt + 1) * 128
                    nb_ = (nk + 511) // 512
                    s_t, s_b = ssb[it % 3]
                    p_t, p_b = pb[it % 3]
                    s4, s4b = st4s[it % 3]
                    for b in range(nb_):
                        w = min(512, nk - b * 512)
                        last = (b == nb_ - 1)
                        k.mm(k.ps[b][:, 0:w], k.psb[b], q_t[:, qt * 128:(qt + 1) * 128], k_t[:, b * 512:b * 512 + w], True, not last, reads=[q_b, k_b])
                        if last:
                            k.mm(k.ps[b][:, w - 128:w], k.psb[b], idb[:], maskbf[:], False, True, reads=[idbb, maskbfb])
                        k.op("dve", lambda e: e.tensor_tensor(out=s_t[:, b * 512:b * 512 + w], in0=k.ps[b][:, 0:w], in1=negck[:, b * 512:b * 512 + w], op=ALU.add),
                             reads=[k.psb[b], negckb], writes=[s_b])
                    k.op("dve", lambda e: e.tensor_reduce(out=s4[:, 1:2], in_=s_t[:, 0:nk], axis=AX.X, op=ALU.max, negate=True), reads=[s_b, s4b], writes=[s4b])
                    k.op("act", lambda e: e.activation(out=p_t[:, 0:nk], in_=s_t[:, 0:nk], func=AF.Exp, bias=s4[:, 1:2], scale=1.0, accum_out=s4[:, 2:3]),
                         reads=[s_b, s4b], writes=[p_b, s4b])

                pvb = [Buf("pv6"), Buf("pv7")]
                tgb = [Buf("tg6"), Buf("tg7")]

                def stage_B(it, h, qt):
                    hg, hl = h // 4, h % 4
                    if qt == 0:
                        if h + 1 < NH:
                            load_head(h + 1)
                    p_t, p_b = pb[it % 3]
                    pt_t, pt_b = ptb[it % 2]
                    for c8 in range((qt + 8) // 8):
                        bk = 4 + (c8 % 2)
                        pv = k.ps[bk][:].bitcast(BF16)
                        n8 = min(8, qt + 1 - c8 * 8)
                        for j in range(n8):
                            kt = c8 * 8 + j
                            k.tr(pv[:, j * 128:(j + 1) * 128], k.psb[bk], p_t[:, kt * 128:(kt + 1) * 128], idb[:], reads=[p_b, idbb])
                        k.op("act", lambda e: e.activation(out=pt_t[:, c8 * 8:c8 * 8 + n8, :], in_=pv[:, 0:n8 * 128].rearrange("p (a b) -> p a b", a=n8), func=AF.Copy),
                             reads=[k.psb[bk]], writes=[pt_b])

                def stage_C1(it, h, qt):
                    hg, hl = h // 4, h % 4
                    if qt == 0 and hl == 0:
                        load_group(hg)
                    v_t, v_b = vg[0]
                    g_t, g_b = sgg[0]
                    pt_t, pt_b = ptb[it % 2]
                    o_t, o_b = ogt[it % 2]
                    s4, s4b = st4s[it % 3]
                    po = k.ps[6 + (it % 2)][:, 0:128]
                    for kt in range(qt + 1):
                        k.mm(po, k.psb[6 + (it % 2)], pt_t[:, kt, :], v_t[:, kt, hl * 128:(hl + 1) * 128], kt == 0, kt == qt, reads=[pt_b, v_b])
                    k.op("dve", lambda e: e.reciprocal(s4[:, 3:4], s4[:, 2:3]), reads=[s4b], writes=[s4b])
                    k.op("dve", lambda e: e.scalar_tensor_tensor(out=o_t[:], in0=po, scalar=s4[:, 3:4], in1=g_t[:, qt, hl * 128:(hl + 1) * 128],
                                                                 op0=ALU.mult, op1=ALU.mult), reads=[k.psb[6 + (it % 2)], s4b, g_b], writes=[o_b])

                def stage_C2(it, h, qt):
                    o_t, o_b = ogt[it % 2]
                    pv7 = k.ps[6 + (it % 2)][:].bitcast(BF16)
                    to = pv7[:, 512:640]
                    k.tr(to, k.psb[6 + (it % 2)], o_t[:], idb[:], reads=[o_b, idbb])
                    k.op("act", lambda e: e.activation(out=ogT[:, h, qt * 128:(qt + 1) * 128], in_=to, func=AF.Copy), reads=[k.psb[6 + (it % 2)]], writes=[ogTb[qt]])

                aitems = [(h, qt) for h in range(NH) for qt in range(NT)]
                NI = len(aitems)
                for n in range(-3, NI):
                    if 0 <= n + 3 < NI:
                        stage_A(n + 3, *aitems[n + 3])
                    if 0 <= n + 2 < NI:
                        stage_B(n + 2, *aitems[n + 2])
                    if 0 <= n + 1 < NI:
                        stage_C1(n + 1, *aitems[n + 1])
                    if 0 <= n < NI:
                        stage_C2(n, *aitems[n])
            with Scope(k) as sc:
                wo, wob = sc.sb("wo", [128, NH, D], BF16)
                wv = A["l1_fox_w_out"].rearrange("(c p) n -> p c n", p=128)
                for i in range(4):
                    k.dma("pool", wo[:, 4 * i:4 * i + 4, :], wv[:, 4 * i:4 * i + 4, :], wob, writes=[wob])
                xt = [sc.sb("xt", [128, D], F32) for _ in range(2)]
                ot = [sc.sb("ot", [128, D], F32) for _ in range(2)]
                for t in range(NT):
                    x_t, x_b = xt[t % 2]
                    o_t, o_b = ot[t % 2]
                    k.dma("sp", x_t[:], h_in[t * 128:(t + 1) * 128, :], x_b, writes=[x_b])
                    for n4 in range(4):
                        bk = (t % 2) * 4 + n4
                        for c in range(NH):
                            k.mm(k.ps[bk][:], k.psb[bk], ogT[:, c, t * 128:(t + 1) * 128], wo[:, c, n4 * 512:(n4 + 1) * 512], c == 0, c == NH - 1,
                                 reads=[ogTb[t], wob])
                        k.op("dve", lambda e: e.tensor_tensor(out=o_t[:, n4 * 512:(n4 + 1) * 512], in0=k.ps[bk][:], in1=x_t[:, n4 * 512:(n4 + 1) * 512], op=ALU.add),
                             reads=[k.psb[bk], x_b], writes=[o_b])
                    k.dma("sp", h_out[t * 128:(t + 1) * 128, :], o_t[:], o_b, reads=[o_b])


_CACHE = {}


def build_full(arr_shapes):
    k = K()
    nc = k.nc
    A = {}
    for nm, shp in arr_shapes.items():
        A[nm] = nc.dram_tensor(nm, list(shp), F32, kind="ExternalInput").ap()
    out = nc.dram_tensor("out", [S, D], F32, kind="ExternalOutput").ap()
    hA = nc.dram_tensor("hA", [S, D], F32, kind="Internal").ap()
    hB = nc.dram_tensor("hB", [S, D], F32, kind="Internal").ap()
    yg = nc.dram_tensor("yg", [NG, CAP, D], F32, kind="Internal").ap()
    qTd = nc.dram_tensor("qTd", [NH, 128, S], BF16, kind="Internal").ap()
    kTd = nc.dram_tensor("kTd", [NH, 128, S], BF16, kind="Internal").ap()
    vd = nc.dram_tensor("vd", [S, D], BF16, kind="Internal").ap()
    sgd = nc.dram_tensor("sgd", [S, D], BF16, kind="Internal").ap()
    gTd = nc.dram_tensor("gTd", [NQ, 128, S], BF16, kind="Internal").ap()
    phase_s5(k, A, A["x"], hA, gTd)
    phase_moe(k, A, "l0", hA, hB, yg)
    phase_fox(k, A, hB, hA, qTd, kTd, vd, sgd)
    phase_moe(k, A, "l1", hA, out, yg, final_gain=A["final_norm"])
    k.barrier()
    return k


def kernel(**inputs):
    x = np.asarray(inputs["x"], np.float32)
    arrs = prep_common(inputs)
    shapes = {nm: a.shape for nm, a in arrs.items()}
    shapes["x"] = (S, D)
    k = build_full(shapes)
    in_maps = []
    for c in range(8):
        m = dict(arrs)
        m["x"] = np.ascontiguousarray(x[c])
        in_maps.append(m)
    res = run_bass_kernel_spmd(k.nc, in_maps, core_ids=list(range(8)))
    return np.stack([np.asarray(r["out"], np.float32) for r in res.results], axis=0)
```
